# Optimizing a Trainium2 kernel written in Bass

```python
import jax, jax.numpy as jnp
from jax import lax
import numpy as np

D_MODEL = 1024
BATCH = 16
SEQ = 4096
DEPTH = 1

HEAD_DIM = 64
DILATED_GROUPS = ((128, 1), (512, 4), (2048, 16))
N_ATT_GROUPS = len(DILATED_GROUPS)
HEADS_PER_GROUP = 4
ATT_HEADS = N_ATT_GROUPS * HEADS_PER_GROUP
ATT_WIDTH = ATT_HEADS * HEAD_DIM
ATT_OUT_WIDTH = HEADS_PER_GROUP * HEAD_DIM
BAND_BLOCK = 128
ROPE_THETA = 10000.0

GMLP_CHUNK = 128
GMLP_GROUPS = 4
GMLP_WIDTH = 512
GMLP_GROUP_WIDTH = GMLP_WIDTH // GMLP_GROUPS

IN_COLS = 3 * ATT_WIDTH + 2 * GMLP_WIDTH + 2 * D_MODEL

N_MEM = 256
XATTN_HEADS = 4
XATTN_HEAD_DIM = D_MODEL // XATTN_HEADS

N_EXPERT_GROUPS = 4
EXPERTS_PER_GROUP = 8
N_EXPERTS = N_EXPERT_GROUPS * EXPERTS_PER_GROUP
TOP_K_INNER = 2
EXPERT_FF = D_MODEL // 2
MOE_BLOCK = 128

RMS_EPS = 1e-6
LN_EPS = 1e-5
NEG_INF = -1e30

kernel_name = "hybrid_dilated_gmlp_hmoe_block"


def rmsnorm(x, g):
    xf = x.astype(jnp.float32)
    y = xf * lax.rsqrt(jnp.mean(xf * xf, axis=-1, keepdims=True) + RMS_EPS)
    return (y * g.astype(jnp.float32)).astype(x.dtype)


def layernorm(x, g, b):
    xf = x.astype(jnp.float32)
    mu = jnp.mean(xf, axis=-1, keepdims=True)
    var = jnp.mean(jnp.square(xf - mu), axis=-1, keepdims=True)
    y = (xf - mu) * lax.rsqrt(var + LN_EPS)
    return (y * g.astype(jnp.float32) + b.astype(jnp.float32)).astype(x.dtype)


def rope(x, positions):
    half = x.shape[-1] // 2
    inv_freq = ROPE_THETA ** (-jnp.arange(half, dtype=jnp.float32) / half)
    ang = positions.astype(jnp.float32)[..., None] * inv_freq
    cos = jnp.cos(ang)[:, :, None, :]
    sin = jnp.sin(ang)[:, :, None, :]
    xf = x.astype(jnp.float32)
    x1, x2 = xf[..., :half], xf[..., half:]
    return jnp.concatenate([x1 * cos - x2 * sin, x2 * cos + x1 * sin], axis=-1).astype(x.dtype)


def dilated_window_attention(q, k, v, window, dilation):
    B, S, H, Dh = q.shape
    span = window // dilation
    L = S // dilation
    nb = -(-L // BAND_BLOCK)
    Lp = nb * BAND_BLOCK

    def to_strided(t):
        t = t.reshape(B, L, dilation, H, Dh).transpose(0, 2, 1, 3, 4)
        t = jnp.pad(t, ((0, 0), (0, 0), (0, Lp - L), (0, 0), (0, 0)))
        return t.reshape(B, dilation, nb, BAND_BLOCK, H, Dh)

    def with_prev(t):
        prev = jnp.pad(t, ((0, 0), (0, 0), (1, 0), (0, 0), (0, 0), (0, 0)))[:, :, :-1]
        return jnp.concatenate([prev, t], axis=3)

    qb = to_strided(q)
    kk = with_prev(to_strided(k))
    vv = with_prev(to_strided(v))

    s = jnp.einsum('brnqhd,brnkhd->brnhqk', qb, kk,
                   preferred_element_type=jnp.float32) * (Dh ** -0.5)
    qi = jnp.arange(BAND_BLOCK)[:, None]
    kc = jnp.arange(2 * BAND_BLOCK)[None, :]
    dist = qi + BAND_BLOCK - kc
    band = (dist >= 0) & (dist <= span)
    blk = jnp.arange(nb)[:, None, None]
    mask = band[None] & ((blk > 0) | (kc[None] >= BAND_BLOCK))
    s = jnp.where(mask[None, None, :, None], s, NEG_INF)

    m = jnp.max(s, axis=-1, keepdims=True)
    p = jnp.exp(s - m)
    den = jnp.sum(p, axis=-1, keepdims=True)
    lse = (m + jnp.log(den))[..., 0]
    o = jnp.einsum('brnhqk,brnkhd->brnqhd', p / den, vv.astype(jnp.float32))

    o = o.reshape(B, dilation, Lp, H, Dh)[:, :, :L].transpose(0, 2, 1, 3, 4).reshape(B, S, H, Dh)
    lse = lse.transpose(0, 1, 2, 4, 3).reshape(B, dilation, Lp, H)[:, :, :L]
    lse = lse.transpose(0, 2, 1, 3).reshape(B, S, H)
    return o, lse


def dilated_mixture(q, k, v):
    outs, lses = [], []
    for g, (window, dilation) in enumerate(DILATED_GROUPS):
        sl = slice(g * HEADS_PER_GROUP, (g + 1) * HEADS_PER_GROUP)
        o, l = dilated_window_attention(q[:, :, sl], k[:, :, sl], v[:, :, sl], window, dilation)
        outs.append(o)
        lses.append(l)
    wts = jax.nn.softmax(jnp.stack(lses, axis=0), axis=0)
    o = jnp.sum(wts[..., None] * jnp.stack(outs, axis=0), axis=0)
    B, S = o.shape[:2]
    return o.reshape(B, S, ATT_OUT_WIDTH)


def chunked_spatial_gating(zb, w_spatial, b_spatial, v_norm_g, v_norm_b):
    B, S, _ = zb.shape
    z = jax.nn.gelu(zb)
    u, v = z[..., :GMLP_WIDTH], z[..., GMLP_WIDTH:]
    v = layernorm(v, v_norm_g, v_norm_b)
    causal = jnp.tril(jnp.ones((GMLP_CHUNK, GMLP_CHUNK), dtype=bool))
    ws = jnp.where(causal[None], w_spatial, 0.0).astype(v.dtype)
    vc = v.reshape(B, S // GMLP_CHUNK, GMLP_CHUNK, GMLP_GROUPS, GMLP_GROUP_WIDTH)
    mixed = jnp.einsum('gts,bnsgc->bntgc', ws, vc) + b_spatial.T[:, :, None].astype(v.dtype)
    return u * mixed.reshape(B, S, GMLP_WIDTH)


def hybrid_mixer(xn, positions, w_in, b_gates, w_spatial, b_spatial, v_norm_g, v_norm_b,
                 w_out_a, w_out_b, w_out):
    B, S, D = xn.shape
    proj = xn @ w_in
    cuts = np.cumsum([ATT_WIDTH, ATT_WIDTH, ATT_WIDTH, 2 * GMLP_WIDTH]).tolist()
    q, k, v, zb, gate_logits = jnp.split(proj, cuts, axis=-1)
    q = rope(q.reshape(B, S, ATT_HEADS, HEAD_DIM), positions)
    k = rope(k.reshape(B, S, ATT_HEADS, HEAD_DIM), positions)
    v = v.reshape(B, S, ATT_HEADS, HEAD_DIM)
    y_a = dilated_mixture(q, k, v).astype(xn.dtype)
    y_b = chunked_spatial_gating(zb, w_spatial, b_spatial, v_norm_g, v_norm_b)
    gates = jax.nn.sigmoid(gate_logits + b_gates)
    g_a, g_b = gates[..., :D], gates[..., D:]
    merged = g_a * (y_a @ w_out_a) + g_b * (y_b @ w_out_b)
    return merged @ w_out


def memory_cross_attention(hn, mn, w_q, w_kv, w_o):
    B, S, D = hn.shape
    M = mn.shape[1]
    q = (hn @ w_q).reshape(B, S, XATTN_HEADS, XATTN_HEAD_DIM)
    kv = (mn @ w_kv).reshape(B, M, 2, XATTN_HEADS, XATTN_HEAD_DIM)
    k, v = kv[:, :, 0], kv[:, :, 1]
    s = jnp.einsum('bshd,bmhd->bhsm', q, k,
                   preferred_element_type=jnp.float32) * (XATTN_HEAD_DIM ** -0.5)
    p = jax.nn.softmax(s, axis=-1)
    o = jnp.einsum('bhsm,bmhd->bshd', p.astype(v.dtype), v).reshape(B, S, D)
    return o @ w_o


def hierarchical_moe(hn, w_router_grp, b_router_grp, w_router_exp, b_router_exp,
                     w_gate_e, w_up_e, w_down_e):
    B, S, D = hn.shape
    T = B * S
    xt = hn.reshape(T, D)
    grp_logits = (xt @ w_router_grp).astype(jnp.float32) + b_router_grp.astype(jnp.float32)
    grp_prob = jax.nn.softmax(grp_logits, axis=-1)
    grp = jnp.argmax(grp_logits, axis=-1).astype(jnp.int32)
    grp_gate = jnp.take_along_axis(grp_prob, grp[:, None], axis=1)[:, 0]
    exp_logits = ((xt @ w_router_exp).astype(jnp.float32) + b_router_exp.astype(jnp.float32))
    exp_logits = exp_logits.reshape(T, N_EXPERT_GROUPS, EXPERTS_PER_GROUP)
    in_grp = jnp.take_along_axis(exp_logits, grp[:, None, None], axis=1)[:, 0]
    top_v, top_i = lax.top_k(in_grp, TOP_K_INNER)
    gate = grp_gate[:, None] * jax.nn.softmax(top_v, axis=-1)

    A = T * TOP_K_INNER
    eid = (grp[:, None] * EXPERTS_PER_GROUP + top_i.astype(jnp.int32)).reshape(A)
    wgt = gate.reshape(A)
    e_sorted, a_sorted = lax.sort((eid, jnp.arange(A, dtype=jnp.int32)), num_keys=1, is_stable=True)
    tok_sorted = a_sorted // TOP_K_INNER
    w_sorted = wgt[a_sorted]

    counts = jax.ops.segment_sum(jnp.ones((A,), jnp.int32), eid, num_segments=N_EXPERTS)
    padded = ((counts + MOE_BLOCK - 1) // MOE_BLOCK) * MOE_BLOCK
    start = jnp.cumsum(counts) - counts
    pend = jnp.cumsum(padded)
    pstart = pend - padded
    dest = pstart[e_sorted] + (jnp.arange(A, dtype=jnp.int32) - start[e_sorted])
    P = A + N_EXPERTS * MOE_BLOCK
    n_blocks = P // MOE_BLOCK
    tok_buf = jnp.full((P,), T, jnp.int32).at[dest].set(tok_sorted)
    w_buf = jnp.zeros((P,), jnp.float32).at[dest].set(w_sorted)
    blk_start = jnp.arange(n_blocks, dtype=jnp.int32) * MOE_BLOCK
    blk_expert = jnp.minimum(jnp.searchsorted(pend, blk_start, side='right'),
                             N_EXPERTS - 1).astype(jnp.int32)

    def expert_block(args):
        idx, e = args
        xb = jnp.take(xt, idx, axis=0, mode='fill', fill_value=0)
        hb = jax.nn.silu(xb @ w_gate_e[e]) * (xb @ w_up_e[e])
        return hb @ w_down_e[e]

    yb = lax.map(expert_block, (tok_buf.reshape(n_blocks, MOE_BLOCK), blk_expert))
    yb = yb.reshape(P, D) * w_buf[:, None].astype(yb.dtype)
    y = jnp.zeros((T, D), yb.dtype).at[tok_buf].add(yb, mode='drop')
    return y.reshape(B, S, D)


def setup_inputs(seed: int = 0) -> dict:
    key = jax.random.key(seed)
    ks = iter(jax.random.split(key, 40))
    f32 = jnp.float32
    L, D = DEPTH, D_MODEL

    def nrm(shape, scale):
        return jax.random.normal(next(ks), shape, f32) * scale

    def gain(shape):
        return 1.0 + nrm(shape, 0.02)

    x = nrm((BATCH, SEQ, D), 1.0)
    mem = nrm((BATCH, N_MEM, D), 1.0)
    positions = (jnp.arange(SEQ, dtype=jnp.int32)[None, :]
                 + jax.random.randint(next(ks), (BATCH, 1), 0, 512, dtype=jnp.int32))
    return {
        "x": x,
        "mem": mem,
        "positions": positions,
        "mix_norm_g": gain((L, D)),
        "w_in": nrm((L, D, IN_COLS), D ** -0.5),
        "b_gates": nrm((L, 2 * D), 0.1),
        "w_spatial": nrm((L, GMLP_GROUPS, GMLP_CHUNK, GMLP_CHUNK), GMLP_CHUNK ** -0.5),
        "b_spatial": 1.0 + nrm((L, GMLP_GROUPS, GMLP_CHUNK), 0.02),
        "v_norm_g": gain((L, GMLP_WIDTH)),
        "v_norm_b": nrm((L, GMLP_WIDTH), 0.02),
        "w_out_a": nrm((L, ATT_OUT_WIDTH, D), ATT_OUT_WIDTH ** -0.5),
        "w_out_b": nrm((L, GMLP_WIDTH, D), GMLP_WIDTH ** -0.5),
        "w_out": nrm((L, D, D), D ** -0.5),
        "xattn_norm_g": gain((L, D)),
        "mem_norm_g": gain((L, D)),
        "w_q_x": nrm((L, D, D), D ** -0.5),
        "w_kv_x": nrm((L, D, 2 * D), D ** -0.5),
        "w_o_x": nrm((L, D, D), D ** -0.5),
        "moe_norm_g": gain((L, D)),
        "w_router_grp": nrm((L, D, N_EXPERT_GROUPS), D ** -0.5),
        "b_router_grp": nrm((L, N_EXPERT_GROUPS), 0.01),
        "w_router_exp": nrm((L, D, N_EXPERTS), D ** -0.5),
        "b_router_exp": nrm((L, N_EXPERTS), 0.01),
        "w_gate_e": nrm((L, N_EXPERTS, D, EXPERT_FF), D ** -0.5),
        "w_up_e": nrm((L, N_EXPERTS, D, EXPERT_FF), D ** -0.5),
        "w_down_e": nrm((L, N_EXPERTS, EXPERT_FF, D), EXPERT_FF ** -0.5),
        "final_norm_g": gain((D,)),
    }


def reference(x, mem, positions, mix_norm_g, w_in, b_gates, w_spatial, b_spatial, v_norm_g,
              v_norm_b, w_out_a, w_out_b, w_out, xattn_norm_g, mem_norm_g, w_q_x, w_kv_x,
              w_o_x, moe_norm_g, w_router_grp, b_router_grp, w_router_exp, b_router_exp,
              w_gate_e, w_up_e, w_down_e, final_norm_g):
    h = x
    for layer in range(DEPTH):
        h = h + hybrid_mixer(rmsnorm(h, mix_norm_g[layer]), positions, w_in[layer],
                             b_gates[layer], w_spatial[layer], b_spatial[layer],
                             v_norm_g[layer], v_norm_b[layer], w_out_a[layer],
                             w_out_b[layer], w_out[layer])
        h = h + memory_cross_attention(rmsnorm(h, xattn_norm_g[layer]),
                                       rmsnorm(mem, mem_norm_g[layer]),
                                       w_q_x[layer], w_kv_x[layer], w_o_x[layer])
        h = h + hierarchical_moe(rmsnorm(h, moe_norm_g[layer]), w_router_grp[layer],
                                 b_router_grp[layer], w_router_exp[layer], b_router_exp[layer],
                                 w_gate_e[layer], w_up_e[layer], w_down_e[layer])
    return rmsnorm(h, final_norm_g)
```

```python
import math
from contextlib import ExitStack

import numpy as np
import concourse.bass as bass
import concourse.mybir as mybir
from concourse.bass_utils import run_bass_kernel_spmd

F32 = mybir.dt.float32
BF16 = mybir.dt.bfloat16
I32 = mybir.dt.int32
ALU = mybir.AluOpType
AF = mybir.ActivationFunctionType
AX = mybir.AxisListType

D = 1024
KC = 8
NMEM = 256
NEXP = 32
FF = 512
RMS_EPS = 1e-6
LN_EPS = 1e-5
TWO_PI = 2.0 * math.pi
CW1 = 6.28125
CW2 = TWO_PI - CW1
PI_SAFE = 3.1415925

ENGS = ("pe", "act", "dve", "pool", "sp")


class Buf:
    __slots__ = ("name", "w", "r")

    def __init__(self, name):
        self.name = name
        self.w = None
        self.r = []


class Op:
    __slots__ = ("eng", "fn", "dma", "deps", "signal", "count", "sem", "semval", "prev_on_sem")

    def __init__(self, eng, fn, dma):
        self.eng = eng
        self.fn = fn
        self.dma = dma
        self.deps = []
        self.signal = False
        self.count = 0
        self.sem = None
        self.semval = 0
        self.prev_on_sem = None


class Sched:
    def __init__(self, n_dma_sems=40):
        self.ops = {e: [] for e in ENGS}
        self.n_dma_sems = n_dma_sems
        self.dma_rr = 0
        self.dma_last = [None] * n_dma_sems
        self.dma_val = [0] * n_dma_sems
        self.nops = 0
        self.pool_regs = {}
        self.pool_reg_handles = {}

    def add(self, eng, fn, reads=(), writes=(), dma=False):
        op = Op(eng, fn, dma)
        self.nops += 1
        deps = {}
        for b in reads:
            if b.w is not None:
                deps[id(b.w)] = (b.w, "raw")
        for b in writes:
            if b.w is not None and id(b.w) not in deps:
                deps[id(b.w)] = (b.w, "waw")
            for r in b.r:
                if id(r) not in deps:
                    deps[id(r)] = (r, "war")
        for b in reads:
            b.r.append(op)
        for b in writes:
            b.w = op
            b.r = []
        for d, kind in deps.values():
            if d is op:
                continue
            if d.dma:
                op.deps.append(d)
            elif d.eng != eng:
                op.deps.append(d)
                d.signal = True
            elif kind == "raw" and eng != "pe":
                op.deps.append(d)
                d.signal = True
        if dma:
            s = self.dma_rr
            self.dma_rr = (self.dma_rr + 1) % self.n_dma_sems
            op.sem = s
            op.prev_on_sem = self.dma_last[s]
            self.dma_val[s] += 16
            op.semval = self.dma_val[s]
            self.dma_last[s] = op
        self.ops[eng].append(op)
        return op

    def emit(self, block, esems, dsems):
        for e in ENGS:
            c = 0
            for op in self.ops[e]:
                if not op.dma and op.signal:
                    c += 1
                    op.count = c
        sched = self

        def run(e, eng):
            seen = {}
            for op in sched.ops[e]:
                best = {}
                if op.dma and op.prev_on_sem is not None:
                    p = op.prev_on_sem
                    best[("d", p.sem)] = p.semval
                for d in op.deps:
                    k = ("d", d.sem) if d.dma else ("e", d.eng)
                    v = d.semval if d.dma else d.count
                    if v > best.get(k, 0):
                        best[k] = v
                for k, v in best.items():
                    if seen.get(k, 0) >= v:
                        continue
                    seen[k] = v
                    eng.wait_ge(dsems[k[1]] if k[0] == "d" else esems[k[1]], v)
                ins = op.fn(eng)
                if op.dma:
                    ins.then_inc(dsems[op.sem], 16)
                elif op.signal:
                    ins.then_inc(esems[e], 1)

        @block.tensor
        def _(eng):
            run("pe", eng)

        @block.scalar
        def _(eng):
            run("act", eng)

        @block.vector
        def _(eng):
            run("dve", eng)

        @block.gpsimd
        def _(eng):
            for name_, val_ in sched.pool_regs.items():
                r_ = eng.alloc_register(name_)
                eng.reg_mov(r_, val_)
                sched.pool_reg_handles[name_] = r_
            run("pool", eng)

        @block.sync
        def _(eng):
            run("sp", eng)
            for s_ in range(sched.n_dma_sems):
                if sched.dma_val[s_] > 0:
                    eng.wait_ge(dsems[s_], sched.dma_val[s_])


class Arena:
    def __init__(self, base_ap, nbytes):
        self.base = base_ap
        self.cap = nbytes
        self.top = 0
        self.hi = nbytes
        self.live = []
        self.freed = []
        self.peak = 0

    def alloc(self, name, free_shape, dtype, nbufs=1, parts=128, top=False):
        esz = 2 if dtype == BF16 else 4
        n = int(np.prod(free_shape))
        nbytes = (n * esz + 63) // 64 * 64
        if top:
            end = self.hi
            start = end - nbytes
            assert start >= self.top, f"SBUF arena overflow (top) allocating {name}"
            self.hi = start
        else:
            start = self.top
            end = start + nbytes
            assert end <= self.hi, f"SBUF arena overflow allocating {name}: {end} > {self.hi}"
            self.top = end
        self.peak = max(self.peak, self.top + (self.cap - self.hi))
        v = self.base[0:parts, start // 2:start // 2 + n * esz // 2]
        if dtype != BF16:
            v = v.bitcast(dtype)
        if len(free_shape) == 2:
            v = v.rearrange("p (a b) -> p a b", a=free_shape[0])
        elif len(free_shape) == 3:
            v = v.rearrange("p (a b c) -> p a b c", a=free_shape[0], b=free_shape[1])
        hz = []
        for (s, e, ops) in self.freed:
            if s < end and e > start:
                hz.extend(ops)
        bufs = []
        for i in range(nbufs):
            b = Buf(f"{name}{i}")
            b.r = list(hz)
            bufs.append(b)
        self.live.append((start, end, bufs))
        return (v, bufs[0]) if nbufs == 1 else (v, bufs)

    def mark(self):
        return self.top

    def release_top(self):
        keep = []
        for (s, e, bufs) in self.live:
            if s >= self.hi:
                ops = []
                for b in bufs:
                    if b.w is not None:
                        ops.append(b.w)
                    ops.extend(b.r)
                self.freed.append((s, e, ops))
            else:
                keep.append((s, e, bufs))
        self.live = keep
        self.hi = self.cap

    def release(self, mark):
        keep = []
        for (s, e, bufs) in self.live:
            if s >= mark and e <= self.hi:
                ops = []
                for b in bufs:
                    if b.w is not None:
                        ops.append(b.w)
                    ops.extend(b.r)
                self.freed.append((s, e, ops))
            else:
                keep.append((s, e, bufs))
        self.live = keep
        self.top = mark


def build(NB, S, C, dev=False):
    NT = S // 128
    NBK = S // 512
    TOK = NB * S
    NTT = TOK // 128
    CT = C // 128
    NSLOT = NEXP * C
    DIL = (1, 4, 16)

    nc = bass.Bass("TRN2", target_bir_lowering=False)

    def din(name, shape, dt=F32):
        return nc.dram_tensor(name, shape, dt, kind="ExternalInput").ap()

    x = din("x", [NB, S, D])
    mem = din("mem", [NB, NMEM, D])
    pos = din("pos", [NB, S], I32)
    w_in = din("w_in", [D, 5376])
    g_mix = din("g_mix", [D])
    g_x = din("g_x", [D])
    g_mem = din("g_mem", [D])
    g_moe = din("g_moe", [D])
    g_fin = din("g_fin", [D])
    bgt = din("bgt", [128, 16])
    ws_t = din("ws_t", [4, 128, 128])
    bsp = din("bsp", [512])
    vgam = din("vgam", [512])
    vbet_t = din("vbet_t", [128, 4])
    w_oa = din("w_oa", [256, D])
    w_ob = din("w_ob", [512, D])
    w_o = din("w_o", [D, D])
    w_qx = din("w_qx", [D, D])
    w_kvx = din("w_kvx", [D, 2 * D])
    w_ox = din("w_ox", [D, D])
    w_r = din("w_r", [D, 36])
    b_r = din("b_r", [36])
    w_ge = din("w_ge", [NEXP, D, FF])
    w_ue = din("w_ue", [NEXP, D, FF])
    w_de = din("w_de", [NEXP, FF, D])
    c_invf = din("c_invf", [128, 1])
    c_mask = din("c_mask", [128, 256])
    c_ltri = din("c_ltri", [128, 128])
    c_ident = din("c_ident", [128, 128])
    c_ec = din("c_ec", [128, 32])
    c_sel = din("c_sel", [65, 64])
    c_iota = din("c_iota", [128, 1])

    out = nc.dram_tensor("out", [TOK, D], F32, kind="ExternalOutput").ap()
    cnt_out = nc.dram_tensor("cnt", [128, 32], F32, kind="ExternalOutput").ap()
    dbg = {}
    if dev:
        dbg["ya"] = nc.dram_tensor("dbg_ya", [NB, 64, 4, S], F32, kind="ExternalOutput").ap()
        dbg["yb"] = nc.dram_tensor("dbg_yb", [NB, 128, 4, S], F32, kind="ExternalOutput").ap()
        dbg["h1"] = nc.dram_tensor("dbg_h1", [TOK, D], F32, kind="ExternalOutput").ap()
        dbg["h2"] = nc.dram_tensor("dbg_h2", [TOK, D], F32, kind="ExternalOutput").ap()
        dbg["dest"] = nc.dram_tensor("dbg_dest", [128, NTT, 2], I32, kind="ExternalOutput").ap()
        dbg["slot"] = nc.dram_tensor("dbg_slot", [NSLOT, 2], I32, kind="ExternalOutput").ap()
        dbg["ys"] = nc.dram_tensor("dbg_ys", [NSLOT + 128, D], F32, kind="ExternalOutput").ap()
        dbg["hn3"] = nc.dram_tensor("dbg_hn3", [TOK + 128, D], BF16, kind="ExternalOutput").ap()

    COS = nc.dram_tensor("scr_cos", [NB, 128, S], F32, kind="Internal").ap()
    SIN = nc.dram_tensor("scr_sin", [NB, 128, S], F32, kind="Internal").ap()
    H = nc.dram_tensor("scr_h", [TOK, D], F32, kind="Internal").ap()
    XN = nc.dram_tensor("scr_xn", [NB, 128, KC, S], BF16, kind="Internal").ap()
    HN3 = nc.dram_tensor("scr_hn3", [TOK + 128, D], BF16, kind="Internal").ap()
    SLOT = nc.dram_tensor("scr_slot", [NSLOT + 128, 2], I32, kind="Internal").ap()
    YS = nc.dram_tensor("scr_ys", [NSLOT + 128, D], F32, kind="Internal").ap()

    ARENA_BYTES = 204 * 1024
    es = ExitStack()
    with es:
        arena_t = es.enter_context(nc.sbuf_tensor("arena", [128, ARENA_BYTES // 2], BF16))
        banks = [es.enter_context(nc.psum_tensor(f"bank{i}", [128, 512], F32)) for i in range(8)]
        esems = {e: es.enter_context(nc.semaphore("es_" + e)) for e in ENGS}
        NDS = 40
        dsems = [es.enter_context(nc.semaphore(f"ds{i}")) for i in range(NDS)]
        block = es.enter_context(nc.Block())
        Sc = Sched(NDS)
        Sc.pool_regs = {"bc_slot": NSLOT + 127, "bc_tok": TOK + 127}
        PR = Sc.pool_reg_handles
        A = Arena(arena_t[:, :], ARENA_BYTES)
        real_add = Sc.add
        cur_add = [Sc.add]

        def add(*a_, **k_):
            return cur_add[0](*a_, **k_)

        def record(fn_):
            lst = []
            prev_ = cur_add[0]
            cur_add[0] = make_recorder(lst)
            fn_()
            cur_add[0] = prev_
            return lst

        def emit_interleaved(lists):
            n_ = max(len(l_) for l_ in lists)
            for i_ in range(n_):
                for l_ in lists:
                    if i_ < len(l_):
                        cur_add[0](*l_[i_])

        def emit_merged(la, lb):
            na, nb_ = len(la), len(lb)
            ia = 0
            for ib in range(nb_):
                cur_add[0](*lb[ib])
                want = ((ib + 1) * na) // max(nb_, 1)
                while ia < want:
                    cur_add[0](*la[ia])
                    ia += 1
            while ia < na:
                cur_add[0](*la[ia])
                ia += 1

        def make_recorder(lst):
            def rec_(eng, fn, reads=(), writes=(), dma=False):
                lst.append((eng, fn, tuple(reads), tuple(writes), dma))
            return rec_

        bank_bufs = [Buf(f"bank{i}") for i in range(8)]
        bank_rr = [0]

        psum_pool = [None]
        pool_rr = {"a": 0, "b": 0}
        pool_banks = {"a": [0, 1, 2, 3], "b": [4, 5, 6, 7]}

        def psum():
            if psum_pool[0] is not None:
                pl = pool_banks[psum_pool[0]]
                i = pl[pool_rr[psum_pool[0]] % len(pl)]
                pool_rr[psum_pool[0]] += 1
                return banks[i], bank_bufs[i]
            i = bank_rr[0]
            bank_rr[0] = (i + 1) % 8
            return banks[i], bank_bufs[i]

        B_COS = [Buf(f"cos{b}") for b in range(NB)]
        B_H = [Buf(f"H{i}") for i in range(NTT)]
        B_HN3 = Buf("HN3")
        B_XN = [Buf(f"XN{k}") for k in range(NBK)]
        B_SLOT = Buf("SLOT")
        B_YS = [Buf(f"YS{e}") for e in range(NEXP)]
        B_OUT = Buf("OUT")

        class Rot:
            def __init__(self, name, free_shape, dtype, n, parts=128):
                self.items = [A.alloc(f"{name}{i}", free_shape, dtype, parts=parts) for i in range(n)]
                self.i = 0

            def next(self):
                it = self.items[self.i]
                self.i = (self.i + 1) % len(self.items)
                return it

        def dma(eng, out_ap, in_ap, reads, writes):
            return add(eng, lambda e: e.dma_start(out=out_ap, in_=in_ap), reads=reads, writes=writes, dma=True)

        ident_b, b_ident_b = A.alloc("ident_b", [128], BF16)
        ident_f, b_ident_f = A.alloc("ident_f", [128], F32)
        ones_b, b_ones = A.alloc("ones_b", [128], BF16)
        mask_b, b_mask = A.alloc("mask_b", [256], BF16)
        mask4, b_mask4 = A.alloc("mask4", [4, 256], BF16)
        ltri_b, b_ltri = A.alloc("ltri_b", [128], BF16)
        invf, b_invf = A.alloc("invf", [1], F32)
        ec_t, b_ec = A.alloc("ec", [32], F32)
        sel_f, b_sel = A.alloc("sel", [64], BF16)
        iota_f, b_iota = A.alloc("iota", [1], F32)
        bgt_t, b_bgt = A.alloc("bgt", [16], F32)
        wr_t, b_wr = A.alloc("wr", [KC, 36], F32)
        br_t, b_br = A.alloc("br", [36], F32)
        base_t, b_base = A.alloc("base", [32], F32)
        dest_all, b_dest = A.alloc("dest_all", [NTT, 2], I32)
        eps_rms, b_eps = A.alloc("eps_rms", [1], F32)
        eps_ln, b_epsln = A.alloc("eps_ln", [1], F32)

        dma("pool", ident_b, c_ident, [], [b_ident_b])
        dma("sp", ident_f, c_ident, [], [b_ident_f])
        dma("pool", mask_b, c_mask, [], [b_mask])
        dma("pool", ltri_b, c_ltri, [], [b_ltri])
        dma("sp", invf, c_invf, [], [b_invf])
        dma("sp", ec_t, c_ec, [], [b_ec])
        dma("pool", sel_f[0:65, :], c_sel, [], [b_sel])
        dma("sp", iota_f, c_iota, [], [b_iota])
        dma("sp", bgt_t, bgt, [], [b_bgt])
        dma("sp", wr_t, w_r.rearrange("(kc p) n -> p kc n", p=128), [], [b_wr])
        dma("sp", br_t, b_r.partition_broadcast(128), [], [b_br])
        add("pool", lambda e: e.memset(ones_b, 1.0), writes=[b_ones])
        for hh_ in range(4):
            add("pool", lambda e, hh_=hh_: e.tensor_copy(out=mask4[:, hh_, :], in_=mask_b), reads=[b_mask], writes=[b_mask4])
        add("pool", lambda e: e.memset(base_t, 0.0), writes=[b_base])
        add("pool", lambda e: e.memset(eps_rms, RMS_EPS), writes=[b_eps])
        add("pool", lambda e: e.memset(eps_ln, LN_EPS), writes=[b_epsln])

        m0 = A.mark()
        zrow, b_zrow = A.alloc("zrow", [D], BF16)
        slot0, b_slot0 = A.alloc("slot0", [NSLOT // 128, 2], I32)
        add("pool", lambda e: e.memset(zrow, 0.0), writes=[b_zrow])
        dma("sp", HN3[TOK:TOK + 128, :], zrow, [b_zrow], [B_HN3])
        zrowf, b_zrowf = A.alloc("zrowf", [D], F32)
        add("pool", lambda e: e.memset(zrowf, 0.0), writes=[b_zrowf])
        dma("sp", YS[NSLOT:NSLOT + 128, :], zrowf, [b_zrowf], [B_YS[0]])
        add("pool", lambda e: e.memset(slot0[:, :, 0:1], TOK), writes=[b_slot0])
        add("pool", lambda e: e.memset(slot0[:, :, 1:2], 0), writes=[b_slot0])
        dma("sp", SLOT[0:NSLOT, :].rearrange("(p a) c -> p a c", p=128), slot0, [b_slot0], [B_SLOT])
        A.release(m0)

        def rms_tile(src_ap, b_src, g_tile, b_g, dst_bf, b_dst, small, junk, dst_f32=None, b_dst32=None):
            (ss, b_ss) = small.next()
            (jk, b_jk) = junk
            add("pool", lambda e: e.memset(ss, 0.0), writes=[b_ss])
            add("act", lambda e: e.activation(out=jk, in_=src_ap, func=AF.Square, accum_out=ss[:, 0:1]),
                reads=[b_src], writes=[b_jk, b_ss])
            add("act", lambda e: e.activation(out=ss[:, 1:2], in_=ss[:, 0:1], func=AF.Ln, bias=eps_rms[:, 0:1],
                                              scale=1.0 / D), reads=[b_ss, b_eps], writes=[b_ss])
            add("act", lambda e: e.activation(out=ss[:, 0:1], in_=ss[:, 1:2], func=AF.Exp, scale=-0.5), reads=[b_ss], writes=[b_ss])
            if dst_f32 is not None:
                add("dve", lambda e: e.scalar_tensor_tensor(out=dst_f32, in0=src_ap, scalar=ss[:, 0:1], in1=g_tile,
                                                            op0=ALU.mult, op1=ALU.mult),
                    reads=[b_src, b_ss, b_g], writes=[b_dst32])
                add("act", lambda e: e.copy(out=dst_bf, in_=dst_f32), reads=[b_dst32], writes=[b_dst])
            else:
                add("dve", lambda e: e.scalar_tensor_tensor(out=dst_bf, in0=src_ap, scalar=ss[:, 0:1], in1=g_tile,
                                                            op0=ALU.mult, op1=ALU.mult),
                    reads=[b_src, b_ss, b_g], writes=[b_dst])

        def transpose_to(src_bf, b_src, dst_ap3, b_dst, eng="act"):
            (bk, b_bk) = psum()
            pv = bk[:, :].bitcast(BF16)
            for c in range(KC):
                add("pe", lambda e, c=c: e.transpose(out=pv[:, c * 128:(c + 1) * 128], in_=src_bf[:, c * 128:(c + 1) * 128],
                                                     identity=ident_b), reads=[b_src, b_ident_b], writes=[b_bk])
            pv3 = pv.rearrange("p (c t) -> p c t", c=KC)
            if eng == "act":
                add("act", lambda e: e.copy(out=dst_ap3, in_=pv3), reads=[b_bk], writes=[b_dst])
            else:
                add("dve", lambda e: e.tensor_copy(out=dst_ap3, in_=pv3), reads=[b_bk], writes=[b_dst])

        def gelu_from_psum(bk, b_bk, dst, b_dst, tmp_rot, width):
            (t1, b_t1) = tmp_rot.next()
            (t2, b_t2) = tmp_rot.next()
            pz = bk[:, 0:width]
            add("act", lambda e: e.activation(out=t1[:, 0:width], in_=pz, func=AF.Square), reads=[b_bk], writes=[b_t1])
            add("dve", lambda e: e.tensor_scalar(out=t1[:, 0:width], in0=t1[:, 0:width], scalar1=0.044715, scalar2=1.0,
                                                 op0=ALU.mult, op1=ALU.add), reads=[b_t1], writes=[b_t1])
            add("dve", lambda e: e.tensor_tensor(out=t2[:, 0:width], in0=pz, in1=t1[:, 0:width], op=ALU.mult),
                reads=[b_bk, b_t1], writes=[b_t2])
            add("act", lambda e: e.activation(out=t2[:, 0:width], in_=t2[:, 0:width], func=AF.Sigmoid,
                                              scale=1.5957691216057308), reads=[b_t2], writes=[b_t2])
            add("dve", lambda e: e.tensor_tensor(out=dst, in0=pz, in1=t2[:, 0:width], op=ALU.mult),
                reads=[b_bk, b_t2], writes=[b_dst])

        wsT, b_wsT = A.alloc("wsT", [4, 128], BF16)
        B2, b_B2 = A.alloc("B2", [4, 128], F32)
        gamrow, b_gamrow = A.alloc("gamrow", [512], F32)
        m0 = A.mark()
        bspb, b_bspb = A.alloc("bspb", [4, 128], F32)
        bet_t, b_bet = A.alloc("bet", [4], F32)
        dma("pool", wsT, ws_t.rearrange("g s t -> s g t"), [], [b_wsT])
        dma("sp", bspb, bsp.partition_broadcast(128).rearrange("p (g t) -> p g t", g=4), [], [b_bspb])
        dma("sp", bet_t, vbet_t, [], [b_bet])
        dma("sp", gamrow, vgam.partition_broadcast(128), [], [b_gamrow])
        add("dve", lambda e: e.tensor_tensor(out=wsT, in0=wsT, in1=mask_b[:, 0:128].unsqueeze(1).to_broadcast([128, 4, 128]),
                                             op=ALU.mult), reads=[b_wsT, b_mask], writes=[b_wsT])
        (bk, b_bk) = psum()
        add("pe", lambda e: e.matmul(bk[:, :], lhsT=ones_b, rhs=wsT.rearrange("p g t -> p (g t)"), start=True, stop=True),
            reads=[b_ones, b_wsT], writes=[b_bk])
        for g in range(4):
            add("dve", lambda e, g=g: e.scalar_tensor_tensor(out=B2[:, g, :], in0=bk[:, g * 128:(g + 1) * 128],
                                                             scalar=bet_t[:, g:g + 1], in1=bspb[:, g, :],
                                                             op0=ALU.mult, op1=ALU.add),
                reads=[b_bk, b_bet, b_bspb], writes=[b_B2])
        A.release(m0)

        persist_mark = A.mark()

        for b in range(NB):
            m_r = A.mark()
            posi, b_posi = A.alloc("posi", [S], I32)
            dma("sp", posi, pos[b].partition_broadcast(128), [], [b_posi])
            rt = Rot("rt", [512], F32, 8)
            ri = Rot("ri", [512], I32, 8)
            ang_rot = Rot("ang", [512], F32, 4)
            tb_rot = Rot("tb", [512], F32, 8)
            def r_block(k, b=b):
                blk = slice(k * 512, (k + 1) * 512)
                (ang, b_ang) = ang_rot.next()
                add("dve", lambda e, ang=ang, blk=blk: e.tensor_copy(out=ang, in_=posi[:, blk]), reads=[b_posi], writes=[b_ang])
                add("dve", lambda e, ang=ang: e.tensor_scalar(out=ang, in0=ang, scalar1=invf[:, 0:1], scalar2=None, op0=ALU.mult),
                    reads=[b_ang, b_invf], writes=[b_ang])
                for (dst, shift) in ((SIN, 0.0), (COS, 0.5 * math.pi)):
                    (u, b_u) = rt.next()
                    (ki, b_ki) = ri.next()
                    (tb, b_tb) = tb_rot.next()
                    add("dve", lambda e, u=u, ang=ang, shift=shift: e.tensor_scalar(
                        out=u, in0=ang, scalar1=shift, scalar2=1.0 / TWO_PI, op0=ALU.add, op1=ALU.mult),
                        reads=[b_ang], writes=[b_u])
                    add("dve", lambda e, u=u, ki=ki: e.tensor_copy(out=ki, in_=u), reads=[b_u], writes=[b_ki])
                    add("dve", lambda e, u=u, ki=ki: e.tensor_copy(out=u, in_=ki), reads=[b_ki], writes=[b_u])
                    add("dve", lambda e, u=u, ang=ang, tb=tb: e.scalar_tensor_tensor(
                        out=tb, in0=u, scalar=-CW1, in1=ang, op0=ALU.mult, op1=ALU.add), reads=[b_u, b_ang], writes=[b_tb])
                    add("dve", lambda e, u=u, tb=tb: e.scalar_tensor_tensor(
                        out=tb, in0=u, scalar=-CW2, in1=tb, op0=ALU.mult, op1=ALU.add), reads=[b_u, b_tb], writes=[b_tb])
                    add("dve", lambda e, tb=tb, shift=shift: e.tensor_scalar(
                        out=tb, in0=tb, scalar1=shift, scalar2=PI_SAFE, op0=ALU.add, op1=ALU.min), reads=[b_tb], writes=[b_tb])
                    add("dve", lambda e, tb=tb: e.tensor_scalar_max(out=tb, in0=tb, scalar1=-PI_SAFE), reads=[b_tb], writes=[b_tb])
                    add("act", lambda e, tb=tb: e.activation(out=tb, in_=tb, func=AF.Sin), reads=[b_tb], writes=[b_tb])
                    dma("sp", dst[b, :, blk], tb, [b_tb], [B_COS[b]])
            for k0 in range(0, NBK, 4):
                emit_interleaved([record(lambda k=k: r_block(k)) for k in range(k0, min(NBK, k0 + 4))])
            A.release(m_r)

        for b in range(NB):
            A.release(persist_mark)

            xnT, b_xnT = A.alloc("xnT", [KC, S], BF16, nbufs=NBK)
            m_a = A.mark()
            xt_rot = Rot("xt", [D], F32, 4)
            xnb_rot = Rot("xnb", [D], BF16, 4)
            small = Rot("ss", [2], F32, 8)
            junk = A.alloc("junk", [D], BF16)
            gmix_t, b_gmix = A.alloc("gmix", [D], F32)
            dma("sp", gmix_t, g_mix.partition_broadcast(128), [], [b_gmix])
            def a_tile(i, b=b):
                (xt, b_xt) = xt_rot.next()
                (xnb, b_xnb) = xnb_rot.next()
                dma("sp", xt, x[b, i * 128:(i + 1) * 128, :], [], [b_xt])
                rms_tile(xt, b_xt, gmix_t, b_gmix, xnb, b_xnb, small, junk)
                transpose_to(xnb, b_xnb, xnT[:, :, i * 128:(i + 1) * 128], b_xnT[i // 4], eng="act" if i % 2 == 0 else "dve")

            for kk in range(NBK):
                emit_interleaved([record(lambda i=i: a_tile(i)) for i in range(kk * 4, kk * 4 + 4)])
                dma("sp", XN[b, :, :, kk * 512:(kk + 1) * 512], xnT[:, :, kk * 512:(kk + 1) * 512], [b_xnT[kk]], [B_XN[kk]])
            A.release(m_a)

            acc, b_acc = A.alloc("acc", [4, S], BF16, parts=128)
            m_b = A.mark()
            qk, b_qk = A.alloc("qk", [4, S], BF16)
            wg_rot = Rot("wg", [KC, 768], BF16, 1)
            cs_rot = Rot("cs", [2, 512], F32, 2)
            ev_rot = Rot("ev", [512], F32, 4)
            rp_rot = Rot("rp", [512], F32, 4)
            pe_rot = Rot("pexp", [4, 256], BF16, 4)
            v_rot = [A.alloc(f"vv{i}", [4, 65], BF16) for i in range(4)]
            for (vv, b_vv) in v_rot:
                add("pool", lambda e, vv=vv: e.memset(vv, 1.0), writes=[b_vv])
            for g in range(3):
                d = DIL[g]
                L = S // d
                nb = L // 128
                (wg, b_wg) = wg_rot.next()
                dma("pool", wg, w_in.rearrange("(kc p) n -> p kc n", p=128)[:, :, g * 768:(g + 1) * 768], [], [b_wg])
                for k in range(NBK):
                    blk = slice(k * 512, (k + 1) * 512)
                    (cs, b_cs) = cs_rot.next()
                    dma("sp", cs[:, 0, :], COS[b, :, blk], [B_COS[b]], [b_cs])
                    dma("sp", cs[:, 1, :], SIN[b, :, blk], [B_COS[b]], [b_cs])
                    for which in range(2):
                        evs = []
                        for part in range(2):
                            col = (which * 2 + part) * 128
                            (bk, b_bk) = psum()
                            for kc in range(KC):
                                add("pe", lambda e, bk=bk, kc=kc, col=col, blk=blk, wg=wg: e.matmul(
                                    bk[:, :], lhsT=wg[:, kc, col:col + 128], rhs=xnT[:, kc, blk],
                                    start=(kc == 0), stop=(kc == KC - 1)), reads=[b_wg, b_xnT[k]], writes=[b_bk])
                            (ev, b_ev) = ev_rot.next()
                            add("act", lambda e, ev=ev, bk=bk: e.copy(out=ev, in_=bk[:, :]), reads=[b_bk], writes=[b_ev])
                            evs.append((ev, b_ev))
                        (eA, b_eA), (eB, b_eB) = evs
                        (t1, b_t1) = rp_rot.next()
                        (t2, b_t2) = rp_rot.next()
                        oA = qk[:, which * 2, blk]
                        oB = qk[:, which * 2 + 1, blk]
                        add("dve", lambda e, t1=t1, eA=eA, cs=cs: e.tensor_tensor(out=t1, in0=eA, in1=cs[:, 0, :], op=ALU.mult),
                            reads=[b_eA, b_cs], writes=[b_t1])
                        add("dve", lambda e, t2=t2, eB=eB, cs=cs: e.tensor_tensor(out=t2, in0=eB, in1=cs[:, 1, :], op=ALU.mult),
                            reads=[b_eB, b_cs], writes=[b_t2])
                        add("dve", lambda e, t1=t1, t2=t2, oA=oA: e.tensor_tensor(out=oA, in0=t1, in1=t2, op=ALU.subtract),
                            reads=[b_t1, b_t2], writes=[b_qk])
                        add("dve", lambda e, t1=t1, eB=eB, cs=cs: e.tensor_tensor(out=t1, in0=eB, in1=cs[:, 0, :], op=ALU.mult),
                            reads=[b_eB, b_cs], writes=[b_t1])
                        add("dve", lambda e, t2=t2, eA=eA, cs=cs: e.tensor_tensor(out=t2, in0=eA, in1=cs[:, 1, :], op=ALU.mult),
                            reads=[b_eA, b_cs], writes=[b_t2])
                        add("dve", lambda e, t1=t1, t2=t2, oB=oB: e.tensor_tensor(out=oB, in0=t1, in1=t2, op=ALU.add),
                            reads=[b_t1, b_t2], writes=[b_qk])
                vcount = [0]
                for r in range(d):
                    def tsl(jj, n=1, r=r):
                        st = r + d * 128 * jj
                        return slice(st, st + d * (128 * n - 1) + 1, d) if d > 1 else slice(st, st + 128 * n)
                    items = []

                    def emit_pv(j, items=items, tsl=tsl):
                        (vv, b_vv, px, b_px) = items[j]
                        (bk3, b_bk3) = psum()
                        for hh in range(4):
                            o_ap = bk3[0:65, hh * 128:(hh + 1) * 128]
                            if j > 0:
                                (pvv, b_pvv, ppx, b_ppx) = items[j - 1]
                                add("pe", lambda e, o_ap=o_ap, pvv=pvv, ppx=ppx, hh=hh: e.matmul(
                                    o_ap, lhsT=pvv[:, hh, :], rhs=ppx[:, hh, 128:256], start=True, stop=False),
                                    reads=[b_pvv, b_ppx], writes=[b_bk3])
                            add("pe", lambda e, o_ap=o_ap, vv=vv, px=px, hh=hh, first=(j == 0): e.matmul(
                                o_ap, lhsT=vv[:, hh, :], rhs=px[:, hh, 0:128], start=first, stop=True),
                                reads=[b_vv, b_px], writes=[b_bk3])
                        a_ap = acc[0:65, :, tsl(j)]
                        p_ap = bk3[0:65, :].rearrange("p (h q) -> p h q", h=4)
                        if g == 0:
                            add("dve", lambda e, a_ap=a_ap, p_ap=p_ap: e.tensor_copy(out=a_ap, in_=p_ap), reads=[b_bk3], writes=[b_acc])
                        else:
                            add("dve", lambda e, a_ap=a_ap, p_ap=p_ap: e.tensor_tensor(out=a_ap, in0=p_ap, in1=a_ap, op=ALU.add),
                                reads=[b_bk3, b_acc], writes=[b_acc])

                    for j in range(nb):
                        (vv, b_vv) = v_rot[vcount[0] % 4]
                        vcount[0] += 1
                        (bk, b_bk) = psum()
                        for kc in range(KC):
                            add("pe", lambda e, bk=bk, kc=kc, wg=wg, ts=tsl(j): e.matmul(
                                bk[:, 0:256], lhsT=xnT[:, kc, ts], rhs=wg[:, kc, 512:768],
                                start=(kc == 0), stop=(kc == KC - 1)), reads=[b_wg] + b_xnT, writes=[b_bk])
                        add("act", lambda e, vv=vv, bk=bk: e.copy(out=vv[:, :, 0:64], in_=bk[:, 0:256].rearrange("p (h d) -> p h d", h=4)),
                            reads=[b_bk], writes=[b_vv])
                        nq = 2 if j < nb - 1 else 1
                        (px, b_px) = pe_rot.next()
                        for hh in range(4):
                            (bk2, b_bk2) = psum()
                            for part in range(2):
                                add("pe", lambda e, bk2=bk2, hh=hh, part=part, ks=tsl(j), qs=tsl(j, nq), nq=nq: e.matmul(
                                    bk2[:, 0:128 * nq], lhsT=qk[32 * hh:32 * hh + 32, 2 + part, ks],
                                    rhs=qk[32 * hh:32 * hh + 32, part, qs], start=(part == 0), stop=(part == 1),
                                    tile_position=(32 * hh, 0)), reads=[b_qk], writes=[b_bk2])
                            add("act", lambda e, px=px, bk2=bk2, hh=hh, nq=nq: e.activation(
                                out=px[:, hh, 0:128 * nq], in_=bk2[:, 0:128 * nq], func=AF.Exp, scale=0.125),
                                reads=[b_bk2], writes=[b_px])
                        add("dve", lambda e, px=px, nq=nq: e.tensor_tensor(
                            out=px[:, :, 0:128 * nq], in0=px[:, :, 0:128 * nq],
                            in1=mask4[:, :, 0:128 * nq], op=ALU.mult),
                            reads=[b_px, b_mask4], writes=[b_px])
                        items.append((vv, b_vv, px, b_px))
                        if j >= 2:
                            emit_pv(j - 2)
                    if nb >= 2:
                        emit_pv(nb - 2)
                    emit_pv(nb - 1)
            A.release(m_b)
            yaT, b_yaT = A.alloc("yaT", [4, S], BF16, top=True)
            m_n = A.mark()
            rd_rot = Rot("rden", [512], F32, 2)
            for k in range(NBK):
                blk = slice(k * 512, (k + 1) * 512)
                for hh in range(4):
                    (bk, b_bk) = psum()
                    add("pe", lambda e, bk=bk, hh=hh, blk=blk: e.matmul(bk[0:64, :], lhsT=sel_f[0:65, :], rhs=acc[0:65, hh, blk],
                                                                     start=True, stop=True), reads=[b_sel, b_acc], writes=[b_bk])
                    (rd, b_rd) = rd_rot.next()
                    add("dve", lambda e, rd=rd, bk=bk: e.reciprocal(out=rd[0:64, :], in_=bk[0:64, :]), reads=[b_bk], writes=[b_rd])
                    add("dve", lambda e, rd=rd, hh=hh, blk=blk: e.tensor_tensor(out=yaT[0:64, hh, blk], in0=acc[0:64, hh, blk],
                                                                             in1=rd[0:64, :], op=ALU.mult),
                        reads=[b_acc, b_rd], writes=[b_yaT])
            if dev:
                (dd, b_dd) = A.alloc("dbgya", [4, S], F32)
                add("dve", lambda e, dd=dd: e.tensor_copy(out=dd[0:64], in_=yaT[0:64]), reads=[b_yaT], writes=[b_dd])
                dma("sp", dbg["ya"][b], dd[0:64], [b_dd], [B_OUT])
            A.release(m_n)
            A.release(persist_mark)

            ybT, b_ybT = A.alloc("ybT", [4, S], BF16, top=True)
            m_c = A.mark()
            xs_rot = Rot("xs", [KC, 512], BF16, 2)
            wuv, b_wuv = A.alloc("wuv", [KC, 1024], BF16)
            dma("pool", wuv, w_in.rearrange("(kc p) n -> p kc n", p=128)[:, :, 2304:3328], [], [b_wuv])
            uT_rot = Rot("uT", [4, 512], BF16, 2)
            gtmp = Rot("gtmp", [512], F32, 8)
            vg_rot = Rot("vg", [512], F32, 4)
            vh_rot = Rot("vh", [512], BF16, 4)
            st_rot = Rot("lnst", [4], F32, 8)
            ljunk = A.alloc("ljunk", [512], BF16)
            yt_rot = Rot("ytmp", [512], F32, 4)
            for k in range(NBK):
                blk = slice(k * 512, (k + 1) * 512)
                (uT, b_uT) = uT_rot.next()
                (xs, b_xs) = xs_rot.next()
                dma("sp", xs, XN[b, :, :, blk], [B_XN[k]], [b_xs])
                def c_uchunk(c, xs=xs, b_xs=b_xs, uT=uT, b_uT=b_uT):
                    (bk, b_bk) = psum()
                    for kc in range(KC):
                        add("pe", lambda e, bk=bk, kc=kc, c=c, xs=xs: e.matmul(
                            bk[:, :], lhsT=wuv[:, kc, c * 128:(c + 1) * 128], rhs=xs[:, kc, :],
                            start=(kc == 0), stop=(kc == KC - 1)), reads=[b_wuv, b_xs], writes=[b_bk])
                    gelu_from_psum(bk, b_bk, uT[:, c, :], b_uT, gtmp, 512)

                def c_tile(t, k=k, xs=xs, b_xs=b_xs, uT=uT, b_uT=b_uT):
                    tok = slice(k * 512 + t * 128, k * 512 + (t + 1) * 128)
                    (bk, b_bk) = psum()
                    for kc in range(KC):
                        add("pe", lambda e, bk=bk, kc=kc, t=t, xs=xs: e.matmul(
                            bk[:, :], lhsT=xs[:, kc, t * 128:(t + 1) * 128], rhs=wuv[:, kc, 512:1024],
                            start=(kc == 0), stop=(kc == KC - 1)), reads=[b_wuv, b_xs], writes=[b_bk])
                    (vg, b_vg) = vg_rot.next()
                    gelu_from_psum(bk, b_bk, vg, b_vg, gtmp, 512)
                    (st, b_st) = st_rot.next()
                    (lj, b_lj) = ljunk
                    add("pool", lambda e, st=st: e.memset(st, 0.0), writes=[b_st])
                    add("act", lambda e, st=st, vg=vg, lj=lj: e.activation(out=lj, in_=vg, func=AF.Copy, accum_out=st[:, 0:1]),
                        reads=[b_vg], writes=[b_lj, b_st])
                    add("act", lambda e, st=st, vg=vg, lj=lj: e.activation(out=lj, in_=vg, func=AF.Square, accum_out=st[:, 1:2]),
                        reads=[b_vg], writes=[b_lj, b_st])
                    add("dve", lambda e, st=st: e.tensor_scalar(out=st[:, 0:2], in0=st[:, 0:2], scalar1=1.0 / 512, scalar2=None, op0=ALU.mult),
                        reads=[b_st], writes=[b_st])
                    add("dve", lambda e, st=st: e.tensor_tensor(out=st[:, 2:3], in0=st[:, 0:1], in1=st[:, 0:1], op=ALU.mult),
                        reads=[b_st], writes=[b_st])
                    add("dve", lambda e, st=st: e.tensor_tensor(out=st[:, 1:2], in0=st[:, 1:2], in1=st[:, 2:3], op=ALU.subtract),
                        reads=[b_st], writes=[b_st])
                    add("act", lambda e, st=st: e.activation(out=st[:, 1:2], in_=st[:, 1:2], func=AF.Sqrt, bias=eps_ln[:, 0:1], scale=1.0),
                        reads=[b_st, b_epsln], writes=[b_st])
                    add("dve", lambda e, st=st: e.reciprocal(out=st[:, 1:2], in_=st[:, 1:2]), reads=[b_st], writes=[b_st])
                    add("dve", lambda e, st=st, vg=vg: e.tensor_scalar(out=vg, in0=vg, scalar1=st[:, 0:1], scalar2=st[:, 1:2],
                                                                     op0=ALU.subtract, op1=ALU.mult), reads=[b_vg, b_st], writes=[b_vg])
                    (vh, b_vh) = vh_rot.next()
                    add("dve", lambda e, vh=vh, vg=vg: e.tensor_tensor(out=vh, in0=vg, in1=gamrow, op=ALU.mult),
                        reads=[b_vg, b_gamrow], writes=[b_vh])
                    (bk2, b_bk2) = psum()
                    for g in range(4):
                        add("pe", lambda e, bk2=bk2, g=g, vh=vh: e.matmul(
                            bk2[:, g * 128:(g + 1) * 128], lhsT=vh[:, g * 128:(g + 1) * 128], rhs=wsT[:, g, :],
                            start=True, stop=True), reads=[b_vh, b_wsT], writes=[b_bk2])
                    (yt, b_yt) = yt_rot.next()
                    add("dve", lambda e, yt=yt, bk2=bk2: e.tensor_tensor(out=yt, in0=bk2[:, :], in1=B2.rearrange("p g t -> p (g t)"), op=ALU.add),
                        reads=[b_bk2, b_B2], writes=[b_yt])
                    add("dve", lambda e, yt=yt, uT=uT, t=t, tok=tok: e.tensor_tensor(
                        out=ybT[:, :, tok], in0=yt.rearrange("p (g t) -> p g t", g=4), in1=uT[:, :, t * 128:(t + 1) * 128], op=ALU.mult),
                        reads=[b_yt, b_uT], writes=[b_ybT])

                emit_interleaved([record(lambda c=c: c_uchunk(c)) for c in range(4)])
                emit_interleaved([record(lambda t=t: c_tile(t)) for t in (0, 1)])
                emit_interleaved([record(lambda t=t: c_tile(t)) for t in (2, 3)])
            if dev:
                (dd, b_dd) = A.alloc("dbgyb", [4, S], F32)
                add("dve", lambda e, dd=dd: e.tensor_copy(out=dd, in_=ybT), reads=[b_ybT], writes=[b_dd])
                dma("sp", dbg["yb"][b], dd, [b_dd], [B_OUT])
            A.release(m_c)

            m_d = A.mark()
            wgt, b_wgt = A.alloc("wgt", [KC, 2048], BF16)
            woa, b_woa = A.alloc("woa", [4, D], BF16)
            wob, b_wob = A.alloc("wob", [4, D], BF16)
            wo, b_wo = A.alloc("wo", [KC, D], BF16)
            dma("pool", wgt, w_in.rearrange("(kc p) n -> p kc n", p=128)[:, :, 3328:5376], [], [b_wgt])
            dma("pool", woa[0:64], w_oa.rearrange("(h p) n -> p h n", p=64), [], [b_woa])
            dma("pool", wob, w_ob.rearrange("(g p) n -> p g n", p=128), [], [b_wob])
            dma("pool", wo, w_o.rearrange("(kc p) n -> p kc n", p=128), [], [b_wo])
            mg_rot = Rot("merged", [KC, 512], BF16, 2)
            sg_rot = Rot("sg", [512], F32, 4)
            mm_rot = Rot("mm", [512], F32, 4)
            xr_rot = Rot("xr", [D], F32, 2)
            xs_rot = Rot("xsd", [KC, 512], BF16, 2)
            for k in range(NBK):
                blk = slice(k * 512, (k + 1) * 512)
                (mg, b_mg) = mg_rot.next()
                (xs, b_xs) = xs_rot.next()
                dma("sp", xs, XN[b, :, :, blk], [B_XN[k]], [b_xs])
                for c in range(KC):
                    (bka, b_bka) = psum()
                    (bkb, b_bkb) = psum()
                    (bkA, b_bkA) = psum()
                    (bkB, b_bkB) = psum()
                    for kc in range(KC):
                        add("pe", lambda e, bka=bka, kc=kc, c=c, xs=xs: e.matmul(
                            bka[:, :], lhsT=wgt[:, kc, c * 128:(c + 1) * 128], rhs=xs[:, kc, :],
                            start=(kc == 0), stop=(kc == KC - 1)), reads=[b_wgt, b_xs], writes=[b_bka])
                    for kc in range(KC):
                        add("pe", lambda e, bkb=bkb, kc=kc, c=c, xs=xs: e.matmul(
                            bkb[:, :], lhsT=wgt[:, kc, 1024 + c * 128:1024 + (c + 1) * 128], rhs=xs[:, kc, :],
                            start=(kc == 0), stop=(kc == KC - 1)), reads=[b_wgt, b_xs], writes=[b_bkb])
                    for hh in range(4):
                        add("pe", lambda e, bkA=bkA, hh=hh, c=c, blk=blk: e.matmul(
                            bkA[:, :], lhsT=woa[0:64, hh, c * 128:(c + 1) * 128], rhs=yaT[0:64, hh, blk],
                            start=(hh == 0), stop=(hh == 3)), reads=[b_woa, b_yaT], writes=[b_bkA])
                    for g in range(4):
                        add("pe", lambda e, bkB=bkB, g=g, c=c, blk=blk: e.matmul(
                            bkB[:, :], lhsT=wob[:, g, c * 128:(c + 1) * 128], rhs=ybT[:, g, blk],
                            start=(g == 0), stop=(g == 3)), reads=[b_wob, b_ybT], writes=[b_bkB])
                    (sa, b_sa) = sg_rot.next()
                    (sb_, b_sb) = sg_rot.next()
                    add("act", lambda e, sa=sa, bka=bka, c=c: e.activation(out=sa, in_=bka[:, :], func=AF.Sigmoid, bias=bgt_t[:, c:c + 1], scale=1.0),
                        reads=[b_bka, b_bgt], writes=[b_sa])
                    add("act", lambda e, sb_=sb_, bkb=bkb, c=c: e.activation(out=sb_, in_=bkb[:, :], func=AF.Sigmoid, bias=bgt_t[:, 8 + c:9 + c], scale=1.0),
                        reads=[b_bkb, b_bgt], writes=[b_sb])
                    (m1, b_m1) = mm_rot.next()
                    (m2, b_m2) = mm_rot.next()
                    add("dve", lambda e, m1=m1, sa=sa, bkA=bkA: e.tensor_tensor(out=m1, in0=bkA[:, :], in1=sa, op=ALU.mult),
                        reads=[b_bkA, b_sa], writes=[b_m1])
                    add("dve", lambda e, m2=m2, sb_=sb_, bkB=bkB: e.tensor_tensor(out=m2, in0=bkB[:, :], in1=sb_, op=ALU.mult),
                        reads=[b_bkB, b_sb], writes=[b_m2])
                    add("dve", lambda e, m1=m1, m2=m2, mg=mg, c=c: e.tensor_tensor(out=mg[:, c, :], in0=m1, in1=m2, op=ALU.add),
                        reads=[b_m1, b_m2], writes=[b_mg])
                for t in range(4):
                    ti = k * 4 + t
                    gi = b * NT + ti
                    (xr, b_xr) = xr_rot.next()
                    (h1t, b_h1t) = (xr, b_xr)
                    dma("sp", xr, x[b, ti * 128:(ti + 1) * 128, :], [], [b_xr])
                    for n in range(2):
                        (bk, b_bk) = psum()
                        for c in range(KC):
                            add("pe", lambda e, bk=bk, c=c, t=t, n=n, mg=mg: e.matmul(
                                bk[:, :], lhsT=mg[:, c, t * 128:(t + 1) * 128], rhs=wo[:, c, n * 512:(n + 1) * 512],
                                start=(c == 0), stop=(c == KC - 1)), reads=[b_mg, b_wo], writes=[b_bk])
                        add("dve", lambda e, bk=bk, xr=xr, h1t=h1t, n=n: e.tensor_tensor(
                            out=h1t[:, n * 512:(n + 1) * 512], in0=bk[:, :], in1=xr[:, n * 512:(n + 1) * 512], op=ALU.add),
                            reads=[b_bk, b_xr], writes=[b_h1t])
                    dma("sp", H[gi * 128:(gi + 1) * 128, :], h1t, [b_h1t], [B_H[gi]])
                    if dev:
                        dma("sp", dbg["h1"][gi * 128:(gi + 1) * 128, :], h1t, [b_h1t], [B_OUT])
            A.release(m_d)
            A.release(persist_mark)
            A.release_top()

            m_e = A.mark()
            kT, b_kT = A.alloc("kT", [KC, NMEM], BF16)
            vm, b_vm = A.alloc("vm", [2, D], BF16)
            wq, b_wq = A.alloc("wq", [KC, D], BF16)
            wox, b_wox = A.alloc("wox", [KC, D], BF16)
            dma("pool", wq, w_qx.rearrange("(kc p) n -> p kc n", p=128), [], [b_wq])
            dma("pool", wox, w_ox.rearrange("(kc p) n -> p kc n", p=128), [], [b_wox])
            m_e0 = A.mark()
            wkv, b_wkv = A.alloc("wkv", [KC, 2 * D], BF16)
            dma("pool", wkv, w_kvx.rearrange("(kc p) n -> p kc n", p=128), [], [b_wkv])
            gmem_t, b_gmem = A.alloc("gmem", [D], F32)
            dma("sp", gmem_t, g_mem.partition_broadcast(128), [], [b_gmem])
            mnT, b_mnT = A.alloc("mnT", [KC, NMEM], BF16)
            mt_rot = Rot("mt", [D], F32, 2)
            mb_rot = Rot("mb", [D], BF16, 2)
            small = Rot("ssm", [2], F32, 4)
            junk = A.alloc("junkm", [D], BF16)
            for i in range(2):
                (mt, b_mt) = mt_rot.next()
                (mb, b_mb) = mb_rot.next()
                dma("sp", mt, mem[b, i * 128:(i + 1) * 128, :], [], [b_mt])
                rms_tile(mt, b_mt, gmem_t, b_gmem, mb, b_mb, small, junk)
                transpose_to(mb, b_mb, mnT[:, :, i * 128:(i + 1) * 128], b_mnT)
            for oc in range(KC):
                (bk, b_bk) = psum()
                for kc in range(KC):
                    add("pe", lambda e, bk=bk, kc=kc, oc=oc: e.matmul(
                        bk[:, 0:NMEM], lhsT=wkv[:, kc, oc * 128:(oc + 1) * 128], rhs=mnT[:, kc, :],
                        start=(kc == 0), stop=(kc == KC - 1)), reads=[b_wkv, b_mnT], writes=[b_bk])
                add("act", lambda e, bk=bk, oc=oc: e.copy(out=kT[:, oc, :], in_=bk[:, 0:NMEM]), reads=[b_bk], writes=[b_kT])
            for mchunk in range(2):
                for n in range(2):
                    (bk, b_bk) = psum()
                    for kc in range(KC):
                        add("pe", lambda e, bk=bk, kc=kc, mchunk=mchunk, n=n: e.matmul(
                            bk[:, :], lhsT=mnT[:, kc, mchunk * 128:(mchunk + 1) * 128], rhs=wkv[:, kc, D + n * 512:D + (n + 1) * 512],
                            start=(kc == 0), stop=(kc == KC - 1)), reads=[b_wkv, b_mnT], writes=[b_bk])
                    add("act", lambda e, bk=bk, mchunk=mchunk, n=n: e.copy(out=vm[:, mchunk, n * 512:(n + 1) * 512], in_=bk[:, :]),
                        reads=[b_bk], writes=[b_vm])
            A.release(m_e0)

            gx_t, b_gx = A.alloc("gx", [D], F32)
            gmoe_t, b_gmoe = A.alloc("gmoe", [D], F32)
            dma("sp", gx_t, g_x.partition_broadcast(128), [], [b_gx])
            dma("sp", gmoe_t, g_moe.partition_broadcast(128), [], [b_gmoe])
            h_rot = Rot("ht", [D], F32, 8)
            hb_rot = Rot("hb", [D], BF16, 4)
            small = Rot("sse", [2], F32, 8)
            junk = A.alloc("junke", [D], BF16)
            hnT_rot = Rot("hnT", [KC, 512], BF16, 2)
            qT_rot = Rot("qT", [KC, 512], BF16, 2)
            oT_rot = Rot("oT", [KC, 512], BF16, 2)
            px_rot = Rot("pxx", [2, 512], BF16, 3)
            rd_rot = Rot("rdx", [512], F32, 2)
            h2_rot = Rot("h2t", [D], F32, 2)
            hn3f_rot = Rot("hn3f", [D], F32, 2)
            hn3b_rot = Rot("hn3b", [D], BF16, 2)
            hn3T_rot = Rot("hn3T", [KC, 128], F32, 2)
            rs_rot = Rot("rsm", [16], F32, 4)
            ls_rot = Rot("ls", [36], F32, 2)
            el_rot = Rot("el", [32], F32, 2)
            mk_rot = Rot("mk", [3, 32], F32, 2)
            mkb_rot = Rot("mkb", [32], BF16, 2)
            t8_rot = Rot("t8", [8], F32, 2)
            rec_rot = Rot("rec", [2, 2], I32, 2)
            dsc_rot = Rot("dsc", [2], I32, 2)

            def e_stage1(k, b=b):
                (hnT, b_hnT) = hnT_rot.next()
                hts = []

                def e_tile(t):
                    gi = b * NT + k * 4 + t
                    (ht, b_ht) = h_rot.next()
                    (hb, b_hb) = hb_rot.next()
                    dma("sp", ht, H[gi * 128:(gi + 1) * 128, :], [B_H[gi]], [b_ht])
                    rms_tile(ht, b_ht, gx_t, b_gx, hb, b_hb, small, junk)
                    transpose_to(hb, b_hb, hnT[:, :, t * 128:(t + 1) * 128], b_hnT, eng="act" if t % 2 == 0 else "dve")
                    hts.append((ht, b_ht, gi))

                emit_interleaved([record(lambda t=t: e_tile(t)) for t in range(4)])
                (qT, b_qT) = qT_rot.next()
                for oc in range(KC):
                    (bk, b_bk) = psum()
                    for kc in range(KC):
                        add("pe", lambda e, bk=bk, kc=kc, oc=oc, hnT=hnT: e.matmul(
                            bk[:, :], lhsT=wq[:, kc, oc * 128:(oc + 1) * 128], rhs=hnT[:, kc, :],
                            start=(kc == 0), stop=(kc == KC - 1)), reads=[b_wq, b_hnT], writes=[b_bk])
                    if oc % 2 == 0:
                        add("act", lambda e, bk=bk, oc=oc, qT=qT: e.copy(out=qT[:, oc, :], in_=bk[:, :]), reads=[b_bk], writes=[b_qT])
                    else:
                        add("dve", lambda e, bk=bk, oc=oc, qT=qT: e.tensor_copy(out=qT[:, oc, :], in_=bk[:, :]), reads=[b_bk], writes=[b_qT])
                return (hts, qT, b_qT)

            def e_stage2(k, st1, b=b):
                (hts, qT, b_qT) = st1
                pending = []
                outer_add = cur_add[0]

                def flush_router():
                    n1 = max(len(p[0]) for p in pending)
                    for i_ in range(n1):
                        for p in pending:
                            if i_ < len(p[0]):
                                outer_add(*p[0][i_])
                    for p in pending:
                        for it in p[1]:
                            outer_add(*it)
                    pending.clear()
                (oT, b_oT) = oT_rot.next()
                pxs = {}

                def e_scores(h):
                    (px, b_px) = px_rot.next()
                    pxs[h] = (px, b_px)
                    for mchunk in range(2):
                        (bk, b_bk) = psum()
                        for cc in range(2):
                            add("pe", lambda e, bk=bk, cc=cc, h=h, mchunk=mchunk, qT=qT: e.matmul(
                                bk[:, :], lhsT=kT[:, 2 * h + cc, mchunk * 128:(mchunk + 1) * 128], rhs=qT[:, 2 * h + cc, :],
                                start=(cc == 0), stop=(cc == 1)), reads=[b_kT, b_qT], writes=[b_bk])
                        add("act", lambda e, bk=bk, px=px, mchunk=mchunk: e.activation(out=px[:, mchunk, :], in_=bk[:, :], func=AF.Exp, scale=1.0 / 16.0),
                            reads=[b_bk], writes=[b_px])

                def e_pv(h):
                    (px, b_px) = pxs[h]
                    (bkd, b_bkd) = psum()
                    for mchunk in range(2):
                        add("pe", lambda e, bkd=bkd, px=px, mchunk=mchunk: e.matmul(
                            bkd[:, :], lhsT=ones_b, rhs=px[:, mchunk, :], start=(mchunk == 0), stop=(mchunk == 1)),
                            reads=[b_ones, b_px], writes=[b_bkd])
                    (rd, b_rd) = rd_rot.next()
                    add("dve", lambda e, rd=rd, bkd=bkd: e.reciprocal(out=rd, in_=bkd[:, :]), reads=[b_bkd], writes=[b_rd])
                    for cc in range(2):
                        (bk, b_bk) = psum()
                        for mchunk in range(2):
                            add("pe", lambda e, bk=bk, px=px, mchunk=mchunk, h=h, cc=cc: e.matmul(
                                bk[:, :], lhsT=vm[:, mchunk, (2 * h + cc) * 128:(2 * h + cc + 1) * 128], rhs=px[:, mchunk, :],
                                start=(mchunk == 0), stop=(mchunk == 1)), reads=[b_vm, b_px], writes=[b_bk])
                        add("dve", lambda e, bk=bk, rd=rd, oT=oT, h=h, cc=cc: e.tensor_tensor(
                            out=oT[:, 2 * h + cc, :], in0=bk[:, :], in1=rd, op=ALU.mult), reads=[b_bk, b_rd], writes=[b_oT])

                e_scores(0)
                for h in range(4):
                    if h + 1 < 4:
                        e_scores(h + 1)
                    e_pv(h)
                for t in range(4):
                    (ht, b_ht, gi) = hts[t]
                    (h2t, b_h2t) = h2_rot.next()
                    for n in range(2):
                        (bk, b_bk) = psum()
                        for c in range(KC):
                            add("pe", lambda e, bk=bk, c=c, t=t, n=n, oT=oT: e.matmul(
                                bk[:, :], lhsT=oT[:, c, t * 128:(t + 1) * 128], rhs=wox[:, c, n * 512:(n + 1) * 512],
                                start=(c == 0), stop=(c == KC - 1)), reads=[b_oT, b_wox], writes=[b_bk])
                        add("dve", lambda e, bk=bk, ht=ht, h2t=h2t, n=n: e.tensor_tensor(
                            out=h2t[:, n * 512:(n + 1) * 512], in0=bk[:, :], in1=ht[:, n * 512:(n + 1) * 512], op=ALU.add),
                            reads=[b_bk, b_ht], writes=[b_h2t])
                    dma("sp", H[gi * 128:(gi + 1) * 128, :], h2t, [b_h2t], [B_H[gi]])
                    if dev:
                        dma("sp", dbg["h2"][gi * 128:(gi + 1) * 128, :], h2t, [b_h2t], [B_OUT])
                    lst1, lst2 = [], []
                    cur_add[0] = make_recorder(lst1)
                    (hn3f, b_hn3f) = hn3f_rot.next()
                    (hn3b, b_hn3b) = hn3b_rot.next()
                    rms_tile(h2t, b_h2t, gmoe_t, b_gmoe, hn3b, b_hn3b, small, junk, dst_f32=hn3f, b_dst32=b_hn3f)
                    dma("sp", HN3[gi * 128:(gi + 1) * 128, :], hn3b, [b_hn3b], [B_HN3])
                    (hn3T, b_hn3T) = hn3T_rot.next()
                    (bk, b_bk) = psum()
                    for half in range(2):
                        for c4 in range(4):
                            c = half * 4 + c4
                            add("pe", lambda e, bk=bk, c=c, c4=c4, hn3f=hn3f: e.transpose(
                                out=bk[:, c4 * 128:(c4 + 1) * 128], in_=hn3f[:, c * 128:(c + 1) * 128], identity=ident_f),
                                reads=[b_hn3f, b_ident_f], writes=[b_bk])
                        add("act", lambda e, bk=bk, half=half, hn3T=hn3T: e.copy(
                            out=hn3T[:, half * 4:(half + 1) * 4, :], in_=bk[:, :].rearrange("p (c t) -> p c t", c=4)),
                            reads=[b_bk], writes=[b_hn3T])
                    (bk, b_bk) = psum()
                    (bkL, b_bkL) = (bk, b_bk)
                    for c in range(KC):
                        add("pe", lambda e, bk=bk, c=c, hn3T=hn3T: e.matmul(
                            bk[:, 0:36], lhsT=hn3T[:, c, :], rhs=wr_t[:, c, :], start=(c == 0), stop=(c == KC - 1)),
                            reads=[b_hn3T, b_wr], writes=[b_bk])
                    (ls, b_ls) = ls_rot.next()
                    (rs, b_rs) = rs_rot.next()
                    (el, b_el) = el_rot.next()
                    (mk, b_mk) = mk_rot.next()
                    (mkb, b_mkb) = mkb_rot.next()
                    (t8, b_t8) = t8_rot.next()
                    (rec, b_rec) = rec_rot.next()
                    add("dve", lambda e, ls=ls, bk=bk: e.tensor_tensor(out=ls, in0=bk[:, 0:36], in1=br_t, op=ALU.add),
                        reads=[b_bk, b_br], writes=[b_ls])
                    add("dve", lambda e, ls=ls, rs=rs: e.reduce_max(out=rs[:, 0:1], in_=ls[:, 0:4], axis=AX.X), reads=[b_ls], writes=[b_rs])
                    add("dve", lambda e, rs=rs: e.tensor_scalar(out=rs[:, 1:2], in0=rs[:, 0:1], scalar1=-1.0, scalar2=None, op0=ALU.mult),
                        reads=[b_rs], writes=[b_rs])
                    add("pool", lambda e, rs=rs: e.memset(rs[:, 2:3], 0.0), writes=[b_rs])
                    add("act", lambda e, ls=ls, rs=rs, mk=mk: e.activation(out=mk[:, 2, 0:4], in_=ls[:, 0:4], func=AF.Exp, bias=rs[:, 1:2], scale=1.0,
                                                                          accum_out=rs[:, 2:3]), reads=[b_ls, b_rs], writes=[b_mk, b_rs])
                    add("dve", lambda e, rs=rs: e.reciprocal(out=rs[:, 3:4], in_=rs[:, 2:3]), reads=[b_rs], writes=[b_rs])
                    add("dve", lambda e, ls=ls, rs=rs, mk=mk: e.tensor_scalar(out=mk[:, 2, 8:12], in0=ls[:, 0:4], scalar1=rs[:, 0:1], scalar2=None, op0=ALU.is_ge),
                        reads=[b_ls, b_rs], writes=[b_mk])
                    add("dve", lambda e, mk=mk: e.tensor_scalar(out=mk[:, 2, 8:12], in0=mk[:, 2, 8:12], scalar1=1e30, scalar2=-1e30, op0=ALU.mult, op1=ALU.add),
                        reads=[b_mk], writes=[b_mk])
                    add("dve", lambda e, ls=ls, el=el, mk=mk: e.tensor_tensor(
                        out=el.rearrange("p (g x) -> p g x", g=4), in0=ls[:, 4:36].rearrange("p (g x) -> p g x", g=4),
                        in1=mk[:, 2, 8:12].unsqueeze(2).to_broadcast([128, 4, 8]), op=ALU.add), reads=[b_ls, b_mk], writes=[b_el])
                    add("dve", lambda e, el=el, t8=t8: e.max(out=t8, in_=el), reads=[b_el], writes=[b_t8])
                    add("dve", lambda e, el=el, t8=t8, mk=mk: e.tensor_scalar(out=mk[:, 0, :], in0=el, scalar1=t8[:, 0:1], scalar2=None, op0=ALU.is_ge),
                        reads=[b_el, b_t8], writes=[b_mk])
                    add("dve", lambda e, el=el, t8=t8, mk=mk: e.tensor_scalar(out=mk[:, 1, :], in0=el, scalar1=t8[:, 1:2], scalar2=None, op0=ALU.is_ge),
                        reads=[b_el, b_t8], writes=[b_mk])
                    add("dve", lambda e, mk=mk, mkb=mkb: e.tensor_copy(out=mkb, in_=mk[:, 1, :]), reads=[b_mk], writes=[b_mkb])
                    add("dve", lambda e, mk=mk: e.tensor_tensor(out=mk[:, 1, :], in0=mk[:, 1, :], in1=mk[:, 0, :], op=ALU.subtract),
                        reads=[b_mk], writes=[b_mk])
                    add("dve", lambda e, t8=t8, rs=rs: e.tensor_tensor(out=rs[:, 4:5], in0=t8[:, 0:1], in1=t8[:, 1:2], op=ALU.subtract),
                        reads=[b_t8], writes=[b_rs])
                    add("act", lambda e, rs=rs: e.activation(out=rs[:, 5:6], in_=rs[:, 4:5], func=AF.Exp, scale=-1.0), reads=[b_rs], writes=[b_rs])
                    add("dve", lambda e, rs=rs: e.tensor_scalar(out=rs[:, 5:6], in0=rs[:, 5:6], scalar1=1.0, scalar2=None, op0=ALU.add), reads=[b_rs], writes=[b_rs])
                    add("dve", lambda e, rs=rs: e.reciprocal(out=rs[:, 5:6], in_=rs[:, 5:6]), reads=[b_rs], writes=[b_rs])
                    add("dve", lambda e, rs=rs: e.tensor_tensor(out=rs[:, 6:7], in0=rs[:, 5:6], in1=rs[:, 3:4], op=ALU.mult), reads=[b_rs], writes=[b_rs])
                    add("dve", lambda e, rs=rs: e.tensor_tensor(out=rs[:, 7:8], in0=rs[:, 3:4], in1=rs[:, 6:7], op=ALU.subtract), reads=[b_rs], writes=[b_rs])
                    (bkp, b_bkp) = (bkL, b_bkL)
                    add("pe", lambda e, bkp=bkp, mkb=mkb: e.matmul(bkp[:, 64:96], lhsT=ltri_b, rhs=mkb, start=True, stop=True),
                        reads=[b_ltri, b_mkb], writes=[b_bkp])
                    add("pe", lambda e, bkp=bkp, mkb=mkb: e.matmul(bkp[:, 96:128], lhsT=ones_b, rhs=mkb, start=True, stop=True),
                        reads=[b_ones, b_mkb], writes=[b_bkp])
                    cur_add[0] = make_recorder(lst2)
                    add("dve", lambda e, bkp=bkp, mk=mk: e.tensor_tensor(out=mk[:, 2, :], in0=bkp[:, 64:96], in1=base_t, op=ALU.add),
                        reads=[b_bkp, b_base], writes=[b_mk])
                    add("dve", lambda e, bkp=bkp: e.tensor_tensor(out=base_t, in0=bkp[:, 96:128], in1=base_t, op=ALU.add),
                        reads=[b_bkp, b_base], writes=[b_base])
                    add("dve", lambda e, el=el, mk=mk: e.tensor_scalar(out=el, in0=mk[:, 2, :], scalar1=float(C), scalar2=1e6, op0=ALU.is_ge, op1=ALU.mult),
                        reads=[b_mk], writes=[b_el])
                    add("dve", lambda e, el=el, mk=mk: e.tensor_tensor(out=mk[:, 2, :], in0=mk[:, 2, :], in1=el, op=ALU.add), reads=[b_mk, b_el], writes=[b_mk])
                    add("dve", lambda e, mk=mk: e.tensor_tensor(out=mk[:, 2, :], in0=mk[:, 2, :], in1=ec_t, op=ALU.add), reads=[b_mk, b_ec], writes=[b_mk])
                    for sl in range(2):
                        add("dve", lambda e, mk=mk, el=el, sl=sl: e.tensor_tensor(out=el, in0=mk[:, sl, :], in1=mk[:, 2, :], op=ALU.mult),
                            reads=[b_mk], writes=[b_el])
                        add("dve", lambda e, el=el, rs=rs, sl=sl: e.reduce_sum(out=rs[:, 8 + sl:9 + sl], in_=el, axis=AX.X), reads=[b_el], writes=[b_rs])
                    add("dve", lambda e, rs=rs: e.tensor_scalar_min(out=rs[:, 8:10], in0=rs[:, 8:10], scalar1=float(NSLOT)), reads=[b_rs], writes=[b_rs])
                    add("dve", lambda e, rs=rs, gi=gi: e.tensor_copy(out=dest_all[:, gi, :], in_=rs[:, 8:10]), reads=[b_rs], writes=[b_dest])
                    for sl in range(2):
                        add("dve", lambda e, rec=rec, sl=sl, rs=rs: e.tensor_copy(out=rec[:, sl, 1:2].bitcast(F32), in_=rs[:, 6 + sl:7 + sl]),
                            reads=[b_rs], writes=[b_rec])
                        add("dve", lambda e, rec=rec, sl=sl, gi=gi: e.tensor_scalar(out=rec[:, sl, 0:1], in0=iota_f[:, 0:1], scalar1=float(gi * 128),
                                                                                    scalar2=None, op0=ALU.add), reads=[b_iota], writes=[b_rec])
                    for sl in range(2):
                        add("pool", lambda e, rec=rec, sl=sl, gi=gi: e.indirect_dma_start(
                            out=SLOT, out_offset=bass.IndirectOffsetOnAxis(ap=dest_all[:, gi, sl:sl + 1], axis=0),
                            in_=rec[:, sl, :], in_offset=None, bounds_check=PR["bc_slot"], oob_is_err=False),
                            reads=[b_rec, b_dest], writes=[B_SLOT], dma=True)
                    cur_add[0] = outer_add
                    pending.append((lst1, lst2))
                    if len(pending) == 1:
                        flush_router()
            stbox = []

            def run_s1(k_):
                psum_pool[0] = "a"
                stbox.append(e_stage1(k_))
                psum_pool[0] = None

            def run_s2(k_, st_):
                psum_pool[0] = "b"
                e_stage2(k_, st_)
                psum_pool[0] = None

            run_s1(0)
            for k in range(NBK):
                st_cur = stbox[k]
                la = record(lambda: run_s1(k + 1)) if k + 1 < NBK else []
                lb = record(lambda: run_s2(k, st_cur))
                emit_merged(la, lb)
            A.release(m_e)

        if dev:
            dma("sp", dbg["dest"], dest_all, [b_dest], [B_OUT])

        dma("sp", cnt_out, base_t, [b_base], [B_OUT])
        A.release(persist_mark)
        m_f = A.mark()
        wge_rot = Rot("wge", [KC, FF], BF16, 3)
        wue_rot = Rot("wue", [KC, FF], BF16, 3)
        wde_rot = Rot("wde", [4, D], BF16, 3)
        srec_rot = Rot("srec", [2], I32, 2 * CT)
        xe_rot = Rot("xe", [D], BF16, 4)
        xeT_rot = Rot("xeT", [KC, C], BF16, 2)
        aT_rot = Rot("aT", [4, C], BF16, 2)
        sl_rot = Rot("silu", [512], F32, 3)
        ys_rot = Rot("ysb", [D], F32, 3)
        segs = [(s0, min(512, C - s0)) for s0 in range(0, C, 512)]

        def f_load(ex):
            (wge, b_wge) = wge_rot.next()
            (wue, b_wue) = wue_rot.next()
            (wde, b_wde) = wde_rot.next()
            dma("pool", wge, w_ge[ex].rearrange("(kc p) f -> p kc f", p=128), [], [b_wge])
            dma("pool", wue, w_ue[ex].rearrange("(kc p) f -> p kc f", p=128), [], [b_wue])
            dma("pool", wde, w_de[ex].rearrange("(f p) n -> p f n", p=128), [], [b_wde])
            (xeT, b_xeT) = xeT_rot.next()
            srecs = []
            for j in range(CT):
                (srec, b_srec) = srec_rot.next()
                (xe, b_xe) = xe_rot.next()
                row0 = ex * C + j * 128
                dma("sp", srec, SLOT[row0:row0 + 128, :], [B_SLOT], [b_srec])
                add("pool", lambda e, xe=xe, srec=srec: e.indirect_dma_start(
                    out=xe, out_offset=None, in_=HN3, in_offset=bass.IndirectOffsetOnAxis(ap=srec[:, 0:1], axis=0),
                    bounds_check=PR["bc_tok"], oob_is_err=False),
                    reads=[b_srec, B_HN3], writes=[b_xe], dma=True)
                transpose_to(xe, b_xe, xeT[:, :, j * 128:(j + 1) * 128], b_xeT, eng="act" if j % 2 == 0 else "dve")
                srecs.append((srec, b_srec))
            return (wge, b_wge, wue, b_wue, wde, b_wde, xeT, b_xeT, srecs)

        def f_gateup(st):
            (wge, b_wge, wue, b_wue, wde, b_wde, xeT, b_xeT, srecs) = st
            (aT, b_aT) = aT_rot.next()
            for f in range(4):
                for (s0, sn) in segs:
                    (bkg, b_bkg) = psum()
                    (bku, b_bku) = psum()
                    for kc in range(KC):
                        add("pe", lambda e, bkg=bkg, kc=kc, f=f, s0=s0, sn=sn, wge=wge, xeT=xeT: e.matmul(
                            bkg[:, 0:sn], lhsT=wge[:, kc, f * 128:(f + 1) * 128], rhs=xeT[:, kc, s0:s0 + sn],
                            start=(kc == 0), stop=(kc == KC - 1)), reads=[b_wge, b_xeT], writes=[b_bkg])
                    for kc in range(KC):
                        add("pe", lambda e, bku=bku, kc=kc, f=f, s0=s0, sn=sn, wue=wue, xeT=xeT: e.matmul(
                            bku[:, 0:sn], lhsT=wue[:, kc, f * 128:(f + 1) * 128], rhs=xeT[:, kc, s0:s0 + sn],
                            start=(kc == 0), stop=(kc == KC - 1)), reads=[b_wue, b_xeT], writes=[b_bku])
                    (sl_, b_sl) = sl_rot.next()
                    add("act", lambda e, sl_=sl_, bkg=bkg, sn=sn: e.activation(out=sl_[:, 0:sn], in_=bkg[:, 0:sn], func=AF.Silu),
                        reads=[b_bkg], writes=[b_sl])
                    add("dve", lambda e, sl_=sl_, bku=bku, aT=aT, f=f, s0=s0, sn=sn: e.tensor_tensor(
                        out=aT[:, f, s0:s0 + sn], in0=bku[:, 0:sn], in1=sl_[:, 0:sn], op=ALU.mult), reads=[b_bku, b_sl], writes=[b_aT])
            return (aT, b_aT)

        def f_down(ex, st, aTb):
            (wge, b_wge, wue, b_wue, wde, b_wde, xeT, b_xeT, srecs) = st
            (aT, b_aT) = aTb
            for j in range(CT):
                (srec, b_srec) = srecs[j]
                (ysb, b_ysb) = ys_rot.next()
                row0 = ex * C + j * 128
                for n in range(2):
                    (bk, b_bk) = psum()
                    for f in range(4):
                        add("pe", lambda e, bk=bk, f=f, j=j, n=n, aT=aT, wde=wde: e.matmul(
                            bk[:, :], lhsT=aT[:, f, j * 128:(j + 1) * 128], rhs=wde[:, f, n * 512:(n + 1) * 512],
                            start=(f == 0), stop=(f == 3)), reads=[b_aT, b_wde], writes=[b_bk])
                    if n == 0:
                        add("act", lambda e, bk=bk, ysb=ysb, n=n, srec=srec: e.activation(
                            out=ysb[:, n * 512:(n + 1) * 512], in_=bk[:, :], func=AF.Copy, scale=srec[:, 1:2].bitcast(F32)),
                            reads=[b_bk, b_srec], writes=[b_ysb])
                    else:
                        add("dve", lambda e, bk=bk, ysb=ysb, n=n, srec=srec: e.tensor_scalar(
                            out=ysb[:, n * 512:(n + 1) * 512], in0=bk[:, :], scalar1=srec[:, 1:2].bitcast(F32), scalar2=None, op0=ALU.mult),
                            reads=[b_bk, b_srec], writes=[b_ysb])
                dma("sp", YS[row0:row0 + 128, :], ysb, [b_ysb], [B_YS[ex]])

        st_next = f_load(0)
        for ex in range(NEXP):
            st_cur = st_next
            aTb = f_gateup(st_cur)
            if ex + 1 < NEXP:
                st_next = f_load(ex + 1)
            f_down(ex, st_cur, aTb)
        A.release(m_f)

        m_g = A.mark()
        hg_rot = Rot("hg", [D], F32, 3)
        y_rot = Rot("yg", [2, D], F32, 3)
        og_rot = Rot("og", [D], F32, 2)
        small = Rot("ssg", [2], F32, 4)
        junk = A.alloc("junkg", [D], BF16)
        gfin_t, b_gfin = A.alloc("gfin", [D], F32)
        dma("sp", gfin_t, g_fin.partition_broadcast(128), [], [b_gfin])
        for gi in range(NTT):
            (hg, b_hg) = hg_rot.next()
            (yg, b_yg) = y_rot.next()
            (og, b_og) = og_rot.next()
            dma("sp", hg, H[gi * 128:(gi + 1) * 128, :], [B_H[gi]], [b_hg])
            for sl in range(2):
                add("pool", lambda e, yg=yg, sl=sl, gi=gi: e.indirect_dma_start(
                    out=yg[:, sl, :], out_offset=None, in_=YS, in_offset=bass.IndirectOffsetOnAxis(ap=dest_all[:, gi, sl:sl + 1], axis=0),
                    bounds_check=PR["bc_slot"], oob_is_err=False),
                    reads=[b_dest] + B_YS, writes=[b_yg], dma=True)
            add("dve", lambda e, hg=hg, yg=yg: e.tensor_tensor(out=hg, in0=hg, in1=yg[:, 0, :], op=ALU.add), reads=[b_hg, b_yg], writes=[b_hg])
            add("dve", lambda e, hg=hg, yg=yg: e.tensor_tensor(out=hg, in0=hg, in1=yg[:, 1, :], op=ALU.add), reads=[b_hg, b_yg], writes=[b_hg])
            (ss, b_ss) = small.next()
            (jk, b_jk) = junk
            add("pool", lambda e, ss=ss: e.memset(ss, 0.0), writes=[b_ss])
            add("act", lambda e, ss=ss, hg=hg, jk=jk: e.activation(out=jk, in_=hg, func=AF.Square, accum_out=ss[:, 0:1]),
                reads=[b_hg], writes=[b_jk, b_ss])
            add("act", lambda e, ss=ss: e.activation(out=ss[:, 1:2], in_=ss[:, 0:1], func=AF.Ln, bias=eps_rms[:, 0:1], scale=1.0 / D),
                reads=[b_ss, b_eps], writes=[b_ss])
            add("act", lambda e, ss=ss: e.activation(out=ss[:, 0:1], in_=ss[:, 1:2], func=AF.Exp, scale=-0.5), reads=[b_ss], writes=[b_ss])
            add("dve", lambda e, ss=ss, hg=hg, og=og: e.scalar_tensor_tensor(out=og, in0=hg, scalar=ss[:, 0:1], in1=gfin_t, op0=ALU.mult, op1=ALU.mult),
                reads=[b_hg, b_ss, b_gfin], writes=[b_og])
            dma("sp", out[gi * 128:(gi + 1) * 128, :], og, [b_og], [B_OUT])
        A.release(m_g)
        if dev:
            dsl, b_dsl = A.alloc("dsl", [NSLOT // 128, 2], I32)
            dma("sp", dsl, SLOT[0:NSLOT, :].rearrange("(p a) c -> p a c", p=128), [B_SLOT], [b_dsl])
            dma("sp", dbg["slot"].rearrange("(p a) c -> p a c", p=128), dsl, [b_dsl], [B_OUT])
            dy_rot = Rot("dy", [D], F32, 2)
            dh_rot = Rot("dh", [D], BF16, 2)
            for i_ in range((NSLOT + 128) // 128):
                (dy, b_dy) = dy_rot.next()
                dma("sp", dy, YS[i_ * 128:(i_ + 1) * 128, :], B_YS, [b_dy])
                dma("sp", dbg["ys"][i_ * 128:(i_ + 1) * 128, :], dy, [b_dy], [B_OUT])
            for i_ in range((TOK + 128) // 128):
                (dh, b_dh) = dh_rot.next()
                dma("sp", dh, HN3[i_ * 128:(i_ + 1) * 128, :], [B_HN3], [b_dh])
                dma("sp", dbg["hn3"][i_ * 128:(i_ + 1) * 128, :], dh, [b_dh], [B_OUT])

        Sc.emit(block, esems, dsems)
        build.stats = dict(nops=Sc.nops, peak=A.peak)
    return nc


def _consts(C):
    p = np.arange(128)
    half = 32
    inv_freq = (10000.0 ** (-np.arange(half, dtype=np.float32) / half)).astype(np.float32)
    c_invf = inv_freq[p % 32].reshape(128, 1).astype(np.float32)
    kk = np.arange(128)[:, None]
    qq = np.arange(128)[None, :]
    c_mask = np.concatenate([(kk <= qq), (kk >= qq)], axis=1).astype(np.float32)
    c_ltri = (kk < qq).astype(np.float32)
    c_ident = np.eye(128, dtype=np.float32)
    c_ec = np.broadcast_to((np.arange(32, dtype=np.float32) * C)[None, :], (128, 32)).copy()
    c_sel = np.zeros((65, 64), np.float32)
    c_sel[64, :] = 1.0
    c_iota = np.arange(128, dtype=np.float32).reshape(128, 1)
    return dict(c_invf=c_invf, c_mask=c_mask, c_ltri=c_ltri, c_ident=c_ident, c_ec=c_ec, c_sel=c_sel, c_iota=c_iota)


def _w_in_perm():
    cols = []
    for g in range(3):
        heads = range(4 * g, 4 * g + 4)
        for base in (0, 768):
            for part in (0, 32):
                for h in heads:
                    cols.extend(range(base + 64 * h + part, base + 64 * h + part + 32))
        cols.extend(range(1536 + 256 * g, 1536 + 256 * (g + 1)))
    cols.extend(range(2304, 5376))
    return np.asarray(cols)


def prep_weights(inp, C):
    f = lambda a: np.ascontiguousarray(np.asarray(a, dtype=np.float32))
    w = {}
    w["w_in"] = f(np.asarray(inp["w_in"])[0][:, _w_in_perm()])
    w["g_mix"] = f(inp["mix_norm_g"][0])
    w["g_x"] = f(inp["xattn_norm_g"][0])
    w["g_mem"] = f(inp["mem_norm_g"][0])
    w["g_moe"] = f(inp["moe_norm_g"][0])
    w["g_fin"] = f(inp["final_norm_g"])
    w["bgt"] = f(np.asarray(inp["b_gates"])[0].reshape(16, 128).T)
    w["ws_t"] = f(np.asarray(inp["w_spatial"])[0].transpose(0, 2, 1))
    w["bsp"] = f(np.asarray(inp["b_spatial"])[0].reshape(512))
    w["vgam"] = f(inp["v_norm_g"][0])
    w["vbet_t"] = f(np.asarray(inp["v_norm_b"])[0].reshape(4, 128).T)
    w["w_oa"] = f(inp["w_out_a"][0])
    w["w_ob"] = f(inp["w_out_b"][0])
    w["w_o"] = f(inp["w_out"][0])
    w["w_qx"] = f(inp["w_q_x"][0])
    w["w_kvx"] = f(inp["w_kv_x"][0])
    w["w_ox"] = f(inp["w_o_x"][0])
    w["w_r"] = f(np.concatenate([np.asarray(inp["w_router_grp"])[0], np.asarray(inp["w_router_exp"])[0]], axis=1))
    w["b_r"] = f(np.concatenate([np.asarray(inp["b_router_grp"])[0], np.asarray(inp["b_router_exp"])[0]], axis=0))
    w["w_ge"] = f(inp["w_gate_e"][0])
    w["w_ue"] = f(inp["w_up_e"][0])
    w["w_de"] = f(inp["w_down_e"][0])
    w.update(_consts(C))
    return w


def run(inp, n_cores, NB, S, C, dev=False):
    nc = build(NB, S, C, dev=dev)
    w = prep_weights(inp, C)
    x = np.asarray(inp["x"], dtype=np.float32)
    mem = np.asarray(inp["mem"], dtype=np.float32)
    pos = np.asarray(inp["positions"], dtype=np.int32)
    in_maps = []
    for c in range(n_cores):
        m = dict(w)
        m["x"] = np.ascontiguousarray(x[c * NB:(c + 1) * NB])
        m["mem"] = np.ascontiguousarray(mem[c * NB:(c + 1) * NB])
        m["pos"] = np.ascontiguousarray(pos[c * NB:(c + 1) * NB])
        in_maps.append(m)
    res = run_bass_kernel_spmd(nc, in_maps, core_ids=list(range(n_cores)))
    outs = [r["out"].reshape(NB, S, D) for r in res.results]
    try:
        print("[kernel] max routed rows per (core, expert):", [int(r["cnt"][0].max()) for r in res.results], "capacity", C, flush=True)
    except Exception:
        pass
    full = np.concatenate(outs, axis=0).astype(np.float32)
    if dev:
        return full, res.results
    return full


def kernel(**inputs):
    return run(inputs, n_cores=8, NB=2, S=4096, C=1152)
```

```python
import math
from contextlib import ExitStack

import numpy as np
import concourse.bass as bass
import concourse.mybir as mybir
from concourse.bass_utils import run_bass_kernel_spmd

F32 = mybir.dt.float32
BF16 = mybir.dt.bfloat16
I32 = mybir.dt.int32
ALU = mybir.AluOpType
AF = mybir.ActivationFunctionType
AX = mybir.AxisListType

D = 1024
KC = 8
NMEM = 256
NEXP = 32
FF = 512
RMS_EPS = 1e-6
LN_EPS = 1e-5
TWO_PI = 2.0 * math.pi
CW1 = 6.28125
CW2 = TWO_PI - CW1
PI_SAFE = 3.1415925

ENGS = ("pe", "act", "dve", "pool", "sp")


class Buf:
    __slots__ = ("name", "w", "r")

    def __init__(self, name):
        self.name = name
        self.w = None
        self.r = []


class Op:
    __slots__ = ("eng", "fn", "dma", "deps", "signal", "count", "sem", "semval", "prev_on_sem")

    def __init__(self, eng, fn, dma):
        self.eng = eng
        self.fn = fn
        self.dma = dma
        self.deps = []
        self.signal = False
        self.count = 0
        self.sem = None
        self.semval = 0
        self.prev_on_sem = None


class Sched:
    def __init__(self, n_dma_sems=40):
        self.ops = {e: [] for e in ENGS}
        self.n_dma_sems = n_dma_sems
        self.dma_rr = 0
        self.dma_last = [None] * n_dma_sems
        self.dma_val = [0] * n_dma_sems
        self.nops = 0
        self.pool_regs = {}
        self.pool_reg_handles = {}

    def add(self, eng, fn, reads=(), writes=(), dma=False):
        op = Op(eng, fn, dma)
        self.nops += 1
        deps = {}
        for b in reads:
            if b.w is not None:
                deps[id(b.w)] = (b.w, "raw")
        for b in writes:
            if b.w is not None and id(b.w) not in deps:
                deps[id(b.w)] = (b.w, "waw")
            for r in b.r:
                if id(r) not in deps:
                    deps[id(r)] = (r, "war")
        for b in reads:
            b.r.append(op)
        for b in writes:
            b.w = op
            b.r = []
        for d, kind in deps.values():
            if d is op:
                continue
            if d.dma:
                op.deps.append(d)
            elif d.eng != eng:
                op.deps.append(d)
                d.signal = True
            elif kind == "raw" and eng != "pe":
                op.deps.append(d)
                d.signal = True
        if dma:
            s = self.dma_rr
            self.dma_rr = (self.dma_rr + 1) % self.n_dma_sems
            op.sem = s
            op.prev_on_sem = self.dma_last[s]
            self.dma_val[s] += 16
            op.semval = self.dma_val[s]
            self.dma_last[s] = op
        self.ops[eng].append(op)
        return op

    def emit(self, block, esems, dsems):
        for e in ENGS:
            c = 0
            for op in self.ops[e]:
                if not op.dma and op.signal:
                    c += 1
                    op.count = c
        sched = self

        def run(e, eng):
            seen = {}
            for op in sched.ops[e]:
                best = {}
                if op.dma and op.prev_on_sem is not None:
                    p = op.prev_on_sem
                    best[("d", p.sem)] = p.semval
                for d in op.deps:
                    k = ("d", d.sem) if d.dma else ("e", d.eng)
                    v = d.semval if d.dma else d.count
                    if v > best.get(k, 0):
                        best[k] = v
                for k, v in best.items():
                    if seen.get(k, 0) >= v:
                        continue
                    seen[k] = v
                    eng.wait_ge(dsems[k[1]] if k[0] == "d" else esems[k[1]], v)
                ins = op.fn(eng)
                if op.dma:
                    ins.then_inc(dsems[op.sem], 16)
                elif op.signal:
                    ins.then_inc(esems[e], 1)

        @block.tensor
        def _(eng):
            run("pe", eng)

        @block.scalar
        def _(eng):
            run("act", eng)

        @block.vector
        def _(eng):
            run("dve", eng)

        @block.gpsimd
        def _(eng):
            for name_, val_ in sched.pool_regs.items():
                r_ = eng.alloc_register(name_)
                eng.reg_mov(r_, val_)
                sched.pool_reg_handles[name_] = r_
            run("pool", eng)

        @block.sync
        def _(eng):
            run("sp", eng)
            for s_ in range(sched.n_dma_sems):
                if sched.dma_val[s_] > 0:
                    eng.wait_ge(dsems[s_], sched.dma_val[s_])


class Arena:
    def __init__(self, base_ap, nbytes):
        self.base = base_ap
        self.cap = nbytes
        self.top = 0
        self.hi = nbytes
        self.live = []
        self.freed = []
        self.peak = 0

    def alloc(self, name, free_shape, dtype, nbufs=1, parts=128, top=False):
        esz = 2 if dtype == BF16 else 4
        n = int(np.prod(free_shape))
        nbytes = (n * esz + 63) // 64 * 64
        if top:
            end = self.hi
            start = end - nbytes
            assert start >= self.top, f"SBUF arena overflow (top) allocating {name}"
            self.hi = start
        else:
            start = self.top
            end = start + nbytes
            assert end <= self.hi, f"SBUF arena overflow allocating {name}: {end} > {self.hi}"
            self.top = end
        self.peak = max(self.peak, self.top + (self.cap - self.hi))
        v = self.base[0:parts, start // 2:start // 2 + n * esz // 2]
        if dtype != BF16:
            v = v.bitcast(dtype)
        if len(free_shape) == 2:
            v = v.rearrange("p (a b) -> p a b", a=free_shape[0])
        elif len(free_shape) == 3:
            v = v.rearrange("p (a b c) -> p a b c", a=free_shape[0], b=free_shape[1])
        hz = []
        for (s, e, ops) in self.freed:
            if s < end and e > start:
                hz.extend(ops)
        bufs = []
        for i in range(nbufs):
            b = Buf(f"{name}{i}")
            b.r = list(hz)
            bufs.append(b)
        self.live.append((start, end, bufs))
        return (v, bufs[0]) if nbufs == 1 else (v, bufs)

    def mark(self):
        return self.top

    def release_top(self):
        keep = []
        for (s, e, bufs) in self.live:
            if s >= self.hi:
                ops = []
                for b in bufs:
                    if b.w is not None:
                        ops.append(b.w)
                    ops.extend(b.r)
                self.freed.append((s, e, ops))
            else:
                keep.append((s, e, bufs))
        self.live = keep
        self.hi = self.cap

    def release(self, mark):
        keep = []
        for (s, e, bufs) in self.live:
            if s >= mark and e <= self.hi:
                ops = []
                for b in bufs:
                    if b.w is not None:
                        ops.append(b.w)
                    ops.extend(b.r)
                self.freed.append((s, e, ops))
            else:
                keep.append((s, e, bufs))
        self.live = keep
        self.top = mark


def build(NB, S, C, dev=False):
    NT = S // 128
    NBK = S // 512
    TOK = NB * S
    NTT = TOK // 128
    CT = C // 128
    NSLOT = NEXP * C
    DIL = (1, 4, 16)

    nc = bass.Bass("TRN2", target_bir_lowering=False)

    def din(name, shape, dt=F32):
        return nc.dram_tensor(name, shape, dt, kind="ExternalInput").ap()

    x = din("x", [NB, S, D])
    mem = din("mem", [NB, NMEM, D])
    pos = din("pos", [NB, S], I32)
    w_in = din("w_in", [D, 5376])
    g_mix = din("g_mix", [D])
    g_x = din("g_x", [D])
    g_mem = din("g_mem", [D])
    g_moe = din("g_moe", [D])
    g_fin = din("g_fin", [D])
    bgt = din("bgt", [128, 16])
    ws_t = din("ws_t", [4, 128, 128])
    bsp = din("bsp", [512])
    vgam = din("vgam", [512])
    vbet_t = din("vbet_t", [128, 4])
    w_oa = din("w_oa", [256, D])
    w_ob = din("w_ob", [512, D])
    w_o = din("w_o", [D, D])
    w_qx = din("w_qx", [D, D])
    w_kvx = din("w_kvx", [D, 2 * D])
    w_ox = din("w_ox", [D, D])
    w_r = din("w_r", [D, 36])
    b_r = din("b_r", [36])
    w_ge = din("w_ge", [NEXP, D, FF])
    w_ue = din("w_ue", [NEXP, D, FF])
    w_de = din("w_de", [NEXP, FF, D])
    c_invf = din("c_invf", [128, 1])
    c_mask = din("c_mask", [128, 256])
    c_ltri = din("c_ltri", [128, 128])
    c_ident = din("c_ident", [128, 128])
    c_ec = din("c_ec", [128, 32])
    c_sel = din("c_sel", [65, 64])
    c_iota = din("c_iota", [128, 1])

    out = nc.dram_tensor("out", [TOK, D], F32, kind="ExternalOutput").ap()
    cnt_out = nc.dram_tensor("cnt", [128, 32], F32, kind="ExternalOutput").ap()
    dbg = {}
    if dev:
        dbg["ya"] = nc.dram_tensor("dbg_ya", [NB, 64, 4, S], F32, kind="ExternalOutput").ap()
        dbg["yb"] = nc.dram_tensor("dbg_yb", [NB, 128, 4, S], F32, kind="ExternalOutput").ap()
        dbg["h1"] = nc.dram_tensor("dbg_h1", [TOK, D], F32, kind="ExternalOutput").ap()
        dbg["h2"] = nc.dram_tensor("dbg_h2", [TOK, D], F32, kind="ExternalOutput").ap()
        dbg["dest"] = nc.dram_tensor("dbg_dest", [128, NTT, 2], I32, kind="ExternalOutput").ap()
        dbg["slot"] = nc.dram_tensor("dbg_slot", [NSLOT, 2], I32, kind="ExternalOutput").ap()
        dbg["ys"] = nc.dram_tensor("dbg_ys", [NSLOT + 128, D], F32, kind="ExternalOutput").ap()
        dbg["hn3"] = nc.dram_tensor("dbg_hn3", [TOK + 128, D], BF16, kind="ExternalOutput").ap()

    COS = nc.dram_tensor("scr_cos", [NB, 128, S], F32, kind="Internal").ap()
    SIN = nc.dram_tensor("scr_sin", [NB, 128, S], F32, kind="Internal").ap()
    H = nc.dram_tensor("scr_h", [TOK, D], F32, kind="Internal").ap()
    XN = nc.dram_tensor("scr_xn", [NB, 128, KC, S], BF16, kind="Internal").ap()
    HN3 = nc.dram_tensor("scr_hn3", [TOK + 128, D], BF16, kind="Internal").ap()
    SLOT = nc.dram_tensor("scr_slot", [NSLOT + 128, 2], I32, kind="Internal").ap()
    YS = nc.dram_tensor("scr_ys", [NSLOT + 128, D], F32, kind="Internal").ap()

    ARENA_BYTES = 204 * 1024
    es = ExitStack()
    with es:
        arena_t = es.enter_context(nc.sbuf_tensor("arena", [128, ARENA_BYTES // 2], BF16))
        banks = [es.enter_context(nc.psum_tensor(f"bank{i}", [128, 512], F32)) for i in range(8)]
        esems = {e: es.enter_context(nc.semaphore("es_" + e)) for e in ENGS}
        NDS = 40
        dsems = [es.enter_context(nc.semaphore(f"ds{i}")) for i in range(NDS)]
        block = es.enter_context(nc.Block())
        Sc = Sched(NDS)
        Sc.pool_regs = {"bc_slot": NSLOT + 127, "bc_tok": TOK + 127}
        PR = Sc.pool_reg_handles
        A = Arena(arena_t[:, :], ARENA_BYTES)
        real_add = Sc.add
        cur_add = [Sc.add]

        def add(*a_, **k_):
            return cur_add[0](*a_, **k_)

        def record(fn_):
            lst = []
            prev_ = cur_add[0]
            cur_add[0] = make_recorder(lst)
            fn_()
            cur_add[0] = prev_
            return lst

        def emit_interleaved(lists):
            n_ = max(len(l_) for l_ in lists)
            for i_ in range(n_):
                for l_ in lists:
                    if i_ < len(l_):
                        cur_add[0](*l_[i_])

        def emit_merged(la, lb):
            na, nb_ = len(la), len(lb)
            ia = 0
            for ib in range(nb_):
                cur_add[0](*lb[ib])
                want = ((ib + 1) * na) // max(nb_, 1)
                while ia < want:
                    cur_add[0](*la[ia])
                    ia += 1
            while ia < na:
                cur_add[0](*la[ia])
                ia += 1

        def make_recorder(lst):
            def rec_(eng, fn, reads=(), writes=(), dma=False):
                lst.append((eng, fn, tuple(reads), tuple(writes), dma))
            return rec_

        bank_bufs = [Buf(f"bank{i}") for i in range(8)]
        bank_rr = [0]

        psum_pool = [None]
        pool_rr = {"a": 0, "b": 0}
        pool_banks = {"a": [0, 1, 2, 3], "b": [4, 5, 6, 7]}

        def psum():
            if psum_pool[0] is not None:
                pl = pool_banks[psum_pool[0]]
                i = pl[pool_rr[psum_pool[0]] % len(pl)]
                pool_rr[psum_pool[0]] += 1
                return banks[i], bank_bufs[i]
            i = bank_rr[0]
            bank_rr[0] = (i + 1) % 8
            return banks[i], bank_bufs[i]

        B_COS = [Buf(f"cos{b}") for b in range(NB)]
        B_H = [Buf(f"H{i}") for i in range(NTT)]
        B_HN3 = Buf("HN3")
        B_XN = [Buf(f"XN{k}") for k in range(NBK)]
        B_SLOT = Buf("SLOT")
        B_YS = [Buf(f"YS{e}") for e in range(NEXP)]
        B_OUT = Buf("OUT")

        class Rot:
            def __init__(self, name, free_shape, dtype, n, parts=128):
                self.items = [A.alloc(f"{name}{i}", free_shape, dtype, parts=parts) for i in range(n)]
                self.i = 0

            def next(self):
                it = self.items[self.i]
                self.i = (self.i + 1) % len(self.items)
                return it

        def dma(eng, out_ap, in_ap, reads, writes):
            return add(eng, lambda e: e.dma_start(out=out_ap, in_=in_ap), reads=reads, writes=writes, dma=True)

        ident_b, b_ident_b = A.alloc("ident_b", [128], BF16)
        ident_f, b_ident_f = A.alloc("ident_f", [128], F32)
        ones_b, b_ones = A.alloc("ones_b", [128], BF16)
        mask_b, b_mask = A.alloc("mask_b", [256], BF16)
        mask4, b_mask4 = A.alloc("mask4", [4, 256], BF16)
        ltri_b, b_ltri = A.alloc("ltri_b", [128], BF16)
        invf, b_invf = A.alloc("invf", [1], F32)
        ec_t, b_ec = A.alloc("ec", [32], F32)
        sel_f, b_sel = A.alloc("sel", [64], BF16)
        iota_f, b_iota = A.alloc("iota", [1], F32)
        bgt_t, b_bgt = A.alloc("bgt", [16], F32)
        wr_t, b_wr = A.alloc("wr", [KC, 36], F32)
        br_t, b_br = A.alloc("br", [36], F32)
        base_t, b_base = A.alloc("base", [32], F32)
        dest_all, b_dest = A.alloc("dest_all", [NTT, 2], I32)
        eps_rms, b_eps = A.alloc("eps_rms", [1], F32)
        eps_ln, b_epsln = A.alloc("eps_ln", [1], F32)

        dma("pool", ident_b, c_ident, [], [b_ident_b])
        dma("sp", ident_f, c_ident, [], [b_ident_f])
        dma("pool", mask_b, c_mask, [], [b_mask])
        dma("pool", ltri_b, c_ltri, [], [b_ltri])
        dma("sp", invf, c_invf, [], [b_invf])
        dma("sp", ec_t, c_ec, [], [b_ec])
        dma("pool", sel_f[0:65, :], c_sel, [], [b_sel])
        dma("sp", iota_f, c_iota, [], [b_iota])
        dma("sp", bgt_t, bgt, [], [b_bgt])
        dma("sp", wr_t, w_r.rearrange("(kc p) n -> p kc n", p=128), [], [b_wr])
        dma("sp", br_t, b_r.partition_broadcast(128), [], [b_br])
        add("pool", lambda e: e.memset(ones_b, 1.0), writes=[b_ones])
        for hh_ in range(4):
            add("pool", lambda e, hh_=hh_: e.tensor_copy(out=mask4[:, hh_, :], in_=mask_b), reads=[b_mask], writes=[b_mask4])
        add("pool", lambda e: e.memset(base_t, 0.0), writes=[b_base])
        add("pool", lambda e: e.memset(eps_rms, RMS_EPS), writes=[b_eps])
        add("pool", lambda e: e.memset(eps_ln, LN_EPS), writes=[b_epsln])

        m0 = A.mark()
        zrow, b_zrow = A.alloc("zrow", [D], BF16)
        slot0, b_slot0 = A.alloc("slot0", [NSLOT // 128, 2], I32)
        add("pool", lambda e: e.memset(zrow, 0.0), writes=[b_zrow])
        dma("sp", HN3[TOK:TOK + 128, :], zrow, [b_zrow], [B_HN3])
        zrowf, b_zrowf = A.alloc("zrowf", [D], F32)
        add("pool", lambda e: e.memset(zrowf, 0.0), writes=[b_zrowf])
        dma("sp", YS[NSLOT:NSLOT + 128, :], zrowf, [b_zrowf], [B_YS[0]])
        add("pool", lambda e: e.memset(slot0[:, :, 0:1], TOK), writes=[b_slot0])
        add("pool", lambda e: e.memset(slot0[:, :, 1:2], 0), writes=[b_slot0])
        dma("sp", SLOT[0:NSLOT, :].rearrange("(p a) c -> p a c", p=128), slot0, [b_slot0], [B_SLOT])
        A.release(m0)

        def rms_tile(src_ap, b_src, g_tile, b_g, dst_bf, b_dst, small, junk, dst_f32=None, b_dst32=None):
            (ss, b_ss) = small.next()
            (jk, b_jk) = junk
            add("pool", lambda e: e.memset(ss, 0.0), writes=[b_ss])
            add("act", lambda e: e.activation(out=jk, in_=src_ap, func=AF.Square, accum_out=ss[:, 0:1]),
                reads=[b_src], writes=[b_jk, b_ss])
            add("act", lambda e: e.activation(out=ss[:, 1:2], in_=ss[:, 0:1], func=AF.Ln, bias=eps_rms[:, 0:1],
                                              scale=1.0 / D), reads=[b_ss, b_eps], writes=[b_ss])
            add("act", lambda e: e.activation(out=ss[:, 0:1], in_=ss[:, 1:2], func=AF.Exp, scale=-0.5), reads=[b_ss], writes=[b_ss])
            if dst_f32 is not None:
                add("dve", lambda e: e.scalar_tensor_tensor(out=dst_f32, in0=src_ap, scalar=ss[:, 0:1], in1=g_tile,
                                                            op0=ALU.mult, op1=ALU.mult),
                    reads=[b_src, b_ss, b_g], writes=[b_dst32])
                add("act", lambda e: e.copy(out=dst_bf, in_=dst_f32), reads=[b_dst32], writes=[b_dst])
            else:
                add("dve", lambda e: e.scalar_tensor_tensor(out=dst_bf, in0=src_ap, scalar=ss[:, 0:1], in1=g_tile,
                                                            op0=ALU.mult, op1=ALU.mult),
                    reads=[b_src, b_ss, b_g], writes=[b_dst])

        def transpose_to(src_bf, b_src, dst_ap3, b_dst, eng="act"):
            (bk, b_bk) = psum()
            pv = bk[:, :].bitcast(BF16)
            for c in range(KC):
                add("pe", lambda e, c=c: e.transpose(out=pv[:, c * 128:(c + 1) * 128], in_=src_bf[:, c * 128:(c + 1) * 128],
                                                     identity=ident_b), reads=[b_src, b_ident_b], writes=[b_bk])
            pv3 = pv.rearrange("p (c t) -> p c t", c=KC)
            if eng == "act":
                add("act", lambda e: e.copy(out=dst_ap3, in_=pv3), reads=[b_bk], writes=[b_dst])
            else:
                add("dve", lambda e: e.tensor_copy(out=dst_ap3, in_=pv3), reads=[b_bk], writes=[b_dst])

        def gelu_from_psum(bk, b_bk, dst, b_dst, tmp_rot, width):
            (t1, b_t1) = tmp_rot.next()
            (t2, b_t2) = tmp_rot.next()
            pz = bk[:, 0:width]
            add("act", lambda e: e.activation(out=t1[:, 0:width], in_=pz, func=AF.Square), reads=[b_bk], writes=[b_t1])
            add("dve", lambda e: e.tensor_scalar(out=t1[:, 0:width], in0=t1[:, 0:width], scalar1=0.044715, scalar2=1.0,
                                                 op0=ALU.mult, op1=ALU.add), reads=[b_t1], writes=[b_t1])
            add("dve", lambda e: e.tensor_tensor(out=t2[:, 0:width], in0=pz, in1=t1[:, 0:width], op=ALU.mult),
                reads=[b_bk, b_t1], writes=[b_t2])
            add("act", lambda e: e.activation(out=t2[:, 0:width], in_=t2[:, 0:width], func=AF.Sigmoid,
                                              scale=1.5957691216057308), reads=[b_t2], writes=[b_t2])
            add("dve", lambda e: e.tensor_tensor(out=dst, in0=pz, in1=t2[:, 0:width], op=ALU.mult),
                reads=[b_bk, b_t2], writes=[b_dst])

        wsT, b_wsT = A.alloc("wsT", [4, 128], BF16)
        B2, b_B2 = A.alloc("B2", [4, 128], F32)
        gamrow, b_gamrow = A.alloc("gamrow", [512], F32)
        m0 = A.mark()
        bspb, b_bspb = A.alloc("bspb", [4, 128], F32)
        bet_t, b_bet = A.alloc("bet", [4], F32)
        dma("pool", wsT, ws_t.rearrange("g s t -> s g t"), [], [b_wsT])
        dma("sp", bspb, bsp.partition_broadcast(128).rearrange("p (g t) -> p g t", g=4), [], [b_bspb])
        dma("sp", bet_t, vbet_t, [], [b_bet])
        dma("sp", gamrow, vgam.partition_broadcast(128), [], [b_gamrow])
        add("dve", lambda e: e.tensor_tensor(out=wsT, in0=wsT, in1=mask_b[:, 0:128].unsqueeze(1).to_broadcast([128, 4, 128]),
                                             op=ALU.mult), reads=[b_wsT, b_mask], writes=[b_wsT])
        (bk, b_bk) = psum()
        add("pe", lambda e: e.matmul(bk[:, :], lhsT=ones_b, rhs=wsT.rearrange("p g t -> p (g t)"), start=True, stop=True),
            reads=[b_ones, b_wsT], writes=[b_bk])
        for g in range(4):
            add("dve", lambda e, g=g: e.scalar_tensor_tensor(out=B2[:, g, :], in0=bk[:, g * 128:(g + 1) * 128],
                                                             scalar=bet_t[:, g:g + 1], in1=bspb[:, g, :],
                                                             op0=ALU.mult, op1=ALU.add),
                reads=[b_bk, b_bet, b_bspb], writes=[b_B2])
        A.release(m0)

        persist_mark = A.mark()

        for b in range(NB):
            m_r = A.mark()
            posi, b_posi = A.alloc("posi", [S], I32)
            dma("sp", posi, pos[b].partition_broadcast(128), [], [b_posi])
            rt = Rot("rt", [512], F32, 8)
            ri = Rot("ri", [512], I32, 8)
            ang_rot = Rot("ang", [512], F32, 4)
            tb_rot = Rot("tb", [512], F32, 8)
            def r_block(k, b=b):
                blk = slice(k * 512, (k + 1) * 512)
                (ang, b_ang) = ang_rot.next()
                add("dve", lambda e, ang=ang, blk=blk: e.tensor_copy(out=ang, in_=posi[:, blk]), reads=[b_posi], writes=[b_ang])
                add("dve", lambda e, ang=ang: e.tensor_scalar(out=ang, in0=ang, scalar1=invf[:, 0:1], scalar2=None, op0=ALU.mult),
                    reads=[b_ang, b_invf], writes=[b_ang])
                for (dst, shift) in ((SIN, 0.0), (COS, 0.5 * math.pi)):
                    (u, b_u) = rt.next()
                    (ki, b_ki) = ri.next()
                    (tb, b_tb) = tb_rot.next()
                    add("dve", lambda e, u=u, ang=ang, shift=shift: e.tensor_scalar(
                        out=u, in0=ang, scalar1=shift, scalar2=1.0 / TWO_PI, op0=ALU.add, op1=ALU.mult),
                        reads=[b_ang], writes=[b_u])
                    add("dve", lambda e, u=u, ki=ki: e.tensor_copy(out=ki, in_=u), reads=[b_u], writes=[b_ki])
                    add("dve", lambda e, u=u, ki=ki: e.tensor_copy(out=u, in_=ki), reads=[b_ki], writes=[b_u])
                    add("dve", lambda e, u=u, ang=ang, tb=tb: e.scalar_tensor_tensor(
                        out=tb, in0=u, scalar=-CW1, in1=ang, op0=ALU.mult, op1=ALU.add), reads=[b_u, b_ang], writes=[b_tb])
                    add("dve", lambda e, u=u, tb=tb: e.scalar_tensor_tensor(
                        out=tb, in0=u, scalar=-CW2, in1=tb, op0=ALU.mult, op1=ALU.add), reads=[b_u, b_tb], writes=[b_tb])
                    add("dve", lambda e, tb=tb, shift=shift: e.tensor_scalar(
                        out=tb, in0=tb, scalar1=shift, scalar2=PI_SAFE, op0=ALU.add, op1=ALU.min), reads=[b_tb], writes=[b_tb])
                    add("dve", lambda e, tb=tb: e.tensor_scalar_max(out=tb, in0=tb, scalar1=-PI_SAFE), reads=[b_tb], writes=[b_tb])
                    add("act", lambda e, tb=tb: e.activation(out=tb, in_=tb, func=AF.Sin), reads=[b_tb], writes=[b_tb])
                    dma("sp", dst[b, :, blk], tb, [b_tb], [B_COS[b]])
            for k0 in range(0, NBK, 4):
                emit_interleaved([record(lambda k=k: r_block(k)) for k in range(k0, min(NBK, k0 + 4))])
            A.release(m_r)

        for b in range(NB):
            A.release(persist_mark)

            xnT, b_xnT = A.alloc("xnT", [KC, S], BF16, nbufs=NBK)
            m_a = A.mark()
            xt_rot = Rot("xt", [D], F32, 4)
            xnb_rot = Rot("xnb", [D], BF16, 4)
            small = Rot("ss", [2], F32, 8)
            junk = A.alloc("junk", [D], BF16)
            gmix_t, b_gmix = A.alloc("gmix", [D], F32)
            dma("sp", gmix_t, g_mix.partition_broadcast(128), [], [b_gmix])
            def a_tile(i, b=b):
                (xt, b_xt) = xt_rot.next()
                (xnb, b_xnb) = xnb_rot.next()
                dma("sp", xt, x[b, i * 128:(i + 1) * 128, :], [], [b_xt])
                rms_tile(xt, b_xt, gmix_t, b_gmix, xnb, b_xnb, small, junk)
                transpose_to(xnb, b_xnb, xnT[:, :, i * 128:(i + 1) * 128], b_xnT[i // 4], eng="act" if i % 2 == 0 else "dve")

            for kk in range(NBK):
                emit_interleaved([record(lambda i=i: a_tile(i)) for i in range(kk * 4, kk * 4 + 4)])
                dma("sp", XN[b, :, :, kk * 512:(kk + 1) * 512], xnT[:, :, kk * 512:(kk + 1) * 512], [b_xnT[kk]], [B_XN[kk]])
            A.release(m_a)

            acc, b_acc = A.alloc("acc", [4, S], BF16, parts=128)
            m_b = A.mark()
            qk, b_qk = A.alloc("qk", [4, S], BF16)
            wg_rot = Rot("wg", [KC, 768], BF16, 1)
            cs_rot = Rot("cs", [2, 512], F32, 2)
            ev_rot = Rot("ev", [512], F32, 4)
            rp_rot = Rot("rp", [512], F32, 4)
            pe_rot = Rot("pexp", [4, 256], BF16, 4)
            v_rot = [A.alloc(f"vv{i}", [4, 65], BF16) for i in range(4)]
            for (vv, b_vv) in v_rot:
                add("pool", lambda e, vv=vv: e.memset(vv, 1.0), writes=[b_vv])
            for g in range(3):
                d = DIL[g]
                L = S // d
                nb = L // 128
                (wg, b_wg) = wg_rot.next()
                dma("pool", wg, w_in.rearrange("(kc p) n -> p kc n", p=128)[:, :, g * 768:(g + 1) * 768], [], [b_wg])
                for k in range(NBK):
                    blk = slice(k * 512, (k + 1) * 512)
                    (cs, b_cs) = cs_rot.next()
                    dma("sp", cs[:, 0, :], COS[b, :, blk], [B_COS[b]], [b_cs])
                    dma("sp", cs[:, 1, :], SIN[b, :, blk], [B_COS[b]], [b_cs])
                    for which in range(2):
                        evs = []
                        for part in range(2):
                            col = (which * 2 + part) * 128
                            (bk, b_bk) = psum()
                            for kc in range(KC):
                                add("pe", lambda e, bk=bk, kc=kc, col=col, blk=blk, wg=wg: e.matmul(
                                    bk[:, :], lhsT=wg[:, kc, col:col + 128], rhs=xnT[:, kc, blk],
                                    start=(kc == 0), stop=(kc == KC - 1)), reads=[b_wg, b_xnT[k]], writes=[b_bk])
                            (ev, b_ev) = ev_rot.next()
                            add("act", lambda e, ev=ev, bk=bk: e.copy(out=ev, in_=bk[:, :]), reads=[b_bk], writes=[b_ev])
                            evs.append((ev, b_ev))
                        (eA, b_eA), (eB, b_eB) = evs
                        (t1, b_t1) = rp_rot.next()
                        (t2, b_t2) = rp_rot.next()
                        oA = qk[:, which * 2, blk]
                        oB = qk[:, which * 2 + 1, blk]
                        add("dve", lambda e, t1=t1, eA=eA, cs=cs: e.tensor_tensor(out=t1, in0=eA, in1=cs[:, 0, :], op=ALU.mult),
                            reads=[b_eA, b_cs], writes=[b_t1])
                        add("dve", lambda e, t2=t2, eB=eB, cs=cs: e.tensor_tensor(out=t2, in0=eB, in1=cs[:, 1, :], op=ALU.mult),
                            reads=[b_eB, b_cs], writes=[b_t2])
                        add("dve", lambda e, t1=t1, t2=t2, oA=oA: e.tensor_tensor(out=oA, in0=t1, in1=t2, op=ALU.subtract),
                            reads=[b_t1, b_t2], writes=[b_qk])
                        add("dve", lambda e, t1=t1, eB=eB, cs=cs: e.tensor_tensor(out=t1, in0=eB, in1=cs[:, 0, :], op=ALU.mult),
                            reads=[b_eB, b_cs], writes=[b_t1])
                        add("dve", lambda e, t2=t2, eA=eA, cs=cs: e.tensor_tensor(out=t2, in0=eA, in1=cs[:, 1, :], op=ALU.mult),
                            reads=[b_eA, b_cs], writes=[b_t2])
                        add("dve", lambda e, t1=t1, t2=t2, oB=oB: e.tensor_tensor(out=oB, in0=t1, in1=t2, op=ALU.add),
                            reads=[b_t1, b_t2], writes=[b_qk])
                vcount = [0]
                for r in range(d):
                    def tsl(jj, n=1, r=r):
                        st = r + d * 128 * jj
                        return slice(st, st + d * (128 * n - 1) + 1, d) if d > 1 else slice(st, st + 128 * n)
                    items = []

                    def emit_pv(j, items=items, tsl=tsl):
                        (vv, b_vv, px, b_px) = items[j]
                        (bk3, b_bk3) = psum()
                        for hh in range(4):
                            o_ap = bk3[0:65, hh * 128:(hh + 1) * 128]
                            if j > 0:
                                (pvv, b_pvv, ppx, b_ppx) = items[j - 1]
                                add("pe", lambda e, o_ap=o_ap, pvv=pvv, ppx=ppx, hh=hh: e.matmul(
                                    o_ap, lhsT=pvv[:, hh, :], rhs=ppx[:, hh, 128:256], start=True, stop=False),
                                    reads=[b_pvv, b_ppx], writes=[b_bk3])
                            add("pe", lambda e, o_ap=o_ap, vv=vv, px=px, hh=hh, first=(j == 0): e.matmul(
                                o_ap, lhsT=vv[:, hh, :], rhs=px[:, hh, 0:128], start=first, stop=True),
                                reads=[b_vv, b_px], writes=[b_bk3])
                        a_ap = acc[0:65, :, tsl(j)]
                        p_ap = bk3[0:65, :].rearrange("p (h q) -> p h q", h=4)
                        if g == 0:
                            add("dve", lambda e, a_ap=a_ap, p_ap=p_ap: e.tensor_copy(out=a_ap, in_=p_ap), reads=[b_bk3], writes=[b_acc])
                        else:
                            add("dve", lambda e, a_ap=a_ap, p_ap=p_ap: e.tensor_tensor(out=a_ap, in0=p_ap, in1=a_ap, op=ALU.add),
                                reads=[b_bk3, b_acc], writes=[b_acc])

                    for j in range(nb):
                        (vv, b_vv) = v_rot[vcount[0] % 4]
                        vcount[0] += 1
                        (bk, b_bk) = psum()
                        for kc in range(KC):
                            add("pe", lambda e, bk=bk, kc=kc, wg=wg, ts=tsl(j): e.matmul(
                                bk[:, 0:256], lhsT=xnT[:, kc, ts], rhs=wg[:, kc, 512:768],
                                start=(kc == 0), stop=(kc == KC - 1)), reads=[b_wg] + b_xnT, writes=[b_bk])
                        add("act", lambda e, vv=vv, bk=bk: e.copy(out=vv[:, :, 0:64], in_=bk[:, 0:256].rearrange("p (h d) -> p h d", h=4)),
                            reads=[b_bk], writes=[b_vv])
                        nq = 2 if j < nb - 1 else 1
                        (px, b_px) = pe_rot.next()
                        for hh in range(4):
                            (bk2, b_bk2) = psum()
                            for part in range(2):
                                add("pe", lambda e, bk2=bk2, hh=hh, part=part, ks=tsl(j), qs=tsl(j, nq), nq=nq: e.matmul(
                                    bk2[:, 0:128 * nq], lhsT=qk[32 * hh:32 * hh + 32, 2 + part, ks],
                                    rhs=qk[32 * hh:32 * hh + 32, part, qs], start=(part == 0), stop=(part == 1),
                                    tile_position=(32 * hh, 0)), reads=[b_qk], writes=[b_bk2])
                            add("act", lambda e, px=px, bk2=bk2, hh=hh, nq=nq: e.activation(
                                out=px[:, hh, 0:128 * nq], in_=bk2[:, 0:128 * nq], func=AF.Exp, scale=0.125),
                                reads=[b_bk2], writes=[b_px])
                        add("dve", lambda e, px=px, nq=nq: e.tensor_tensor(
                            out=px[:, :, 0:128 * nq], in0=px[:, :, 0:128 * nq],
                            in1=mask4[:, :, 0:128 * nq], op=ALU.mult),
                            reads=[b_px, b_mask4], writes=[b_px])
                        items.append((vv, b_vv, px, b_px))
                        if j >= 2:
                            emit_pv(j - 2)
                    if nb >= 2:
                        emit_pv(nb - 2)
                    emit_pv(nb - 1)
            A.release(m_b)
            yaT, b_yaT = A.alloc("yaT", [4, S], BF16, top=True)
            m_n = A.mark()
            rd_rot = Rot("rden", [512], F32, 2)
            for k in range(NBK):
                blk = slice(k * 512, (k + 1) * 512)
                for hh in range(4):
                    (bk, b_bk) = psum()
                    add("pe", lambda e, bk=bk, hh=hh, blk=blk: e.matmul(bk[0:64, :], lhsT=sel_f[0:65, :], rhs=acc[0:65, hh, blk],
                                                                     start=True, stop=True), reads=[b_sel, b_acc], writes=[b_bk])
                    (rd, b_rd) = rd_rot.next()
                    add("dve", lambda e, rd=rd, bk=bk: e.reciprocal(out=rd[0:64, :], in_=bk[0:64, :]), reads=[b_bk], writes=[b_rd])
                    add("dve", lambda e, rd=rd, hh=hh, blk=blk: e.tensor_tensor(out=yaT[0:64, hh, blk], in0=acc[0:64, hh, blk],
                                                                             in1=rd[0:64, :], op=ALU.mult),
                        reads=[b_acc, b_rd], writes=[b_yaT])
            if dev:
                (dd, b_dd) = A.alloc("dbgya", [4, S], F32)
                add("dve", lambda e, dd=dd: e.tensor_copy(out=dd[0:64], in_=yaT[0:64]), reads=[b_yaT], writes=[b_dd])
                dma("sp", dbg["ya"][b], dd[0:64], [b_dd], [B_OUT])
            A.release(m_n)
            A.release(persist_mark)

            ybT, b_ybT = A.alloc("ybT", [4, S], BF16, top=True)
            m_c = A.mark()
            xs_rot = Rot("xs", [KC, 512], BF16, 2)
            wuv, b_wuv = A.alloc("wuv", [KC, 1024], BF16)
            dma("pool", wuv, w_in.rearrange("(kc p) n -> p kc n", p=128)[:, :, 2304:3328], [], [b_wuv])
            uT_rot = Rot("uT", [4, 512], BF16, 2)
            gtmp = Rot("gtmp", [512], F32, 8)
            vg_rot = Rot("vg", [512], F32, 4)
            vh_rot = Rot("vh", [512], BF16, 4)
            st_rot = Rot("lnst", [4], F32, 8)
            ljunk = A.alloc("ljunk", [512], BF16)
            yt_rot = Rot("ytmp", [512], F32, 4)
            for k in range(NBK):
                blk = slice(k * 512, (k + 1) * 512)
                (uT, b_uT) = uT_rot.next()
                (xs, b_xs) = xs_rot.next()
                dma("sp", xs, XN[b, :, :, blk], [B_XN[k]], [b_xs])
                def c_uchunk(c, xs=xs, b_xs=b_xs, uT=uT, b_uT=b_uT):
                    (bk, b_bk) = psum()
                    for kc in range(KC):
                        add("pe", lambda e, bk=bk, kc=kc, c=c, xs=xs: e.matmul(
                            bk[:, :], lhsT=wuv[:, kc, c * 128:(c + 1) * 128], rhs=xs[:, kc, :],
                            start=(kc == 0), stop=(kc == KC - 1)), reads=[b_wuv, b_xs], writes=[b_bk])
                    gelu_from_psum(bk, b_bk, uT[:, c, :], b_uT, gtmp, 512)

                def c_tile(t, k=k, xs=xs, b_xs=b_xs, uT=uT, b_uT=b_uT):
                    tok = slice(k * 512 + t * 128, k * 512 + (t + 1) * 128)
                    (bk, b_bk) = psum()
                    for kc in range(KC):
                        add("pe", lambda e, bk=bk, kc=kc, t=t, xs=xs: e.matmul(
                            bk[:, :], lhsT=xs[:, kc, t * 128:(t + 1) * 128], rhs=wuv[:, kc, 512:1024],
                            start=(kc == 0), stop=(kc == KC - 1)), reads=[b_wuv, b_xs], writes=[b_bk])
                    (vg, b_vg) = vg_rot.next()
                    gelu_from_psum(bk, b_bk, vg, b_vg, gtmp, 512)
                    (st, b_st) = st_rot.next()
                    (lj, b_lj) = ljunk
                    add("pool", lambda e, st=st: e.memset(st, 0.0), writes=[b_st])
                    add("act", lambda e, st=st, vg=vg, lj=lj: e.activation(out=lj, in_=vg, func=AF.Copy, accum_out=st[:, 0:1]),
                        reads=[b_vg], writes=[b_lj, b_st])
                    add("act", lambda e, st=st, vg=vg, lj=lj: e.activation(out=lj, in_=vg, func=AF.Square, accum_out=st[:, 1:2]),
                        reads=[b_vg], writes=[b_lj, b_st])
                    add("dve", lambda e, st=st: e.tensor_scalar(out=st[:, 0:2], in0=st[:, 0:2], scalar1=1.0 / 512, scalar2=None, op0=ALU.mult),
                        reads=[b_st], writes=[b_st])
                    add("dve", lambda e, st=st: e.tensor_tensor(out=st[:, 2:3], in0=st[:, 0:1], in1=st[:, 0:1], op=ALU.mult),
                        reads=[b_st], writes=[b_st])
                    add("dve", lambda e, st=st: e.tensor_tensor(out=st[:, 1:2], in0=st[:, 1:2], in1=st[:, 2:3], op=ALU.subtract),
                        reads=[b_st], writes=[b_st])
                    add("act", lambda e, st=st: e.activation(out=st[:, 1:2], in_=st[:, 1:2], func=AF.Sqrt, bias=eps_ln[:, 0:1], scale=1.0),
                        reads=[b_st, b_epsln], writes=[b_st])
                    add("dve", lambda e, st=st: e.reciprocal(out=st[:, 1:2], in_=st[:, 1:2]), reads=[b_st], writes=[b_st])
                    add("dve", lambda e, st=st, vg=vg: e.tensor_scalar(out=vg, in0=vg, scalar1=st[:, 0:1], scalar2=st[:, 1:2],
                                                                     op0=ALU.subtract, op1=ALU.mult), reads=[b_vg, b_st], writes=[b_vg])
                    (vh, b_vh) = vh_rot.next()
                    add("dve", lambda e, vh=vh, vg=vg: e.tensor_tensor(out=vh, in0=vg, in1=gamrow, op=ALU.mult),
                        reads=[b_vg, b_gamrow], writes=[b_vh])
                    (bk2, b_bk2) = psum()
                    for g in range(4):
                        add("pe", lambda e, bk2=bk2, g=g, vh=vh: e.matmul(
                            bk2[:, g * 128:(g + 1) * 128], lhsT=vh[:, g * 128:(g + 1) * 128], rhs=wsT[:, g, :],
                            start=True, stop=True), reads=[b_vh, b_wsT], writes=[b_bk2])
                    (yt, b_yt) = yt_rot.next()
                    add("dve", lambda e, yt=yt, bk2=bk2: e.tensor_tensor(out=yt, in0=bk2[:, :], in1=B2.rearrange("p g t -> p (g t)"), op=ALU.add),
                        reads=[b_bk2, b_B2], writes=[b_yt])
                    add("dve", lambda e, yt=yt, uT=uT, t=t, tok=tok: e.tensor_tensor(
                        out=ybT[:, :, tok], in0=yt.rearrange("p (g t) -> p g t", g=4), in1=uT[:, :, t * 128:(t + 1) * 128], op=ALU.mult),
                        reads=[b_yt, b_uT], writes=[b_ybT])

                emit_interleaved([record(lambda c=c: c_uchunk(c)) for c in range(4)])
                emit_interleaved([record(lambda t=t: c_tile(t)) for t in (0, 1)])
                emit_interleaved([record(lambda t=t: c_tile(t)) for t in (2, 3)])
            if dev:
                (dd, b_dd) = A.alloc("dbgyb", [4, S], F32)
                add("dve", lambda e, dd=dd: e.tensor_copy(out=dd, in_=ybT), reads=[b_ybT], writes=[b_dd])
                dma("sp", dbg["yb"][b], dd, [b_dd], [B_OUT])
            A.release(m_c)

            m_d = A.mark()
            wgt, b_wgt = A.alloc("wgt", [KC, 2048], BF16)
            woa, b_woa = A.alloc("woa", [4, D], BF16)
            wob, b_wob = A.alloc("wob", [4, D], BF16)
            wo, b_wo = A.alloc("wo", [KC, D], BF16)
            dma("pool", wgt, w_in.rearrange("(kc p) n -> p kc n", p=128)[:, :, 3328:5376], [], [b_wgt])
            dma("pool", woa[0:64], w_oa.rearrange("(h p) n -> p h n", p=64), [], [b_woa])
            dma("pool", wob, w_ob.rearrange("(g p) n -> p g n", p=128), [], [b_wob])
            dma("pool", wo, w_o.rearrange("(kc p) n -> p kc n", p=128), [], [b_wo])
            mg_rot = Rot("merged", [KC, 512], BF16, 2)
            sg_rot = Rot("sg", [512], F32, 4)
            mm_rot = Rot("mm", [512], F32, 4)
            xr_rot = Rot("xr", [D], F32, 2)
            xs_rot = Rot("xsd", [KC, 512], BF16, 2)
            for k in range(NBK):
                blk = slice(k * 512, (k + 1) * 512)
                (mg, b_mg) = mg_rot.next()
                (xs, b_xs) = xs_rot.next()
                dma("sp", xs, XN[b, :, :, blk], [B_XN[k]], [b_xs])
                for c in range(KC):
                    (bka, b_bka) = psum()
                    (bkb, b_bkb) = psum()
                    (bkA, b_bkA) = psum()
                    (bkB, b_bkB) = psum()
                    for kc in range(KC):
                        add("pe", lambda e, bka=bka, kc=kc, c=c, xs=xs: e.matmul(
                            bka[:, :], lhsT=wgt[:, kc, c * 128:(c + 1) * 128], rhs=xs[:, kc, :],
                            start=(kc == 0), stop=(kc == KC - 1)), reads=[b_wgt, b_xs], writes=[b_bka])
                    for kc in range(KC):
                        add("pe", lambda e, bkb=bkb, kc=kc, c=c, xs=xs: e.matmul(
                            bkb[:, :], lhsT=wgt[:, kc, 1024 + c * 128:1024 + (c + 1) * 128], rhs=xs[:, kc, :],
                            start=(kc == 0), stop=(kc == KC - 1)), reads=[b_wgt, b_xs], writes=[b_bkb])
                    for hh in range(4):
                        add("pe", lambda e, bkA=bkA, hh=hh, c=c, blk=blk: e.matmul(
                            bkA[:, :], lhsT=woa[0:64, hh, c * 128:(c + 1) * 128], rhs=yaT[0:64, hh, blk],
                            start=(hh == 0), stop=(hh == 3)), reads=[b_woa, b_yaT], writes=[b_bkA])
                    for g in range(4):
                        add("pe", lambda e, bkB=bkB, g=g, c=c, blk=blk: e.matmul(
                            bkB[:, :], lhsT=wob[:, g, c * 128:(c + 1) * 128], rhs=ybT[:, g, blk],
                            start=(g == 0), stop=(g == 3)), reads=[b_wob, b_ybT], writes=[b_bkB])
                    (sa, b_sa) = sg_rot.next()
                    (sb_, b_sb) = sg_rot.next()
                    add("act", lambda e, sa=sa, bka=bka, c=c: e.activation(out=sa, in_=bka[:, :], func=AF.Sigmoid, bias=bgt_t[:, c:c + 1], scale=1.0),
                        reads=[b_bka, b_bgt], writes=[b_sa])
                    add("act", lambda e, sb_=sb_, bkb=bkb, c=c: e.activation(out=sb_, in_=bkb[:, :], func=AF.Sigmoid, bias=bgt_t[:, 8 + c:9 + c], scale=1.0),
                        reads=[b_bkb, b_bgt], writes=[b_sb])
                    (m1, b_m1) = mm_rot.next()
                    (m2, b_m2) = mm_rot.next()
                    add("dve", lambda e, m1=m1, sa=sa, bkA=bkA: e.tensor_tensor(out=m1, in0=bkA[:, :], in1=sa, op=ALU.mult),
                        reads=[b_bkA, b_sa], writes=[b_m1])
                    add("dve", lambda e, m2=m2, sb_=sb_, bkB=bkB: e.tensor_tensor(out=m2, in0=bkB[:, :], in1=sb_, op=ALU.mult),
                        reads=[b_bkB, b_sb], writes=[b_m2])
                    add("dve", lambda e, m1=m1, m2=m2, mg=mg, c=c: e.tensor_tensor(out=mg[:, c, :], in0=m1, in1=m2, op=ALU.add),
                        reads=[b_m1, b_m2], writes=[b_mg])
                for t in range(4):
                    ti = k * 4 + t
                    gi = b * NT + ti
                    (xr, b_xr) = xr_rot.next()
                    (h1t, b_h1t) = (xr, b_xr)
                    dma("sp", xr, x[b, ti * 128:(ti + 1) * 128, :], [], [b_xr])
                    for n in range(2):
                        (bk, b_bk) = psum()
                        for c in range(KC):
                            add("pe", lambda e, bk=bk, c=c, t=t, n=n, mg=mg: e.matmul(
                                bk[:, :], lhsT=mg[:, c, t * 128:(t + 1) * 128], rhs=wo[:, c, n * 512:(n + 1) * 512],
                                start=(c == 0), stop=(c == KC - 1)), reads=[b_mg, b_wo], writes=[b_bk])
                        add("dve", lambda e, bk=bk, xr=xr, h1t=h1t, n=n: e.tensor_tensor(
                            out=h1t[:, n * 512:(n + 1) * 512], in0=bk[:, :], in1=xr[:, n * 512:(n + 1) * 512], op=ALU.add),
                            reads=[b_bk, b_xr], writes=[b_h1t])
                    dma("sp", H[gi * 128:(gi + 1) * 128, :], h1t, [b_h1t], [B_H[gi]])
                    if dev:
                        dma("sp", dbg["h1"][gi * 128:(gi + 1) * 128, :], h1t, [b_h1t], [B_OUT])
            A.release(m_d)
            A.release(persist_mark)
            A.release_top()

            m_e = A.mark()
            kT, b_kT = A.alloc("kT", [KC, NMEM], BF16)
            vm, b_vm = A.alloc("vm", [2, D], BF16)
            wq, b_wq = A.alloc("wq", [KC, D], BF16)
            wox, b_wox = A.alloc("wox", [KC, D], BF16)
            dma("pool", wq, w_qx.rearrange("(kc p) n -> p kc n", p=128), [], [b_wq])
            dma("pool", wox, w_ox.rearrange("(kc p) n -> p kc n", p=128), [], [b_wox])
            m_e0 = A.mark()
            wkv, b_wkv = A.alloc("wkv", [KC, 2 * D], BF16)
            dma("pool", wkv, w_kvx.rearrange("(kc p) n -> p kc n", p=128), [], [b_wkv])
            gmem_t, b_gmem = A.alloc("gmem", [D], F32)
            dma("sp", gmem_t, g_mem.partition_broadcast(128), [], [b_gmem])
            mnT, b_mnT = A.alloc("mnT", [KC, NMEM], BF16)
            mt_rot = Rot("mt", [D], F32, 2)
            mb_rot = Rot("mb", [D], BF16, 2)
            small = Rot("ssm", [2], F32, 4)
            junk = A.alloc("junkm", [D], BF16)
            for i in range(2):
                (mt, b_mt) = mt_rot.next()
                (mb, b_mb) = mb_rot.next()
                dma("sp", mt, mem[b, i * 128:(i + 1) * 128, :], [], [b_mt])
                rms_tile(mt, b_mt, gmem_t, b_gmem, mb, b_mb, small, junk)
                transpose_to(mb, b_mb, mnT[:, :, i * 128:(i + 1) * 128], b_mnT)
            for oc in range(KC):
                (bk, b_bk) = psum()
                for kc in range(KC):
                    add("pe", lambda e, bk=bk, kc=kc, oc=oc: e.matmul(
                        bk[:, 0:NMEM], lhsT=wkv[:, kc, oc * 128:(oc + 1) * 128], rhs=mnT[:, kc, :],
                        start=(kc == 0), stop=(kc == KC - 1)), reads=[b_wkv, b_mnT], writes=[b_bk])
                add("act", lambda e, bk=bk, oc=oc: e.copy(out=kT[:, oc, :], in_=bk[:, 0:NMEM]), reads=[b_bk], writes=[b_kT])
            for mchunk in range(2):
                for n in range(2):
                    (bk, b_bk) = psum()
                    for kc in range(KC):
                        add("pe", lambda e, bk=bk, kc=kc, mchunk=mchunk, n=n: e.matmul(
                            bk[:, :], lhsT=mnT[:, kc, mchunk * 128:(mchunk + 1) * 128], rhs=wkv[:, kc, D + n * 512:D + (n + 1) * 512],
                            start=(kc == 0), stop=(kc == KC - 1)), reads=[b_wkv, b_mnT], writes=[b_bk])
                    add("act", lambda e, bk=bk, mchunk=mchunk, n=n: e.copy(out=vm[:, mchunk, n * 512:(n + 1) * 512], in_=bk[:, :]),
                        reads=[b_bk], writes=[b_vm])
            A.release(m_e0)

            gx_t, b_gx = A.alloc("gx", [D], F32)
            gmoe_t, b_gmoe = A.alloc("gmoe", [D], F32)
            dma("sp", gx_t, g_x.partition_broadcast(128), [], [b_gx])
            dma("sp", gmoe_t, g_moe.partition_broadcast(128), [], [b_gmoe])
            h_rot = Rot("ht", [D], F32, 8)
            hb_rot = Rot("hb", [D], BF16, 4)
            small = Rot("sse", [2], F32, 8)
            junk = A.alloc("junke", [D], BF16)
            hnT_rot = Rot("hnT", [KC, 512], BF16, 2)
            qT_rot = Rot("qT", [KC, 512], BF16, 2)
            oT_rot = Rot("oT", [KC, 512], BF16, 2)
            px_rot = Rot("pxx", [2, 512], BF16, 3)
            rd_rot = Rot("rdx", [512], F32, 2)
            h2_rot = Rot("h2t", [D], F32, 2)
            hn3f_rot = Rot("hn3f", [D], F32, 2)
            hn3b_rot = Rot("hn3b", [D], BF16, 2)
            hn3T_rot = Rot("hn3T", [KC, 128], F32, 2)
            rs_rot = Rot("rsm", [16], F32, 4)
            ls_rot = Rot("ls", [36], F32, 2)
            el_rot = Rot("el", [32], F32, 2)
            mk_rot = Rot("mk", [3, 32], F32, 2)
            mkb_rot = Rot("mkb", [32], BF16, 2)
            t8_rot = Rot("t8", [8], F32, 2)
            rec_rot = Rot("rec", [2, 2], I32, 2)
            dsc_rot = Rot("dsc", [2], I32, 2)

            def e_stage1(k, b=b):
                (hnT, b_hnT) = hnT_rot.next()
                hts = []

                def e_tile(t):
                    gi = b * NT + k * 4 + t
                    (ht, b_ht) = h_rot.next()
                    (hb, b_hb) = hb_rot.next()
                    dma("sp", ht, H[gi * 128:(gi + 1) * 128, :], [B_H[gi]], [b_ht])
                    rms_tile(ht, b_ht, gx_t, b_gx, hb, b_hb, small, junk)
                    transpose_to(hb, b_hb, hnT[:, :, t * 128:(t + 1) * 128], b_hnT, eng="act" if t % 2 == 0 else "dve")
                    hts.append((ht, b_ht, gi))

                emit_interleaved([record(lambda t=t: e_tile(t)) for t in range(4)])
                (qT, b_qT) = qT_rot.next()
                for oc in range(KC):
                    (bk, b_bk) = psum()
                    for kc in range(KC):
                        add("pe", lambda e, bk=bk, kc=kc, oc=oc, hnT=hnT: e.matmul(
                            bk[:, :], lhsT=wq[:, kc, oc * 128:(oc + 1) * 128], rhs=hnT[:, kc, :],
                            start=(kc == 0), stop=(kc == KC - 1)), reads=[b_wq, b_hnT], writes=[b_bk])
                    if oc % 2 == 0:
                        add("act", lambda e, bk=bk, oc=oc, qT=qT: e.copy(out=qT[:, oc, :], in_=bk[:, :]), reads=[b_bk], writes=[b_qT])
                    else:
                        add("dve", lambda e, bk=bk, oc=oc, qT=qT: e.tensor_copy(out=qT[:, oc, :], in_=bk[:, :]), reads=[b_bk], writes=[b_qT])
                return (hts, qT, b_qT)

            def e_stage2(k, st1, b=b):
                (hts, qT, b_qT) = st1
                pending = []
                outer_add = cur_add[0]

                def flush_router():
                    n1 = max(len(p[0]) for p in pending)
                    for i_ in range(n1):
                        for p in pending:
                            if i_ < len(p[0]):
                                outer_add(*p[0][i_])
                    for p in pending:
                        for it in p[1]:
                            outer_add(*it)
                    pending.clear()
                (oT, b_oT) = oT_rot.next()
                pxs = {}

                def e_scores(h):
                    (px, b_px) = px_rot.next()
                    pxs[h] = (px, b_px)
                    for mchunk in range(2):
                        (bk, b_bk) = psum()
                        for cc in range(2):
                            add("pe", lambda e, bk=bk, cc=cc, h=h, mchunk=mchunk, qT=qT: e.matmul(
                                bk[:, :], lhsT=kT[:, 2 * h + cc, mchunk * 128:(mchunk + 1) * 128], rhs=qT[:, 2 * h + cc, :],
                                start=(cc == 0), stop=(cc == 1)), reads=[b_kT, b_qT], writes=[b_bk])
                        add("act", lambda e, bk=bk, px=px, mchunk=mchunk: e.activation(out=px[:, mchunk, :], in_=bk[:, :], func=AF.Exp, scale=1.0 / 16.0),
                            reads=[b_bk], writes=[b_px])

                def e_pv(h):
                    (px, b_px) = pxs[h]
                    (bkd, b_bkd) = psum()
                    for mchunk in range(2):
                        add("pe", lambda e, bkd=bkd, px=px, mchunk=mchunk: e.matmul(
                            bkd[:, :], lhsT=ones_b, rhs=px[:, mchunk, :], start=(mchunk == 0), stop=(mchunk == 1)),
                            reads=[b_ones, b_px], writes=[b_bkd])
                    (rd, b_rd) = rd_rot.next()
                    add("dve", lambda e, rd=rd, bkd=bkd: e.reciprocal(out=rd, in_=bkd[:, :]), reads=[b_bkd], writes=[b_rd])
                    for cc in range(2):
                        (bk, b_bk) = psum()
                        for mchunk in range(2):
                            add("pe", lambda e, bk=bk, px=px, mchunk=mchunk, h=h, cc=cc: e.matmul(
                                bk[:, :], lhsT=vm[:, mchunk, (2 * h + cc) * 128:(2 * h + cc + 1) * 128], rhs=px[:, mchunk, :],
                                start=(mchunk == 0), stop=(mchunk == 1)), reads=[b_vm, b_px], writes=[b_bk])
                        add("dve", lambda e, bk=bk, rd=rd, oT=oT, h=h, cc=cc: e.tensor_tensor(
                            out=oT[:, 2 * h + cc, :], in0=bk[:, :], in1=rd, op=ALU.mult), reads=[b_bk, b_rd], writes=[b_oT])

                e_scores(0)
                for h in range(4):
                    if h + 1 < 4:
                        e_scores(h + 1)
                    e_pv(h)
                for t in range(4):
                    (ht, b_ht, gi) = hts[t]
                    (h2t, b_h2t) = h2_rot.next()
                    for n in range(2):
                        (bk, b_bk) = psum()
                        for c in range(KC):
                            add("pe", lambda e, bk=bk, c=c, t=t, n=n, oT=oT: e.matmul(
                                bk[:, :], lhsT=oT[:, c, t * 128:(t + 1) * 128], rhs=wox[:, c, n * 512:(n + 1) * 512],
                                start=(c == 0), stop=(c == KC - 1)), reads=[b_oT, b_wox], writes=[b_bk])
                        add("dve", lambda e, bk=bk, ht=ht, h2t=h2t, n=n: e.tensor_tensor(
                            out=h2t[:, n * 512:(n + 1) * 512], in0=bk[:, :], in1=ht[:, n * 512:(n + 1) * 512], op=ALU.add),
                            reads=[b_bk, b_ht], writes=[b_h2t])
                    dma("sp", H[gi * 128:(gi + 1) * 128, :], h2t, [b_h2t], [B_H[gi]])
                    if dev:
                        dma("sp", dbg["h2"][gi * 128:(gi + 1) * 128, :], h2t, [b_h2t], [B_OUT])
                    lst1, lst2 = [], []
                    cur_add[0] = make_recorder(lst1)
                    (hn3f, b_hn3f) = hn3f_rot.next()
                    (hn3b, b_hn3b) = hn3b_rot.next()
                    rms_tile(h2t, b_h2t, gmoe_t, b_gmoe, hn3b, b_hn3b, small, junk, dst_f32=hn3f, b_dst32=b_hn3f)
                    dma("sp", HN3[gi * 128:(gi + 1) * 128, :], hn3b, [b_hn3b], [B_HN3])
                    (hn3T, b_hn3T) = hn3T_rot.next()
                    (bk, b_bk) = psum()
                    for half in range(2):
                        for c4 in range(4):
                            c = half * 4 + c4
                            add("pe", lambda e, bk=bk, c=c, c4=c4, hn3f=hn3f: e.transpose(
                                out=bk[:, c4 * 128:(c4 + 1) * 128], in_=hn3f[:, c * 128:(c + 1) * 128], identity=ident_f),
                                reads=[b_hn3f, b_ident_f], writes=[b_bk])
                        add("act", lambda e, bk=bk, half=half, hn3T=hn3T: e.copy(
                            out=hn3T[:, half * 4:(half + 1) * 4, :], in_=bk[:, :].rearrange("p (c t) -> p c t", c=4)),
                            reads=[b_bk], writes=[b_hn3T])
                    (bk, b_bk) = psum()
                    (bkL, b_bkL) = (bk, b_bk)
                    for c in range(KC):
                        add("pe", lambda e, bk=bk, c=c, hn3T=hn3T: e.matmul(
                            bk[:, 0:36], lhsT=hn3T[:, c, :], rhs=wr_t[:, c, :], start=(c == 0), stop=(c == KC - 1)),
                            reads=[b_hn3T, b_wr], writes=[b_bk])
                    (ls, b_ls) = ls_rot.next()
                    (rs, b_rs) = rs_rot.next()
                    (el, b_el) = el_rot.next()
                    (mk, b_mk) = mk_rot.next()
                    (mkb, b_mkb) = mkb_rot.next()
                    (t8, b_t8) = t8_rot.next()
                    (rec, b_rec) = rec_rot.next()
                    add("dve", lambda e, ls=ls, bk=bk: e.tensor_tensor(out=ls, in0=bk[:, 0:36], in1=br_t, op=ALU.add),
                        reads=[b_bk, b_br], writes=[b_ls])
                    add("dve", lambda e, ls=ls, rs=rs: e.reduce_max(out=rs[:, 0:1], in_=ls[:, 0:4], axis=AX.X), reads=[b_ls], writes=[b_rs])
                    add("dve", lambda e, rs=rs: e.tensor_scalar(out=rs[:, 1:2], in0=rs[:, 0:1], scalar1=-1.0, scalar2=None, op0=ALU.mult),
                        reads=[b_rs], writes=[b_rs])
                    add("pool", lambda e, rs=rs: e.memset(rs[:, 2:3], 0.0), writes=[b_rs])
                    add("act", lambda e, ls=ls, rs=rs, mk=mk: e.activation(out=mk[:, 2, 0:4], in_=ls[:, 0:4], func=AF.Exp, bias=rs[:, 1:2], scale=1.0,
                                                                          accum_out=rs[:, 2:3]), reads=[b_ls, b_rs], writes=[b_mk, b_rs])
                    add("dve", lambda e, rs=rs: e.reciprocal(out=rs[:, 3:4], in_=rs[:, 2:3]), reads=[b_rs], writes=[b_rs])
                    add("dve", lambda e, ls=ls, rs=rs, mk=mk: e.tensor_scalar(out=mk[:, 2, 8:12], in0=ls[:, 0:4], scalar1=rs[:, 0:1], scalar2=None, op0=ALU.is_ge),
                        reads=[b_ls, b_rs], writes=[b_mk])
                    add("dve", lambda e, mk=mk: e.tensor_scalar(out=mk[:, 2, 8:12], in0=mk[:, 2, 8:12], scalar1=1e30, scalar2=-1e30, op0=ALU.mult, op1=ALU.add),
                        reads=[b_mk], writes=[b_mk])
                    add("dve", lambda e, ls=ls, el=el, mk=mk: e.tensor_tensor(
                        out=el.rearrange("p (g x) -> p g x", g=4), in0=ls[:, 4:36].rearrange("p (g x) -> p g x", g=4),
                        in1=mk[:, 2, 8:12].unsqueeze(2).to_broadcast([128, 4, 8]), op=ALU.add), reads=[b_ls, b_mk], writes=[b_el])
                    add("dve", lambda e, el=el, t8=t8: e.max(out=t8, in_=el), reads=[b_el], writes=[b_t8])
                    add("dve", lambda e, el=el, t8=t8, mk=mk: e.tensor_scalar(out=mk[:, 0, :], in0=el, scalar1=t8[:, 0:1], scalar2=None, op0=ALU.is_ge),
                        reads=[b_el, b_t8], writes=[b_mk])
                    add("dve", lambda e, el=el, t8=t8, mk=mk: e.tensor_scalar(out=mk[:, 1, :], in0=el, scalar1=t8[:, 1:2], scalar2=None, op0=ALU.is_ge),
                        reads=[b_el, b_t8], writes=[b_mk])
                    add("dve", lambda e, mk=mk, mkb=mkb: e.tensor_copy(out=mkb, in_=mk[:, 1, :]), reads=[b_mk], writes=[b_mkb])
                    add("dve", lambda e, mk=mk: e.tensor_tensor(out=mk[:, 1, :], in0=mk[:, 1, :], in1=mk[:, 0, :], op=ALU.subtract),
                        reads=[b_mk], writes=[b_mk])
                    add("dve", lambda e, t8=t8, rs=rs: e.tensor_tensor(out=rs[:, 4:5], in0=t8[:, 0:1], in1=t8[:, 1:2], op=ALU.subtract),
                        reads=[b_t8], writes=[b_rs])
                    add("act", lambda e, rs=rs: e.activation(out=rs[:, 5:6], in_=rs[:, 4:5], func=AF.Exp, scale=-1.0), reads=[b_rs], writes=[b_rs])
                    add("dve", lambda e, rs=rs: e.tensor_scalar(out=rs[:, 5:6], in0=rs[:, 5:6], scalar1=1.0, scalar2=None, op0=ALU.add), reads=[b_rs], writes=[b_rs])
                    add("dve", lambda e, rs=rs: e.reciprocal(out=rs[:, 5:6], in_=rs[:, 5:6]), reads=[b_rs], writes=[b_rs])
                    add("dve", lambda e, rs=rs: e.tensor_tensor(out=rs[:, 6:7], in0=rs[:, 5:6], in1=rs[:, 3:4], op=ALU.mult), reads=[b_rs], writes=[b_rs])
                    add("dve", lambda e, rs=rs: e.tensor_tensor(out=rs[:, 7:8], in0=rs[:, 3:4], in1=rs[:, 6:7], op=ALU.subtract), reads=[b_rs], writes=[b_rs])
                    (bkp, b_bkp) = (bkL, b_bkL)
                    add("pe", lambda e, bkp=bkp, mkb=mkb: e.matmul(bkp[:, 64:96], lhsT=ltri_b, rhs=mkb, start=True, stop=True),
                        reads=[b_ltri, b_mkb], writes=[b_bkp])
                    add("pe", lambda e, bkp=bkp, mkb=mkb: e.matmul(bkp[:, 96:128], lhsT=ones_b, rhs=mkb, start=True, stop=True),
                        reads=[b_ones, b_mkb], writes=[b_bkp])
                    cur_add[0] = make_recorder(lst2)
                    add("dve", lambda e, bkp=bkp, mk=mk: e.tensor_tensor(out=mk[:, 2, :], in0=bkp[:, 64:96], in1=base_t, op=ALU.add),
                        reads=[b_bkp, b_base], writes=[b_mk])
                    add("dve", lambda e, bkp=bkp: e.tensor_tensor(out=base_t, in0=bkp[:, 96:128], in1=base_t, op=ALU.add),
                        reads=[b_bkp, b_base], writes=[b_base])
                    add("dve", lambda e, el=el, mk=mk: e.tensor_scalar(out=el, in0=mk[:, 2, :], scalar1=float(C), scalar2=1e6, op0=ALU.is_ge, op1=ALU.mult),
                        reads=[b_mk], writes=[b_el])
                    add("dve", lambda e, el=el, mk=mk: e.tensor_tensor(out=mk[:, 2, :], in0=mk[:, 2, :], in1=el, op=ALU.add), reads=[b_mk, b_el], writes=[b_mk])
                    add("dve", lambda e, mk=mk: e.tensor_tensor(out=mk[:, 2, :], in0=mk[:, 2, :], in1=ec_t, op=ALU.add), reads=[b_mk, b_ec], writes=[b_mk])
                    for sl in range(2):
                        add("dve", lambda e, mk=mk, el=el, sl=sl: e.tensor_tensor(out=el, in0=mk[:, sl, :], in1=mk[:, 2, :], op=ALU.mult),
                            reads=[b_mk], writes=[b_el])
                        add("dve", lambda e, el=el, rs=rs, sl=sl: e.reduce_sum(out=rs[:, 8 + sl:9 + sl], in_=el, axis=AX.X), reads=[b_el], writes=[b_rs])
                    add("dve", lambda e, rs=rs: e.tensor_scalar_min(out=rs[:, 8:10], in0=rs[:, 8:10], scalar1=float(NSLOT)), reads=[b_rs], writes=[b_rs])
                    add("dve", lambda e, rs=rs, gi=gi: e.tensor_copy(out=dest_all[:, gi, :], in_=rs[:, 8:10]), reads=[b_rs], writes=[b_dest])
                    for sl in range(2):
                        add("dve", lambda e, rec=rec, sl=sl, rs=rs: e.tensor_copy(out=rec[:, sl, 1:2].bitcast(F32), in_=rs[:, 6 + sl:7 + sl]),
                            reads=[b_rs], writes=[b_rec])
                        add("dve", lambda e, rec=rec, sl=sl, gi=gi: e.tensor_scalar(out=rec[:, sl, 0:1], in0=iota_f[:, 0:1], scalar1=float(gi * 128),
                                                                                    scalar2=None, op0=ALU.add), reads=[b_iota], writes=[b_rec])
                    for sl in range(2):
                        add("pool", lambda e, rec=rec, sl=sl, gi=gi: e.indirect_dma_start(
                            out=SLOT, out_offset=bass.IndirectOffsetOnAxis(ap=dest_all[:, gi, sl:sl + 1], axis=0),
                            in_=rec[:, sl, :], in_offset=None, bounds_check=PR["bc_slot"], oob_is_err=False),
                            reads=[b_rec, b_dest], writes=[B_SLOT], dma=True)
                    cur_add[0] = outer_add
                    pending.append((lst1, lst2))
                    if len(pending) == 1:
                        flush_router()
            stbox = []

            def run_s1(k_):
                psum_pool[0] = "a"
                stbox.append(e_stage1(k_))
                psum_pool[0] = None

            def run_s2(k_, st_):
                psum_pool[0] = "b"
                e_stage2(k_, st_)
                psum_pool[0] = None

            run_s1(0)
            for k in range(NBK):
                st_cur = stbox[k]
                la = record(lambda: run_s1(k + 1)) if k + 1 < NBK else []
                lb = record(lambda: run_s2(k, st_cur))
                emit_merged(la, lb)
            A.release(m_e)

        if dev:
            dma("sp", dbg["dest"], dest_all, [b_dest], [B_OUT])

        dma("sp", cnt_out, base_t, [b_base], [B_OUT])
        A.release(persist_mark)
        m_f = A.mark()
        wge_rot = Rot("wge", [KC, FF], BF16, 3)
        wue_rot = Rot("wue", [KC, FF], BF16, 3)
        wde_rot = Rot("wde", [4, D], BF16, 3)
        srec_rot = Rot("srec", [2], I32, 2 * CT)
        xe_rot = Rot("xe", [D], BF16, 4)
        xeT_rot = Rot("xeT", [KC, C], BF16, 2)
        aT_rot = Rot("aT", [4, C], BF16, 2)
        sl_rot = Rot("silu", [512], F32, 3)
        ys_rot = Rot("ysb", [D], F32, 3)
        segs = [(s0, min(512, C - s0)) for s0 in range(0, C, 512)]

        def f_load(ex):
            (wge, b_wge) = wge_rot.next()
            (wue, b_wue) = wue_rot.next()
            (wde, b_wde) = wde_rot.next()
            dma("pool", wge, w_ge[ex].rearrange("(kc p) f -> p kc f", p=128), [], [b_wge])
            dma("pool", wue, w_ue[ex].rearrange("(kc p) f -> p kc f", p=128), [], [b_wue])
            dma("pool", wde, w_de[ex].rearrange("(f p) n -> p f n", p=128), [], [b_wde])
            (xeT, b_xeT) = xeT_rot.next()
            srecs = []
            for j in range(CT):
                (srec, b_srec) = srec_rot.next()
                (xe, b_xe) = xe_rot.next()
                row0 = ex * C + j * 128
                dma("sp", srec, SLOT[row0:row0 + 128, :], [B_SLOT], [b_srec])
                add("pool", lambda e, xe=xe, srec=srec: e.indirect_dma_start(
                    out=xe, out_offset=None, in_=HN3, in_offset=bass.IndirectOffsetOnAxis(ap=srec[:, 0:1], axis=0),
                    bounds_check=PR["bc_tok"], oob_is_err=False),
                    reads=[b_srec, B_HN3], writes=[b_xe], dma=True)
                transpose_to(xe, b_xe, xeT[:, :, j * 128:(j + 1) * 128], b_xeT, eng="act" if j % 2 == 0 else "dve")
                srecs.append((srec, b_srec))
            return (wge, b_wge, wue, b_wue, wde, b_wde, xeT, b_xeT, srecs)

        def f_gateup(st):
            (wge, b_wge, wue, b_wue, wde, b_wde, xeT, b_xeT, srecs) = st
            (aT, b_aT) = aT_rot.next()
            for f in range(4):
                for (s0, sn) in segs:
                    (bkg, b_bkg) = psum()
                    (bku, b_bku) = psum()
                    for kc in range(KC):
                        add("pe", lambda e, bkg=bkg, kc=kc, f=f, s0=s0, sn=sn, wge=wge, xeT=xeT: e.matmul(
                            bkg[:, 0:sn], lhsT=wge[:, kc, f * 128:(f + 1) * 128], rhs=xeT[:, kc, s0:s0 + sn],
                            start=(kc == 0), stop=(kc == KC - 1)), reads=[b_wge, b_xeT], writes=[b_bkg])
                    for kc in range(KC):
                        add("pe", lambda e, bku=bku, kc=kc, f=f, s0=s0, sn=sn, wue=wue, xeT=xeT: e.matmul(
                            bku[:, 0:sn], lhsT=wue[:, kc, f * 128:(f + 1) * 128], rhs=xeT[:, kc, s0:s0 + sn],
                            start=(kc == 0), stop=(kc == KC - 1)), reads=[b_wue, b_xeT], writes=[b_bku])
                    (sl_, b_sl) = sl_rot.next()
                    add("act", lambda e, sl_=sl_, bkg=bkg, sn=sn: e.activation(out=sl_[:, 0:sn], in_=bkg[:, 0:sn], func=AF.Silu),
                        reads=[b_bkg], writes=[b_sl])
                    add("dve", lambda e, sl_=sl_, bku=bku, aT=aT, f=f, s0=s0, sn=sn: e.tensor_tensor(
                        out=aT[:, f, s0:s0 + sn], in0=bku[:, 0:sn], in1=sl_[:, 0:sn], op=ALU.mult), reads=[b_bku, b_sl], writes=[b_aT])
            return (aT, b_aT)

        def f_down(ex, st, aTb):
            (wge, b_wge, wue, b_wue, wde, b_wde, xeT, b_xeT, srecs) = st
            (aT, b_aT) = aTb
            for j in range(CT):
                (srec, b_srec) = srecs[j]
                (ysb, b_ysb) = ys_rot.next()
                row0 = ex * C + j * 128
                for n in range(2):
                    (bk, b_bk) = psum()
                    for f in range(4):
                        add("pe", lambda e, bk=bk, f=f, j=j, n=n, aT=aT, wde=wde: e.matmul(
                            bk[:, :], lhsT=aT[:, f, j * 128:(j + 1) * 128], rhs=wde[:, f, n * 512:(n + 1) * 512],
                            start=(f == 0), stop=(f == 3)), reads=[b_aT, b_wde], writes=[b_bk])
                    if n == 0:
                        add("act", lambda e, bk=bk, ysb=ysb, n=n, srec=srec: e.activation(
                            out=ysb[:, n * 512:(n + 1) * 512], in_=bk[:, :], func=AF.Copy, scale=srec[:, 1:2].bitcast(F32)),
                            reads=[b_bk, b_srec], writes=[b_ysb])
                    else:
                        add("dve", lambda e, bk=bk, ysb=ysb, n=n, srec=srec: e.tensor_scalar(
                            out=ysb[:, n * 512:(n + 1) * 512], in0=bk[:, :], scalar1=srec[:, 1:2].bitcast(F32), scalar2=None, op0=ALU.mult),
                            reads=[b_bk, b_srec], writes=[b_ysb])
                dma("sp", YS[row0:row0 + 128, :], ysb, [b_ysb], [B_YS[ex]])

        st_next = f_load(0)
        for ex in range(NEXP):
            st_cur = st_next
            aTb = f_gateup(st_cur)
            if ex + 1 < NEXP:
                st_next = f_load(ex + 1)
            f_down(ex, st_cur, aTb)
        A.release(m_f)

        m_g = A.mark()
        hg_rot = Rot("hg", [D], F32, 3)
        y_rot = Rot("yg", [2, D], F32, 3)
        og_rot = Rot("og", [D], F32, 2)
        small = Rot("ssg", [2], F32, 4)
        junk = A.alloc("junkg", [D], BF16)
        gfin_t, b_gfin = A.alloc("gfin", [D], F32)
        dma("sp", gfin_t, g_fin.partition_broadcast(128), [], [b_gfin])
        for gi in range(NTT):
            (hg, b_hg) = hg_rot.next()
            (yg, b_yg) = y_rot.next()
            (og, b_og) = og_rot.next()
            dma("sp", hg, H[gi * 128:(gi + 1) * 128, :], [B_H[gi]], [b_hg])
            for sl in range(2):
                add("pool", lambda e, yg=yg, sl=sl, gi=gi: e.indirect_dma_start(
                    out=yg[:, sl, :], out_offset=None, in_=YS, in_offset=bass.IndirectOffsetOnAxis(ap=dest_all[:, gi, sl:sl + 1], axis=0),
                    bounds_check=PR["bc_slot"], oob_is_err=False),
                    reads=[b_dest] + B_YS, writes=[b_yg], dma=True)
            add("dve", lambda e, hg=hg, yg=yg: e.tensor_tensor(out=hg, in0=hg, in1=yg[:, 0, :], op=ALU.add), reads=[b_hg, b_yg], writes=[b_hg])
            add("dve", lambda e, hg=hg, yg=yg: e.tensor_tensor(out=hg, in0=hg, in1=yg[:, 1, :], op=ALU.add), reads=[b_hg, b_yg], writes=[b_hg])
            (ss, b_ss) = small.next()
            (jk, b_jk) = junk
            add("pool", lambda e, ss=ss: e.memset(ss, 0.0), writes=[b_ss])
            add("act", lambda e, ss=ss, hg=hg, jk=jk: e.activation(out=jk, in_=hg, func=AF.Square, accum_out=ss[:, 0:1]),
                reads=[b_hg], writes=[b_jk, b_ss])
            add("act", lambda e, ss=ss: e.activation(out=ss[:, 1:2], in_=ss[:, 0:1], func=AF.Ln, bias=eps_rms[:, 0:1], scale=1.0 / D),
                reads=[b_ss, b_eps], writes=[b_ss])
            add("act", lambda e, ss=ss: e.activation(out=ss[:, 0:1], in_=ss[:, 1:2], func=AF.Exp, scale=-0.5), reads=[b_ss], writes=[b_ss])
            add("dve", lambda e, ss=ss, hg=hg, og=og: e.scalar_tensor_tensor(out=og, in0=hg, scalar=ss[:, 0:1], in1=gfin_t, op0=ALU.mult, op1=ALU.mult),
                reads=[b_hg, b_ss, b_gfin], writes=[b_og])
            dma("sp", out[gi * 128:(gi + 1) * 128, :], og, [b_og], [B_OUT])
        A.release(m_g)
        if dev:
            dsl, b_dsl = A.alloc("dsl", [NSLOT // 128, 2], I32)
            dma("sp", dsl, SLOT[0:NSLOT, :].rearrange("(p a) c -> p a c", p=128), [B_SLOT], [b_dsl])
            dma("sp", dbg["slot"].rearrange("(p a) c -> p a c", p=128), dsl, [b_dsl], [B_OUT])
            dy_rot = Rot("dy", [D], F32, 2)
            dh_rot = Rot("dh", [D], BF16, 2)
            for i_ in range((NSLOT + 128) // 128):
                (dy, b_dy) = dy_rot.next()
                dma("sp", dy, YS[i_ * 128:(i_ + 1) * 128, :], B_YS, [b_dy])
                dma("sp", dbg["ys"][i_ * 128:(i_ + 1) * 128, :], dy, [b_dy], [B_OUT])
            for i_ in range((TOK + 128) // 128):
                (dh, b_dh) = dh_rot.next()
                dma("sp", dh, HN3[i_ * 128:(i_ + 1) * 128, :], [B_HN3], [b_dh])
                dma("sp", dbg["hn3"][i_ * 128:(i_ + 1) * 128, :], dh, [b_dh], [B_OUT])

        Sc.emit(block, esems, dsems)
        build.stats = dict(nops=Sc.nops, peak=A.peak)
    return nc


def _consts(C):
    p = np.arange(128)
    half = 32
    inv_freq = (10000.0 ** (-np.arange(half, dtype=np.float32) / half)).astype(np.float32)
    c_invf = inv_freq[p % 32].reshape(128, 1).astype(np.float32)
    kk = np.arange(128)[:, None]
    qq = np.arange(128)[None, :]
    c_mask = np.concatenate([(kk <= qq), (kk >= qq)], axis=1).astype(np.float32)
    c_ltri = (kk < qq).astype(np.float32)
    c_ident = np.eye(128, dtype=np.float32)
    c_ec = np.broadcast_to((np.arange(32, dtype=np.float32) * C)[None, :], (128, 32)).copy()
    c_sel = np.zeros((65, 64), np.float32)
    c_sel[64, :] = 1.0
    c_iota = np.arange(128, dtype=np.float32).reshape(128, 1)
    return dict(c_invf=c_invf, c_mask=c_mask, c_ltri=c_ltri, c_ident=c_ident, c_ec=c_ec, c_sel=c_sel, c_iota=c_iota)


def _w_in_perm():
    cols = []
    for g in range(3):
        heads = range(4 * g, 4 * g + 4)
        for base in (0, 768):
            for part in (0, 32):
                for h in heads:
                    cols.extend(range(base + 64 * h + part, base + 64 * h + part + 32))
        cols.extend(range(1536 + 256 * g, 1536 + 256 * (g + 1)))
    cols.extend(range(2304, 5376))
    return np.asarray(cols)


def prep_weights(inp, C):
    f = lambda a: np.ascontiguousarray(np.asarray(a, dtype=np.float32))
    w = {}
    w["w_in"] = f(np.asarray(inp["w_in"])[0][:, _w_in_perm()])
    w["g_mix"] = f(inp["mix_norm_g"][0])
    w["g_x"] = f(inp["xattn_norm_g"][0])
    w["g_mem"] = f(inp["mem_norm_g"][0])
    w["g_moe"] = f(inp["moe_norm_g"][0])
    w["g_fin"] = f(inp["final_norm_g"])
    w["bgt"] = f(np.asarray(inp["b_gates"])[0].reshape(16, 128).T)
    w["ws_t"] = f(np.asarray(inp["w_spatial"])[0].transpose(0, 2, 1))
    w["bsp"] = f(np.asarray(inp["b_spatial"])[0].reshape(512))
    w["vgam"] = f(inp["v_norm_g"][0])
    w["vbet_t"] = f(np.asarray(inp["v_norm_b"])[0].reshape(4, 128).T)
    w["w_oa"] = f(inp["w_out_a"][0])
    w["w_ob"] = f(inp["w_out_b"][0])
    w["w_o"] = f(inp["w_out"][0])
    w["w_qx"] = f(inp["w_q_x"][0])
    w["w_kvx"] = f(inp["w_kv_x"][0])
    w["w_ox"] = f(inp["w_o_x"][0])
    w["w_r"] = f(np.concatenate([np.asarray(inp["w_router_grp"])[0], np.asarray(inp["w_router_exp"])[0]], axis=1))
    w["b_r"] = f(np.concatenate([np.asarray(inp["b_router_grp"])[0], np.asarray(inp["b_router_exp"])[0]], axis=0))
    w["w_ge"] = f(inp["w_gate_e"][0])
    w["w_ue"] = f(inp["w_up_e"][0])
    w["w_de"] = f(inp["w_down_e"][0])
    w.update(_consts(C))
    return w


def run(inp, n_cores, NB, S, C, dev=False):
    nc = build(NB, S, C, dev=dev)
    w = prep_weights(inp, C)
    x = np.asarray(inp["x"], dtype=np.float32)
    mem = np.asarray(inp["mem"], dtype=np.float32)
    pos = np.asarray(inp["positions"], dtype=np.int32)
    in_maps = []
    for c in range(n_cores):
        m = dict(w)
        m["x"] = np.ascontiguousarray(x[c * NB:(c + 1) * NB])
        m["mem"] = np.ascontiguousarray(mem[c * NB:(c + 1) * NB])
        m["pos"] = np.ascontiguousarray(pos[c * NB:(c + 1) * NB])
        in_maps.append(m)
    res = run_bass_kernel_spmd(nc, in_maps, core_ids=list(range(n_cores)))
    outs = [r["out"].reshape(NB, S, D) for r in res.results]
    try:
        print("[kernel] max routed rows per (core, expert):", [int(r["cnt"][0].max()) for r in res.results], "capacity", C, flush=True)
    except Exception:
        pass
    full = np.concatenate(outs, axis=0).astype(np.float32)
    if dev:
        return full, res.results
    return full


def kernel(**inputs):
    return run(inputs, n_cores=8, NB=2, S=4096, C=1024)
```

```python
import math
from contextlib import ExitStack

import numpy as np
import concourse.bass as bass
import concourse.mybir as mybir
from concourse.bass_utils import run_bass_kernel_spmd

F32 = mybir.dt.float32
BF16 = mybir.dt.bfloat16
I32 = mybir.dt.int32
ALU = mybir.AluOpType
AF = mybir.ActivationFunctionType
AX = mybir.AxisListType

D = 1024
KC = 8
NMEM = 256
NEXP = 32
FF = 512
RMS_EPS = 1e-6
LN_EPS = 1e-5
TWO_PI = 2.0 * math.pi
CW1 = 6.28125
CW2 = TWO_PI - CW1
PI_SAFE = 3.1415925

ENGS = ("pe", "act", "dve", "pool", "sp")


class Buf:
    __slots__ = ("name", "w", "r")

    def __init__(self, name):
        self.name = name
        self.w = None
        self.r = []


class Op:
    __slots__ = ("eng", "fn", "dma", "deps", "signal", "count", "sem", "semval", "prev_on_sem")

    def __init__(self, eng, fn, dma):
        self.eng = eng
        self.fn = fn
        self.dma = dma
        self.deps = []
        self.signal = False
        self.count = 0
        self.sem = None
        self.semval = 0
        self.prev_on_sem = None


class Sched:
    def __init__(self, n_dma_sems=40):
        self.ops = {e: [] for e in ENGS}
        self.n_dma_sems = n_dma_sems
        self.dma_rr = 0
        self.dma_last = [None] * n_dma_sems
        self.dma_val = [0] * n_dma_sems
        self.nops = 0
        self.pool_regs = {}
        self.pool_reg_handles = {}

    def add(self, eng, fn, reads=(), writes=(), dma=False):
        op = Op(eng, fn, dma)
        self.nops += 1
        deps = {}
        for b in reads:
            if b.w is not None:
                deps[id(b.w)] = (b.w, "raw")
        for b in writes:
            if b.w is not None and id(b.w) not in deps:
                deps[id(b.w)] = (b.w, "waw")
            for r in b.r:
                if id(r) not in deps:
                    deps[id(r)] = (r, "war")
        for b in reads:
            b.r.append(op)
        for b in writes:
            b.w = op
            b.r = []
        for d, kind in deps.values():
            if d is op:
                continue
            if d.dma:
                op.deps.append(d)
            elif d.eng != eng:
                op.deps.append(d)
                d.signal = True
            elif kind == "raw" and eng != "pe":
                op.deps.append(d)
                d.signal = True
        if dma:
            s = self.dma_rr
            self.dma_rr = (self.dma_rr + 1) % self.n_dma_sems
            op.sem = s
            op.prev_on_sem = self.dma_last[s]
            self.dma_val[s] += 16
            op.semval = self.dma_val[s]
            self.dma_last[s] = op
        self.ops[eng].append(op)
        return op

    def emit(self, block, esems, dsems):
        for e in ENGS:
            c = 0
            for op in self.ops[e]:
                if not op.dma and op.signal:
                    c += 1
                    op.count = c
        sched = self

        def run(e, eng):
            seen = {}
            for op in sched.ops[e]:
                best = {}
                if op.dma and op.prev_on_sem is not None:
                    p = op.prev_on_sem
                    best[("d", p.sem)] = p.semval
                for d in op.deps:
                    k = ("d", d.sem) if d.dma else ("e", d.eng)
                    v = d.semval if d.dma else d.count
                    if v > best.get(k, 0):
                        best[k] = v
                for k, v in best.items():
                    if seen.get(k, 0) >= v:
                        continue
                    seen[k] = v
                    eng.wait_ge(dsems[k[1]] if k[0] == "d" else esems[k[1]], v)
                ins = op.fn(eng)
                if op.dma:
                    ins.then_inc(dsems[op.sem], 16)
                elif op.signal:
                    ins.then_inc(esems[e], 1)

        @block.tensor
        def _(eng):
            run("pe", eng)

        @block.scalar
        def _(eng):
            run("act", eng)

        @block.vector
        def _(eng):
            run("dve", eng)

        @block.gpsimd
        def _(eng):
            for name_, val_ in sched.pool_regs.items():
                r_ = eng.alloc_register(name_)
                eng.reg_mov(r_, val_)
                sched.pool_reg_handles[name_] = r_
            run("pool", eng)

        @block.sync
        def _(eng):
            run("sp", eng)
            for s_ in range(sched.n_dma_sems):
                if sched.dma_val[s_] > 0:
                    eng.wait_ge(dsems[s_], sched.dma_val[s_])


class Arena:
    def __init__(self, base_ap, nbytes):
        self.base = base_ap
        self.cap = nbytes
        self.top = 0
        self.hi = nbytes
        self.live = []
        self.freed = []
        self.peak = 0

    def alloc(self, name, free_shape, dtype, nbufs=1, parts=128, top=False):
        esz = 2 if dtype == BF16 else 4
        n = int(np.prod(free_shape))
        nbytes = (n * esz + 63) // 64 * 64
        if top:
            end = self.hi
            start = end - nbytes
            assert start >= self.top, f"SBUF arena overflow (top) allocating {name}"
            self.hi = start
        else:
            start = self.top
            end = start + nbytes
            assert end <= self.hi, f"SBUF arena overflow allocating {name}: {end} > {self.hi}"
            self.top = end
        self.peak = max(self.peak, self.top + (self.cap - self.hi))
        v = self.base[0:parts, start // 2:start // 2 + n * esz // 2]
        if dtype != BF16:
            v = v.bitcast(dtype)
        if len(free_shape) == 2:
            v = v.rearrange("p (a b) -> p a b", a=free_shape[0])
        elif len(free_shape) == 3:
            v = v.rearrange("p (a b c) -> p a b c", a=free_shape[0], b=free_shape[1])
        hz = []
        for (s, e, ops) in self.freed:
            if s < end and e > start:
                hz.extend(ops)
        bufs = []
        for i in range(nbufs):
            b = Buf(f"{name}{i}")
            b.r = list(hz)
            bufs.append(b)
        self.live.append((start, end, bufs))
        return (v, bufs[0]) if nbufs == 1 else (v, bufs)

    def mark(self):
        return self.top

    def release_top(self):
        keep = []
        for (s, e, bufs) in self.live:
            if s >= self.hi:
                ops = []
                for b in bufs:
                    if b.w is not None:
                        ops.append(b.w)
                    ops.extend(b.r)
                self.freed.append((s, e, ops))
            else:
                keep.append((s, e, bufs))
        self.live = keep
        self.hi = self.cap

    def release(self, mark):
        keep = []
        for (s, e, bufs) in self.live:
            if s >= mark and e <= self.hi:
                ops = []
                for b in bufs:
                    if b.w is not None:
                        ops.append(b.w)
                    ops.extend(b.r)
                self.freed.append((s, e, ops))
            else:
                keep.append((s, e, bufs))
        self.live = keep
        self.top = mark


def build(NB, S, C, dev=False):
    NT = S // 128
    NBK = S // 512
    TOK = NB * S
    NTT = TOK // 128
    CT = C // 128
    NSLOT = NEXP * C
    DIL = (1, 4, 16)

    nc = bass.Bass("TRN2", target_bir_lowering=False)

    def din(name, shape, dt=F32):
        return nc.dram_tensor(name, shape, dt, kind="ExternalInput").ap()

    x = din("x", [NB, S, D])
    mem = din("mem", [NB, NMEM, D])
    pos = din("pos", [NB, S], I32)
    w_in = din("w_in", [D, 5376])
    g_mix = din("g_mix", [D])
    g_x = din("g_x", [D])
    g_mem = din("g_mem", [D])
    g_moe = din("g_moe", [D])
    g_fin = din("g_fin", [D])
    bgt = din("bgt", [128, 16])
    ws_t = din("ws_t", [4, 128, 128])
    bsp = din("bsp", [512])
    vgam = din("vgam", [512])
    vbet_t = din("vbet_t", [128, 4])
    w_oa = din("w_oa", [256, D])
    w_ob = din("w_ob", [512, D])
    w_o = din("w_o", [D, D])
    w_qx = din("w_qx", [D, D])
    w_kvx = din("w_kvx", [D, 2 * D])
    w_ox = din("w_ox", [D, D])
    w_r = din("w_r", [D, 36])
    b_r = din("b_r", [36])
    w_ge = din("w_ge", [NEXP, D, FF])
    w_ue = din("w_ue", [NEXP, D, FF])
    w_de = din("w_de", [NEXP, FF, D])
    c_invf = din("c_invf", [128, 1])
    c_mask = din("c_mask", [128, 256])
    c_ltri = din("c_ltri", [128, 128])
    c_ident = din("c_ident", [128, 128])
    c_ec = din("c_ec", [128, 32])
    c_sel = din("c_sel", [65, 64])
    c_iota = din("c_iota", [128, 1])

    out = nc.dram_tensor("out", [TOK, D], F32, kind="ExternalOutput").ap()
    cnt_out = nc.dram_tensor("cnt", [128, 32], F32, kind="ExternalOutput").ap()
    dbg = {}
    if dev:
        dbg["ya"] = nc.dram_tensor("dbg_ya", [NB, 64, 4, S], F32, kind="ExternalOutput").ap()
        dbg["yb"] = nc.dram_tensor("dbg_yb", [NB, 128, 4, S], F32, kind="ExternalOutput").ap()
        dbg["h1"] = nc.dram_tensor("dbg_h1", [TOK, D], F32, kind="ExternalOutput").ap()
        dbg["h2"] = nc.dram_tensor("dbg_h2", [TOK, D], F32, kind="ExternalOutput").ap()
        dbg["dest"] = nc.dram_tensor("dbg_dest", [128, NTT, 2], I32, kind="ExternalOutput").ap()
        dbg["slot"] = nc.dram_tensor("dbg_slot", [NSLOT, 2], I32, kind="ExternalOutput").ap()
        dbg["ys"] = nc.dram_tensor("dbg_ys", [NSLOT + 128, D], F32, kind="ExternalOutput").ap()
        dbg["hn3"] = nc.dram_tensor("dbg_hn3", [TOK + 128, D], BF16, kind="ExternalOutput").ap()

    COS = nc.dram_tensor("scr_cos", [NB, 128, S], F32, kind="Internal").ap()
    SIN = nc.dram_tensor("scr_sin", [NB, 128, S], F32, kind="Internal").ap()
    H = nc.dram_tensor("scr_h", [TOK, D], F32, kind="Internal").ap()
    XN = nc.dram_tensor("scr_xn", [NB, 128, KC, S], BF16, kind="Internal").ap()
    HN3 = nc.dram_tensor("scr_hn3", [TOK + 128, D], BF16, kind="Internal").ap()
    SLOT = nc.dram_tensor("scr_slot", [NSLOT + 128, 2], I32, kind="Internal").ap()
    YS = nc.dram_tensor("scr_ys", [NSLOT + 128, D], F32, kind="Internal").ap()

    ARENA_BYTES = 204 * 1024
    es = ExitStack()
    with es:
        arena_t = es.enter_context(nc.sbuf_tensor("arena", [128, ARENA_BYTES // 2], BF16))
        banks = [es.enter_context(nc.psum_tensor(f"bank{i}", [128, 512], F32)) for i in range(8)]
        esems = {e: es.enter_context(nc.semaphore("es_" + e)) for e in ENGS}
        NDS = 40
        dsems = [es.enter_context(nc.semaphore(f"ds{i}")) for i in range(NDS)]
        block = es.enter_context(nc.Block())
        Sc = Sched(NDS)
        Sc.pool_regs = {"bc_slot": NSLOT + 127, "bc_tok": TOK + 127}
        PR = Sc.pool_reg_handles
        A = Arena(arena_t[:, :], ARENA_BYTES)
        real_add = Sc.add
        cur_add = [Sc.add]

        def add(*a_, **k_):
            return cur_add[0](*a_, **k_)

        def record(fn_):
            lst = []
            prev_ = cur_add[0]
            cur_add[0] = make_recorder(lst)
            fn_()
            cur_add[0] = prev_
            return lst

        def emit_interleaved(lists):
            n_ = max(len(l_) for l_ in lists)
            for i_ in range(n_):
                for l_ in lists:
                    if i_ < len(l_):
                        cur_add[0](*l_[i_])

        def emit_merged(la, lb):
            na, nb_ = len(la), len(lb)
            ia = 0
            for ib in range(nb_):
                cur_add[0](*lb[ib])
                want = ((ib + 1) * na) // max(nb_, 1)
                while ia < want:
                    cur_add[0](*la[ia])
                    ia += 1
            while ia < na:
                cur_add[0](*la[ia])
                ia += 1

        def make_recorder(lst):
            def rec_(eng, fn, reads=(), writes=(), dma=False):
                lst.append((eng, fn, tuple(reads), tuple(writes), dma))
            return rec_

        bank_bufs = [Buf(f"bank{i}") for i in range(8)]
        bank_rr = [0]

        psum_pool = [None]
        pool_rr = {"a": 0, "b": 0}
        pool_banks = {"a": [0, 1, 2, 3], "b": [4, 5, 6, 7]}

        def psum():
            if psum_pool[0] is not None:
                pl = pool_banks[psum_pool[0]]
                i = pl[pool_rr[psum_pool[0]] % len(pl)]
                pool_rr[psum_pool[0]] += 1
                return banks[i], bank_bufs[i]
            i = bank_rr[0]
            bank_rr[0] = (i + 1) % 8
            return banks[i], bank_bufs[i]

        B_COS = [Buf(f"cos{b}") for b in range(NB)]
        B_H = [Buf(f"H{i}") for i in range(NTT)]
        B_HN3 = Buf("HN3")
        B_XN = [Buf(f"XN{k}") for k in range(NBK)]
        B_SLOT = Buf("SLOT")
        B_YS = [Buf(f"YS{e}") for e in range(NEXP)]
        B_OUT = Buf("OUT")

        class Rot:
            def __init__(self, name, free_shape, dtype, n, parts=128):
                self.items = [A.alloc(f"{name}{i}", free_shape, dtype, parts=parts) for i in range(n)]
                self.i = 0

            def next(self):
                it = self.items[self.i]
                self.i = (self.i + 1) % len(self.items)
                return it

        def dma(eng, out_ap, in_ap, reads, writes):
            return add(eng, lambda e: e.dma_start(out=out_ap, in_=in_ap), reads=reads, writes=writes, dma=True)

        ident_b, b_ident_b = A.alloc("ident_b", [128], BF16)
        ident_f, b_ident_f = A.alloc("ident_f", [128], F32)
        ones_b, b_ones = A.alloc("ones_b", [128], BF16)
        mask_b, b_mask = A.alloc("mask_b", [256], BF16)
        mask4, b_mask4 = A.alloc("mask4", [4, 256], BF16)
        ltri_b, b_ltri = A.alloc("ltri_b", [128], BF16)
        invf, b_invf = A.alloc("invf", [1], F32)
        ec_t, b_ec = A.alloc("ec", [32], F32)
        sel_f, b_sel = A.alloc("sel", [64], BF16)
        iota_f, b_iota = A.alloc("iota", [1], F32)
        bgt_t, b_bgt = A.alloc("bgt", [16], F32)
        wr_t, b_wr = A.alloc("wr", [KC, 36], F32)
        br_t, b_br = A.alloc("br", [36], F32)
        base_t, b_base = A.alloc("base", [32], F32)
        dest_all, b_dest = A.alloc("dest_all", [NTT, 2], I32)
        eps_rms, b_eps = A.alloc("eps_rms", [1], F32)
        eps_ln, b_epsln = A.alloc("eps_ln", [1], F32)

        dma("pool", ident_b, c_ident, [], [b_ident_b])
        dma("sp", ident_f, c_ident, [], [b_ident_f])
        dma("pool", mask_b, c_mask, [], [b_mask])
        dma("pool", ltri_b, c_ltri, [], [b_ltri])
        dma("sp", invf, c_invf, [], [b_invf])
        dma("sp", ec_t, c_ec, [], [b_ec])
        dma("pool", sel_f[0:65, :], c_sel, [], [b_sel])
        dma("sp", iota_f, c_iota, [], [b_iota])
        dma("sp", bgt_t, bgt, [], [b_bgt])
        dma("sp", wr_t, w_r.rearrange("(kc p) n -> p kc n", p=128), [], [b_wr])
        dma("sp", br_t, b_r.partition_broadcast(128), [], [b_br])
        add("pool", lambda e: e.memset(ones_b, 1.0), writes=[b_ones])
        for hh_ in range(4):
            add("pool", lambda e, hh_=hh_: e.tensor_copy(out=mask4[:, hh_, :], in_=mask_b), reads=[b_mask], writes=[b_mask4])
        add("pool", lambda e: e.memset(base_t, 0.0), writes=[b_base])
        add("pool", lambda e: e.memset(eps_rms, RMS_EPS), writes=[b_eps])
        add("pool", lambda e: e.memset(eps_ln, LN_EPS), writes=[b_epsln])

        m0 = A.mark()
        zrow, b_zrow = A.alloc("zrow", [D], BF16)
        slot0, b_slot0 = A.alloc("slot0", [NSLOT // 128, 2], I32)
        add("pool", lambda e: e.memset(zrow, 0.0), writes=[b_zrow])
        dma("sp", HN3[TOK:TOK + 128, :], zrow, [b_zrow], [B_HN3])
        zrowf, b_zrowf = A.alloc("zrowf", [D], F32)
        add("pool", lambda e: e.memset(zrowf, 0.0), writes=[b_zrowf])
        dma("sp", YS[NSLOT:NSLOT + 128, :], zrowf, [b_zrowf], [B_YS[0]])
        add("pool", lambda e: e.memset(slot0[:, :, 0:1], TOK), writes=[b_slot0])
        add("pool", lambda e: e.memset(slot0[:, :, 1:2], 0), writes=[b_slot0])
        dma("sp", SLOT[0:NSLOT, :].rearrange("(p a) c -> p a c", p=128), slot0, [b_slot0], [B_SLOT])
        A.release(m0)

        def rms_tile(src_ap, b_src, g_tile, b_g, dst_bf, b_dst, small, junk, dst_f32=None, b_dst32=None):
            (ss, b_ss) = small.next()
            (jk, b_jk) = junk
            add("pool", lambda e: e.memset(ss, 0.0), writes=[b_ss])
            add("act", lambda e: e.activation(out=jk, in_=src_ap, func=AF.Square, accum_out=ss[:, 0:1]),
                reads=[b_src], writes=[b_jk, b_ss])
            add("act", lambda e: e.activation(out=ss[:, 1:2], in_=ss[:, 0:1], func=AF.Ln, bias=eps_rms[:, 0:1],
                                              scale=1.0 / D), reads=[b_ss, b_eps], writes=[b_ss])
            add("act", lambda e: e.activation(out=ss[:, 0:1], in_=ss[:, 1:2], func=AF.Exp, scale=-0.5), reads=[b_ss], writes=[b_ss])
            if dst_f32 is not None:
                add("dve", lambda e: e.scalar_tensor_tensor(out=dst_f32, in0=src_ap, scalar=ss[:, 0:1], in1=g_tile,
                                                            op0=ALU.mult, op1=ALU.mult),
                    reads=[b_src, b_ss, b_g], writes=[b_dst32])
                add("act", lambda e: e.copy(out=dst_bf, in_=dst_f32), reads=[b_dst32], writes=[b_dst])
            else:
                add("dve", lambda e: e.scalar_tensor_tensor(out=dst_bf, in0=src_ap, scalar=ss[:, 0:1], in1=g_tile,
                                                            op0=ALU.mult, op1=ALU.mult),
                    reads=[b_src, b_ss, b_g], writes=[b_dst])

        def transpose_to(src_bf, b_src, dst_ap3, b_dst, eng="act"):
            (bk, b_bk) = psum()
            pv = bk[:, :].bitcast(BF16)
            for c in range(KC):
                add("pe", lambda e, c=c: e.transpose(out=pv[:, c * 128:(c + 1) * 128], in_=src_bf[:, c * 128:(c + 1) * 128],
                                                     identity=ident_b), reads=[b_src, b_ident_b], writes=[b_bk])
            pv3 = pv.rearrange("p (c t) -> p c t", c=KC)
            if eng == "act":
                add("act", lambda e: e.copy(out=dst_ap3, in_=pv3), reads=[b_bk], writes=[b_dst])
            else:
                add("dve", lambda e: e.tensor_copy(out=dst_ap3, in_=pv3), reads=[b_bk], writes=[b_dst])

        def gelu_from_psum(bk, b_bk, dst, b_dst, tmp_rot, width):
            (t1, b_t1) = tmp_rot.next()
            (t2, b_t2) = tmp_rot.next()
            pz = bk[:, 0:width]
            add("act", lambda e: e.activation(out=t1[:, 0:width], in_=pz, func=AF.Square), reads=[b_bk], writes=[b_t1])
            add("dve", lambda e: e.tensor_scalar(out=t1[:, 0:width], in0=t1[:, 0:width], scalar1=0.044715, scalar2=1.0,
                                                 op0=ALU.mult, op1=ALU.add), reads=[b_t1], writes=[b_t1])
            add("dve", lambda e: e.tensor_tensor(out=t2[:, 0:width], in0=pz, in1=t1[:, 0:width], op=ALU.mult),
                reads=[b_bk, b_t1], writes=[b_t2])
            add("act", lambda e: e.activation(out=t2[:, 0:width], in_=t2[:, 0:width], func=AF.Sigmoid,
                                              scale=1.5957691216057308), reads=[b_t2], writes=[b_t2])
            add("dve", lambda e: e.tensor_tensor(out=dst, in0=pz, in1=t2[:, 0:width], op=ALU.mult),
                reads=[b_bk, b_t2], writes=[b_dst])

        wsT, b_wsT = A.alloc("wsT", [4, 128], BF16)
        B2, b_B2 = A.alloc("B2", [4, 128], F32)
        gamrow, b_gamrow = A.alloc("gamrow", [512], F32)
        m0 = A.mark()
        bspb, b_bspb = A.alloc("bspb", [4, 128], F32)
        bet_t, b_bet = A.alloc("bet", [4], F32)
        dma("pool", wsT, ws_t.rearrange("g s t -> s g t"), [], [b_wsT])
        dma("sp", bspb, bsp.partition_broadcast(128).rearrange("p (g t) -> p g t", g=4), [], [b_bspb])
        dma("sp", bet_t, vbet_t, [], [b_bet])
        dma("sp", gamrow, vgam.partition_broadcast(128), [], [b_gamrow])
        add("dve", lambda e: e.tensor_tensor(out=wsT, in0=wsT, in1=mask_b[:, 0:128].unsqueeze(1).to_broadcast([128, 4, 128]),
                                             op=ALU.mult), reads=[b_wsT, b_mask], writes=[b_wsT])
        (bk, b_bk) = psum()
        add("pe", lambda e: e.matmul(bk[:, :], lhsT=ones_b, rhs=wsT.rearrange("p g t -> p (g t)"), start=True, stop=True),
            reads=[b_ones, b_wsT], writes=[b_bk])
        for g in range(4):
            add("dve", lambda e, g=g: e.scalar_tensor_tensor(out=B2[:, g, :], in0=bk[:, g * 128:(g + 1) * 128],
                                                             scalar=bet_t[:, g:g + 1], in1=bspb[:, g, :],
                                                             op0=ALU.mult, op1=ALU.add),
                reads=[b_bk, b_bet, b_bspb], writes=[b_B2])
        A.release(m0)

        persist_mark = A.mark()

        for b in range(NB):
            m_r = A.mark()
            posi, b_posi = A.alloc("posi", [S], I32)
            dma("sp", posi, pos[b].partition_broadcast(128), [], [b_posi])
            rt = Rot("rt", [512], F32, 8)
            ri = Rot("ri", [512], I32, 8)
            ang_rot = Rot("ang", [512], F32, 4)
            tb_rot = Rot("tb", [512], F32, 8)
            def r_block(k, b=b):
                blk = slice(k * 512, (k + 1) * 512)
                (ang, b_ang) = ang_rot.next()
                add("dve", lambda e, ang=ang, blk=blk: e.tensor_copy(out=ang, in_=posi[:, blk]), reads=[b_posi], writes=[b_ang])
                add("dve", lambda e, ang=ang: e.tensor_scalar(out=ang, in0=ang, scalar1=invf[:, 0:1], scalar2=None, op0=ALU.mult),
                    reads=[b_ang, b_invf], writes=[b_ang])
                for (dst, shift) in ((SIN, 0.0), (COS, 0.5 * math.pi)):
                    (u, b_u) = rt.next()
                    (ki, b_ki) = ri.next()
                    (tb, b_tb) = tb_rot.next()
                    add("dve", lambda e, u=u, ang=ang, shift=shift: e.tensor_scalar(
                        out=u, in0=ang, scalar1=shift, scalar2=1.0 / TWO_PI, op0=ALU.add, op1=ALU.mult),
                        reads=[b_ang], writes=[b_u])
                    add("dve", lambda e, u=u, ki=ki: e.tensor_copy(out=ki, in_=u), reads=[b_u], writes=[b_ki])
                    add("dve", lambda e, u=u, ki=ki: e.tensor_copy(out=u, in_=ki), reads=[b_ki], writes=[b_u])
                    add("dve", lambda e, u=u, ang=ang, tb=tb: e.scalar_tensor_tensor(
                        out=tb, in0=u, scalar=-CW1, in1=ang, op0=ALU.mult, op1=ALU.add), reads=[b_u, b_ang], writes=[b_tb])
                    add("dve", lambda e, u=u, tb=tb: e.scalar_tensor_tensor(
                        out=tb, in0=u, scalar=-CW2, in1=tb, op0=ALU.mult, op1=ALU.add), reads=[b_u, b_tb], writes=[b_tb])
                    add("dve", lambda e, tb=tb, shift=shift: e.tensor_scalar(
                        out=tb, in0=tb, scalar1=shift, scalar2=PI_SAFE, op0=ALU.add, op1=ALU.min), reads=[b_tb], writes=[b_tb])
                    add("dve", lambda e, tb=tb: e.tensor_scalar_max(out=tb, in0=tb, scalar1=-PI_SAFE), reads=[b_tb], writes=[b_tb])
                    add("act", lambda e, tb=tb: e.activation(out=tb, in_=tb, func=AF.Sin), reads=[b_tb], writes=[b_tb])
                    dma("sp", dst[b, :, blk], tb, [b_tb], [B_COS[b]])
            for k0 in range(0, NBK, 4):
                emit_interleaved([record(lambda k=k: r_block(k)) for k in range(k0, min(NBK, k0 + 4))])
            A.release(m_r)

        for b in range(NB):
            A.release(persist_mark)

            xnT, b_xnT = A.alloc("xnT", [KC, S], BF16, nbufs=NBK)
            m_a = A.mark()
            xt_rot = Rot("xt", [D], F32, 4)
            xnb_rot = Rot("xnb", [D], BF16, 4)
            small = Rot("ss", [2], F32, 8)
            junk = A.alloc("junk", [D], BF16)
            gmix_t, b_gmix = A.alloc("gmix", [D], F32)
            dma("sp", gmix_t, g_mix.partition_broadcast(128), [], [b_gmix])
            def a_tile(i, b=b):
                (xt, b_xt) = xt_rot.next()
                (xnb, b_xnb) = xnb_rot.next()
                dma("sp", xt, x[b, i * 128:(i + 1) * 128, :], [], [b_xt])
                rms_tile(xt, b_xt, gmix_t, b_gmix, xnb, b_xnb, small, junk)
                transpose_to(xnb, b_xnb, xnT[:, :, i * 128:(i + 1) * 128], b_xnT[i // 4], eng="act" if i % 2 == 0 else "dve")

            for kk in range(NBK):
                emit_interleaved([record(lambda i=i: a_tile(i)) for i in range(kk * 4, kk * 4 + 4)])
                dma("sp", XN[b, :, :, kk * 512:(kk + 1) * 512], xnT[:, :, kk * 512:(kk + 1) * 512], [b_xnT[kk]], [B_XN[kk]])
            A.release(m_a)

            acc, b_acc = A.alloc("acc", [4, S], BF16, parts=128)
            m_b = A.mark()
            qk, b_qk = A.alloc("qk", [4, S], BF16)
            wg_rot = Rot("wg", [KC, 768], BF16, 1)
            cs_rot = Rot("cs", [2, 512], F32, 2)
            ev_rot = Rot("ev", [512], F32, 4)
            rp_rot = Rot("rp", [512], F32, 4)
            pe_rot = Rot("pexp", [4, 256], BF16, 4)
            v_rot = [A.alloc(f"vv{i}", [4, 65], BF16) for i in range(4)]
            for (vv, b_vv) in v_rot:
                add("pool", lambda e, vv=vv: e.memset(vv, 1.0), writes=[b_vv])
            for g in range(3):
                d = DIL[g]
                L = S // d
                nb = L // 128
                (wg, b_wg) = wg_rot.next()
                dma("pool", wg, w_in.rearrange("(kc p) n -> p kc n", p=128)[:, :, g * 768:(g + 1) * 768], [], [b_wg])
                for k in range(NBK):
                    blk = slice(k * 512, (k + 1) * 512)
                    (cs, b_cs) = cs_rot.next()
                    dma("sp", cs[:, 0, :], COS[b, :, blk], [B_COS[b]], [b_cs])
                    dma("sp", cs[:, 1, :], SIN[b, :, blk], [B_COS[b]], [b_cs])
                    for which in range(2):
                        evs = []
                        for part in range(2):
                            col = (which * 2 + part) * 128
                            (bk, b_bk) = psum()
                            for kc in range(KC):
                                add("pe", lambda e, bk=bk, kc=kc, col=col, blk=blk, wg=wg: e.matmul(
                                    bk[:, :], lhsT=wg[:, kc, col:col + 128], rhs=xnT[:, kc, blk],
                                    start=(kc == 0), stop=(kc == KC - 1)), reads=[b_wg, b_xnT[k]], writes=[b_bk])
                            (ev, b_ev) = ev_rot.next()
                            add("act", lambda e, ev=ev, bk=bk: e.copy(out=ev, in_=bk[:, :]), reads=[b_bk], writes=[b_ev])
                            evs.append((ev, b_ev))
                        (eA, b_eA), (eB, b_eB) = evs
                        (t1, b_t1) = rp_rot.next()
                        (t2, b_t2) = rp_rot.next()
                        oA = qk[:, which * 2, blk]
                        oB = qk[:, which * 2 + 1, blk]
                        add("dve", lambda e, t1=t1, eA=eA, cs=cs: e.tensor_tensor(out=t1, in0=eA, in1=cs[:, 0, :], op=ALU.mult),
                            reads=[b_eA, b_cs], writes=[b_t1])
                        add("dve", lambda e, t2=t2, eB=eB, cs=cs: e.tensor_tensor(out=t2, in0=eB, in1=cs[:, 1, :], op=ALU.mult),
                            reads=[b_eB, b_cs], writes=[b_t2])
                        add("dve", lambda e, t1=t1, t2=t2, oA=oA: e.tensor_tensor(out=oA, in0=t1, in1=t2, op=ALU.subtract),
                            reads=[b_t1, b_t2], writes=[b_qk])
                        add("dve", lambda e, t1=t1, eB=eB, cs=cs: e.tensor_tensor(out=t1, in0=eB, in1=cs[:, 0, :], op=ALU.mult),
                            reads=[b_eB, b_cs], writes=[b_t1])
                        add("dve", lambda e, t2=t2, eA=eA, cs=cs: e.tensor_tensor(out=t2, in0=eA, in1=cs[:, 1, :], op=ALU.mult),
                            reads=[b_eA, b_cs], writes=[b_t2])
                        add("dve", lambda e, t1=t1, t2=t2, oB=oB: e.tensor_tensor(out=oB, in0=t1, in1=t2, op=ALU.add),
                            reads=[b_t1, b_t2], writes=[b_qk])
                vcount = [0]
                for r in range(d):
                    def tsl(jj, n=1, r=r):
                        st = r + d * 128 * jj
                        return slice(st, st + d * (128 * n - 1) + 1, d) if d > 1 else slice(st, st + 128 * n)
                    items = []

                    def emit_pv(j, items=items, tsl=tsl):
                        (vv, b_vv, px, b_px) = items[j]
                        (bk3, b_bk3) = psum()
                        for hh in range(4):
                            o_ap = bk3[0:65, hh * 128:(hh + 1) * 128]
                            if j > 0:
                                (pvv, b_pvv, ppx, b_ppx) = items[j - 1]
                                add("pe", lambda e, o_ap=o_ap, pvv=pvv, ppx=ppx, hh=hh: e.matmul(
                                    o_ap, lhsT=pvv[:, hh, :], rhs=ppx[:, hh, 128:256], start=True, stop=False),
                                    reads=[b_pvv, b_ppx], writes=[b_bk3])
                            add("pe", lambda e, o_ap=o_ap, vv=vv, px=px, hh=hh, first=(j == 0): e.matmul(
                                o_ap, lhsT=vv[:, hh, :], rhs=px[:, hh, 0:128], start=first, stop=True),
                                reads=[b_vv, b_px], writes=[b_bk3])
                        a_ap = acc[0:65, :, tsl(j)]
                        p_ap = bk3[0:65, :].rearrange("p (h q) -> p h q", h=4)
                        if g == 0:
                            add("dve", lambda e, a_ap=a_ap, p_ap=p_ap: e.tensor_copy(out=a_ap, in_=p_ap), reads=[b_bk3], writes=[b_acc])
                        else:
                            add("dve", lambda e, a_ap=a_ap, p_ap=p_ap: e.tensor_tensor(out=a_ap, in0=p_ap, in1=a_ap, op=ALU.add),
                                reads=[b_bk3, b_acc], writes=[b_acc])

                    for j in range(nb):
                        (vv, b_vv) = v_rot[vcount[0] % 4]
                        vcount[0] += 1
                        (bk, b_bk) = psum()
                        for kc in range(KC):
                            add("pe", lambda e, bk=bk, kc=kc, wg=wg, ts=tsl(j): e.matmul(
                                bk[:, 0:256], lhsT=xnT[:, kc, ts], rhs=wg[:, kc, 512:768],
                                start=(kc == 0), stop=(kc == KC - 1)), reads=[b_wg] + b_xnT, writes=[b_bk])
                        add("act", lambda e, vv=vv, bk=bk: e.copy(out=vv[:, :, 0:64], in_=bk[:, 0:256].rearrange("p (h d) -> p h d", h=4)),
                            reads=[b_bk], writes=[b_vv])
                        nq = 2 if j < nb - 1 else 1
                        (px, b_px) = pe_rot.next()
                        for hh in range(4):
                            (bk2, b_bk2) = psum()
                            for part in range(2):
                                add("pe", lambda e, bk2=bk2, hh=hh, part=part, ks=tsl(j), qs=tsl(j, nq), nq=nq: e.matmul(
                                    bk2[:, 0:128 * nq], lhsT=qk[32 * hh:32 * hh + 32, 2 + part, ks],
                                    rhs=qk[32 * hh:32 * hh + 32, part, qs], start=(part == 0), stop=(part == 1),
                                    tile_position=(32 * hh, 0)), reads=[b_qk], writes=[b_bk2])
                            add("act", lambda e, px=px, bk2=bk2, hh=hh, nq=nq: e.activation(
                                out=px[:, hh, 0:128 * nq], in_=bk2[:, 0:128 * nq], func=AF.Exp, scale=0.125),
                                reads=[b_bk2], writes=[b_px])
                        add("dve", lambda e, px=px, nq=nq: e.tensor_tensor(
                            out=px[:, :, 0:128 * nq], in0=px[:, :, 0:128 * nq],
                            in1=mask4[:, :, 0:128 * nq], op=ALU.mult),
                            reads=[b_px, b_mask4], writes=[b_px])
                        items.append((vv, b_vv, px, b_px))
                        if j >= 2:
                            emit_pv(j - 2)
                    if nb >= 2:
                        emit_pv(nb - 2)
                    emit_pv(nb - 1)
            A.release(m_b)
            yaT, b_yaT = A.alloc("yaT", [4, S], BF16, top=True)
            m_n = A.mark()
            rd_rot = Rot("rden", [512], F32, 2)
            for k in range(NBK):
                blk = slice(k * 512, (k + 1) * 512)
                for hh in range(4):
                    (bk, b_bk) = psum()
                    add("pe", lambda e, bk=bk, hh=hh, blk=blk: e.matmul(bk[0:64, :], lhsT=sel_f[0:65, :], rhs=acc[0:65, hh, blk],
                                                                     start=True, stop=True), reads=[b_sel, b_acc], writes=[b_bk])
                    (rd, b_rd) = rd_rot.next()
                    add("dve", lambda e, rd=rd, bk=bk: e.reciprocal(out=rd[0:64, :], in_=bk[0:64, :]), reads=[b_bk], writes=[b_rd])
                    add("dve", lambda e, rd=rd, hh=hh, blk=blk: e.tensor_tensor(out=yaT[0:64, hh, blk], in0=acc[0:64, hh, blk],
                                                                             in1=rd[0:64, :], op=ALU.mult),
                        reads=[b_acc, b_rd], writes=[b_yaT])
            if dev:
                (dd, b_dd) = A.alloc("dbgya", [4, S], F32)
                add("dve", lambda e, dd=dd: e.tensor_copy(out=dd[0:64], in_=yaT[0:64]), reads=[b_yaT], writes=[b_dd])
                dma("sp", dbg["ya"][b], dd[0:64], [b_dd], [B_OUT])
            A.release(m_n)
            A.release(persist_mark)

            ybT, b_ybT = A.alloc("ybT", [4, S], BF16, top=True)
            m_c = A.mark()
            xs_rot = Rot("xs", [KC, 512], BF16, 2)
            wuv, b_wuv = A.alloc("wuv", [KC, 1024], BF16)
            dma("pool", wuv, w_in.rearrange("(kc p) n -> p kc n", p=128)[:, :, 2304:3328], [], [b_wuv])
            uT_rot = Rot("uT", [4, 512], BF16, 2)
            gtmp = Rot("gtmp", [512], F32, 8)
            vg_rot = Rot("vg", [512], F32, 4)
            vh_rot = Rot("vh", [512], BF16, 4)
            st_rot = Rot("lnst", [4], F32, 8)
            ljunk = A.alloc("ljunk", [512], BF16)
            yt_rot = Rot("ytmp", [512], F32, 4)
            for k in range(NBK):
                blk = slice(k * 512, (k + 1) * 512)
                (uT, b_uT) = uT_rot.next()
                (xs, b_xs) = xs_rot.next()
                dma("sp", xs, XN[b, :, :, blk], [B_XN[k]], [b_xs])
                def c_uchunk(c, xs=xs, b_xs=b_xs, uT=uT, b_uT=b_uT):
                    (bk, b_bk) = psum()
                    for kc in range(KC):
                        add("pe", lambda e, bk=bk, kc=kc, c=c, xs=xs: e.matmul(
                            bk[:, :], lhsT=wuv[:, kc, c * 128:(c + 1) * 128], rhs=xs[:, kc, :],
                            start=(kc == 0), stop=(kc == KC - 1)), reads=[b_wuv, b_xs], writes=[b_bk])
                    gelu_from_psum(bk, b_bk, uT[:, c, :], b_uT, gtmp, 512)

                def c_tile(t, k=k, xs=xs, b_xs=b_xs, uT=uT, b_uT=b_uT):
                    tok = slice(k * 512 + t * 128, k * 512 + (t + 1) * 128)
                    (bk, b_bk) = psum()
                    for kc in range(KC):
                        add("pe", lambda e, bk=bk, kc=kc, t=t, xs=xs: e.matmul(
                            bk[:, :], lhsT=xs[:, kc, t * 128:(t + 1) * 128], rhs=wuv[:, kc, 512:1024],
                            start=(kc == 0), stop=(kc == KC - 1)), reads=[b_wuv, b_xs], writes=[b_bk])
                    (vg, b_vg) = vg_rot.next()
                    gelu_from_psum(bk, b_bk, vg, b_vg, gtmp, 512)
                    (st, b_st) = st_rot.next()
                    (lj, b_lj) = ljunk
                    add("pool", lambda e, st=st: e.memset(st, 0.0), writes=[b_st])
                    add("act", lambda e, st=st, vg=vg, lj=lj: e.activation(out=lj, in_=vg, func=AF.Copy, accum_out=st[:, 0:1]),
                        reads=[b_vg], writes=[b_lj, b_st])
                    add("act", lambda e, st=st, vg=vg, lj=lj: e.activation(out=lj, in_=vg, func=AF.Square, accum_out=st[:, 1:2]),
                        reads=[b_vg], writes=[b_lj, b_st])
                    add("dve", lambda e, st=st: e.tensor_scalar(out=st[:, 0:2], in0=st[:, 0:2], scalar1=1.0 / 512, scalar2=None, op0=ALU.mult),
                        reads=[b_st], writes=[b_st])
                    add("dve", lambda e, st=st: e.tensor_tensor(out=st[:, 2:3], in0=st[:, 0:1], in1=st[:, 0:1], op=ALU.mult),
                        reads=[b_st], writes=[b_st])
                    add("dve", lambda e, st=st: e.tensor_tensor(out=st[:, 1:2], in0=st[:, 1:2], in1=st[:, 2:3], op=ALU.subtract),
                        reads=[b_st], writes=[b_st])
                    add("act", lambda e, st=st: e.activation(out=st[:, 1:2], in_=st[:, 1:2], func=AF.Sqrt, bias=eps_ln[:, 0:1], scale=1.0),
                        reads=[b_st, b_epsln], writes=[b_st])
                    add("dve", lambda e, st=st: e.reciprocal(out=st[:, 1:2], in_=st[:, 1:2]), reads=[b_st], writes=[b_st])
                    add("dve", lambda e, st=st, vg=vg: e.tensor_scalar(out=vg, in0=vg, scalar1=st[:, 0:1], scalar2=st[:, 1:2],
                                                                     op0=ALU.subtract, op1=ALU.mult), reads=[b_vg, b_st], writes=[b_vg])
                    (vh, b_vh) = vh_rot.next()
                    add("dve", lambda e, vh=vh, vg=vg: e.tensor_tensor(out=vh, in0=vg, in1=gamrow, op=ALU.mult),
                        reads=[b_vg, b_gamrow], writes=[b_vh])
                    (bk2, b_bk2) = psum()
                    for g in range(4):
                        add("pe", lambda e, bk2=bk2, g=g, vh=vh: e.matmul(
                            bk2[:, g * 128:(g + 1) * 128], lhsT=vh[:, g * 128:(g + 1) * 128], rhs=wsT[:, g, :],
                            start=True, stop=True), reads=[b_vh, b_wsT], writes=[b_bk2])
                    (yt, b_yt) = yt_rot.next()
                    add("dve", lambda e, yt=yt, bk2=bk2: e.tensor_tensor(out=yt, in0=bk2[:, :], in1=B2.rearrange("p g t -> p (g t)"), op=ALU.add),
                        reads=[b_bk2, b_B2], writes=[b_yt])
                    add("dve", lambda e, yt=yt, uT=uT, t=t, tok=tok: e.tensor_tensor(
                        out=ybT[:, :, tok], in0=yt.rearrange("p (g t) -> p g t", g=4), in1=uT[:, :, t * 128:(t + 1) * 128], op=ALU.mult),
                        reads=[b_yt, b_uT], writes=[b_ybT])

                emit_interleaved([record(lambda c=c: c_uchunk(c)) for c in range(4)])
                emit_interleaved([record(lambda t=t: c_tile(t)) for t in (0, 1)])
                emit_interleaved([record(lambda t=t: c_tile(t)) for t in (2, 3)])
            if dev:
                (dd, b_dd) = A.alloc("dbgyb", [4, S], F32)
                add("dve", lambda e, dd=dd: e.tensor_copy(out=dd, in_=ybT), reads=[b_ybT], writes=[b_dd])
                dma("sp", dbg["yb"][b], dd, [b_dd], [B_OUT])
            A.release(m_c)

            m_d = A.mark()
            wgt, b_wgt = A.alloc("wgt", [KC, 2048], BF16)
            woa, b_woa = A.alloc("woa", [4, D], BF16)
            wob, b_wob = A.alloc("wob", [4, D], BF16)
            wo, b_wo = A.alloc("wo", [KC, D], BF16)
            dma("pool", wgt, w_in.rearrange("(kc p) n -> p kc n", p=128)[:, :, 3328:5376], [], [b_wgt])
            dma("pool", woa[0:64], w_oa.rearrange("(h p) n -> p h n", p=64), [], [b_woa])
            dma("pool", wob, w_ob.rearrange("(g p) n -> p g n", p=128), [], [b_wob])
            dma("pool", wo, w_o.rearrange("(kc p) n -> p kc n", p=128), [], [b_wo])
            mg_rot = Rot("merged", [KC, 512], BF16, 2)
            sg_rot = Rot("sg", [512], F32, 4)
            mm_rot = Rot("mm", [512], F32, 4)
            xr_rot = Rot("xr", [D], F32, 2)
            xs_rot = Rot("xsd", [KC, 512], BF16, 2)
            for k in range(NBK):
                blk = slice(k * 512, (k + 1) * 512)
                (mg, b_mg) = mg_rot.next()
                (xs, b_xs) = xs_rot.next()
                dma("sp", xs, XN[b, :, :, blk], [B_XN[k]], [b_xs])
                for c in range(KC):
                    (bka, b_bka) = psum()
                    (bkb, b_bkb) = psum()
                    (bkA, b_bkA) = psum()
                    (bkB, b_bkB) = psum()
                    for kc in range(KC):
                        add("pe", lambda e, bka=bka, kc=kc, c=c, xs=xs: e.matmul(
                            bka[:, :], lhsT=wgt[:, kc, c * 128:(c + 1) * 128], rhs=xs[:, kc, :],
                            start=(kc == 0), stop=(kc == KC - 1)), reads=[b_wgt, b_xs], writes=[b_bka])
                    for kc in range(KC):
                        add("pe", lambda e, bkb=bkb, kc=kc, c=c, xs=xs: e.matmul(
                            bkb[:, :], lhsT=wgt[:, kc, 1024 + c * 128:1024 + (c + 1) * 128], rhs=xs[:, kc, :],
                            start=(kc == 0), stop=(kc == KC - 1)), reads=[b_wgt, b_xs], writes=[b_bkb])
                    for hh in range(4):
                        add("pe", lambda e, bkA=bkA, hh=hh, c=c, blk=blk: e.matmul(
                            bkA[:, :], lhsT=woa[0:64, hh, c * 128:(c + 1) * 128], rhs=yaT[0:64, hh, blk],
                            start=(hh == 0), stop=(hh == 3)), reads=[b_woa, b_yaT], writes=[b_bkA])
                    for g in range(4):
                        add("pe", lambda e, bkB=bkB, g=g, c=c, blk=blk: e.matmul(
                            bkB[:, :], lhsT=wob[:, g, c * 128:(c + 1) * 128], rhs=ybT[:, g, blk],
                            start=(g == 0), stop=(g == 3)), reads=[b_wob, b_ybT], writes=[b_bkB])
                    (sa, b_sa) = sg_rot.next()
                    (sb_, b_sb) = sg_rot.next()
                    add("act", lambda e, sa=sa, bka=bka, c=c: e.activation(out=sa, in_=bka[:, :], func=AF.Sigmoid, bias=bgt_t[:, c:c + 1], scale=1.0),
                        reads=[b_bka, b_bgt], writes=[b_sa])
                    add("act", lambda e, sb_=sb_, bkb=bkb, c=c: e.activation(out=sb_, in_=bkb[:, :], func=AF.Sigmoid, bias=bgt_t[:, 8 + c:9 + c], scale=1.0),
                        reads=[b_bkb, b_bgt], writes=[b_sb])
                    (m1, b_m1) = mm_rot.next()
                    (m2, b_m2) = mm_rot.next()
                    add("dve", lambda e, m1=m1, sa=sa, bkA=bkA: e.tensor_tensor(out=m1, in0=bkA[:, :], in1=sa, op=ALU.mult),
                        reads=[b_bkA, b_sa], writes=[b_m1])
                    add("dve", lambda e, m2=m2, sb_=sb_, bkB=bkB: e.tensor_tensor(out=m2, in0=bkB[:, :], in1=sb_, op=ALU.mult),
                        reads=[b_bkB, b_sb], writes=[b_m2])
                    add("dve", lambda e, m1=m1, m2=m2, mg=mg, c=c: e.tensor_tensor(out=mg[:, c, :], in0=m1, in1=m2, op=ALU.add),
                        reads=[b_m1, b_m2], writes=[b_mg])
                for t in range(4):
                    ti = k * 4 + t
                    gi = b * NT + ti
                    (xr, b_xr) = xr_rot.next()
                    (h1t, b_h1t) = (xr, b_xr)
                    dma("sp", xr, x[b, ti * 128:(ti + 1) * 128, :], [], [b_xr])
                    for n in range(2):
                        (bk, b_bk) = psum()
                        for c in range(KC):
                            add("pe", lambda e, bk=bk, c=c, t=t, n=n, mg=mg: e.matmul(
                                bk[:, :], lhsT=mg[:, c, t * 128:(t + 1) * 128], rhs=wo[:, c, n * 512:(n + 1) * 512],
                                start=(c == 0), stop=(c == KC - 1)), reads=[b_mg, b_wo], writes=[b_bk])
                        add("dve", lambda e, bk=bk, xr=xr, h1t=h1t, n=n: e.tensor_tensor(
                            out=h1t[:, n * 512:(n + 1) * 512], in0=bk[:, :], in1=xr[:, n * 512:(n + 1) * 512], op=ALU.add),
                            reads=[b_bk, b_xr], writes=[b_h1t])
                    dma("sp", H[gi * 128:(gi + 1) * 128, :], h1t, [b_h1t], [B_H[gi]])
                    if dev:
                        dma("sp", dbg["h1"][gi * 128:(gi + 1) * 128, :], h1t, [b_h1t], [B_OUT])
            A.release(m_d)
            A.release(persist_mark)
            A.release_top()

            m_e = A.mark()
            kT, b_kT = A.alloc("kT", [KC, NMEM], BF16)
            vm, b_vm = A.alloc("vm", [2, D], BF16)
            wq, b_wq = A.alloc("wq", [KC, D], BF16)
            wox, b_wox = A.alloc("wox", [KC, D], BF16)
            dma("pool", wq, w_qx.rearrange("(kc p) n -> p kc n", p=128), [], [b_wq])
            dma("pool", wox, w_ox.rearrange("(kc p) n -> p kc n", p=128), [], [b_wox])
            m_e0 = A.mark()
            wkv, b_wkv = A.alloc("wkv", [KC, 2 * D], BF16)
            dma("pool", wkv, w_kvx.rearrange("(kc p) n -> p kc n", p=128), [], [b_wkv])
            gmem_t, b_gmem = A.alloc("gmem", [D], F32)
            dma("sp", gmem_t, g_mem.partition_broadcast(128), [], [b_gmem])
            mnT, b_mnT = A.alloc("mnT", [KC, NMEM], BF16)
            mt_rot = Rot("mt", [D], F32, 2)
            mb_rot = Rot("mb", [D], BF16, 2)
            small = Rot("ssm", [2], F32, 4)
            junk = A.alloc("junkm", [D], BF16)
            for i in range(2):
                (mt, b_mt) = mt_rot.next()
                (mb, b_mb) = mb_rot.next()
                dma("sp", mt, mem[b, i * 128:(i + 1) * 128, :], [], [b_mt])
                rms_tile(mt, b_mt, gmem_t, b_gmem, mb, b_mb, small, junk)
                transpose_to(mb, b_mb, mnT[:, :, i * 128:(i + 1) * 128], b_mnT)
            for oc in range(KC):
                (bk, b_bk) = psum()
                for kc in range(KC):
                    add("pe", lambda e, bk=bk, kc=kc, oc=oc: e.matmul(
                        bk[:, 0:NMEM], lhsT=wkv[:, kc, oc * 128:(oc + 1) * 128], rhs=mnT[:, kc, :],
                        start=(kc == 0), stop=(kc == KC - 1)), reads=[b_wkv, b_mnT], writes=[b_bk])
                add("act", lambda e, bk=bk, oc=oc: e.copy(out=kT[:, oc, :], in_=bk[:, 0:NMEM]), reads=[b_bk], writes=[b_kT])
            for mchunk in range(2):
                for n in range(2):
                    (bk, b_bk) = psum()
                    for kc in range(KC):
                        add("pe", lambda e, bk=bk, kc=kc, mchunk=mchunk, n=n: e.matmul(
                            bk[:, :], lhsT=mnT[:, kc, mchunk * 128:(mchunk + 1) * 128], rhs=wkv[:, kc, D + n * 512:D + (n + 1) * 512],
                            start=(kc == 0), stop=(kc == KC - 1)), reads=[b_wkv, b_mnT], writes=[b_bk])
                    add("act", lambda e, bk=bk, mchunk=mchunk, n=n: e.copy(out=vm[:, mchunk, n * 512:(n + 1) * 512], in_=bk[:, :]),
                        reads=[b_bk], writes=[b_vm])
            A.release(m_e0)

            gx_t, b_gx = A.alloc("gx", [D], F32)
            gmoe_t, b_gmoe = A.alloc("gmoe", [D], F32)
            dma("sp", gx_t, g_x.partition_broadcast(128), [], [b_gx])
            dma("sp", gmoe_t, g_moe.partition_broadcast(128), [], [b_gmoe])
            h_rot = Rot("ht", [D], F32, 8)
            hb_rot = Rot("hb", [D], BF16, 4)
            small = Rot("sse", [2], F32, 8)
            junk = A.alloc("junke", [D], BF16)
            hnT_rot = Rot("hnT", [KC, 512], BF16, 2)
            qT_rot = Rot("qT", [KC, 512], BF16, 2)
            oT_rot = Rot("oT", [KC, 512], BF16, 2)
            px_rot = Rot("pxx", [2, 512], BF16, 3)
            rd_rot = Rot("rdx", [512], F32, 2)
            h2_rot = Rot("h2t", [D], F32, 2)
            hn3f_rot = Rot("hn3f", [D], F32, 2)
            hn3b_rot = Rot("hn3b", [D], BF16, 2)
            hn3T_rot = Rot("hn3T", [KC, 128], F32, 2)
            rs_rot = Rot("rsm", [16], F32, 4)
            ls_rot = Rot("ls", [36], F32, 2)
            el_rot = Rot("el", [32], F32, 2)
            mk_rot = Rot("mk", [3, 32], F32, 2)
            mkb_rot = Rot("mkb", [32], BF16, 2)
            t8_rot = Rot("t8", [8], F32, 2)
            rec_rot = Rot("rec", [2, 2], I32, 2)
            dsc_rot = Rot("dsc", [2], I32, 2)

            def e_stage1(k, b=b):
                (hnT, b_hnT) = hnT_rot.next()
                hts = []

                def e_tile(t):
                    gi = b * NT + k * 4 + t
                    (ht, b_ht) = h_rot.next()
                    (hb, b_hb) = hb_rot.next()
                    dma("sp", ht, H[gi * 128:(gi + 1) * 128, :], [B_H[gi]], [b_ht])
                    rms_tile(ht, b_ht, gx_t, b_gx, hb, b_hb, small, junk)
                    transpose_to(hb, b_hb, hnT[:, :, t * 128:(t + 1) * 128], b_hnT, eng="act" if t % 2 == 0 else "dve")
                    hts.append((ht, b_ht, gi))

                emit_interleaved([record(lambda t=t: e_tile(t)) for t in range(4)])
                (qT, b_qT) = qT_rot.next()
                for oc in range(KC):
                    (bk, b_bk) = psum()
                    for kc in range(KC):
                        add("pe", lambda e, bk=bk, kc=kc, oc=oc, hnT=hnT: e.matmul(
                            bk[:, :], lhsT=wq[:, kc, oc * 128:(oc + 1) * 128], rhs=hnT[:, kc, :],
                            start=(kc == 0), stop=(kc == KC - 1)), reads=[b_wq, b_hnT], writes=[b_bk])
                    if oc % 2 == 0:
                        add("act", lambda e, bk=bk, oc=oc, qT=qT: e.copy(out=qT[:, oc, :], in_=bk[:, :]), reads=[b_bk], writes=[b_qT])
                    else:
                        add("dve", lambda e, bk=bk, oc=oc, qT=qT: e.tensor_copy(out=qT[:, oc, :], in_=bk[:, :]), reads=[b_bk], writes=[b_qT])
                return (hts, qT, b_qT)

            def e_stage2(k, st1, b=b):
                (hts, qT, b_qT) = st1
                pending = []
                outer_add = cur_add[0]

                def flush_router():
                    n1 = max(len(p[0]) for p in pending)
                    for i_ in range(n1):
                        for p in pending:
                            if i_ < len(p[0]):
                                outer_add(*p[0][i_])
                    for p in pending:
                        for it in p[1]:
                            outer_add(*it)
                    pending.clear()
                (oT, b_oT) = oT_rot.next()
                pxs = {}

                def e_scores(h):
                    (px, b_px) = px_rot.next()
                    pxs[h] = (px, b_px)
                    for mchunk in range(2):
                        (bk, b_bk) = psum()
                        for cc in range(2):
                            add("pe", lambda e, bk=bk, cc=cc, h=h, mchunk=mchunk, qT=qT: e.matmul(
                                bk[:, :], lhsT=kT[:, 2 * h + cc, mchunk * 128:(mchunk + 1) * 128], rhs=qT[:, 2 * h + cc, :],
                                start=(cc == 0), stop=(cc == 1)), reads=[b_kT, b_qT], writes=[b_bk])
                        add("act", lambda e, bk=bk, px=px, mchunk=mchunk: e.activation(out=px[:, mchunk, :], in_=bk[:, :], func=AF.Exp, scale=1.0 / 16.0),
                            reads=[b_bk], writes=[b_px])

                def e_pv(h):
                    (px, b_px) = pxs[h]
                    (bkd, b_bkd) = psum()
                    for mchunk in range(2):
                        add("pe", lambda e, bkd=bkd, px=px, mchunk=mchunk: e.matmul(
                            bkd[:, :], lhsT=ones_b, rhs=px[:, mchunk, :], start=(mchunk == 0), stop=(mchunk == 1)),
                            reads=[b_ones, b_px], writes=[b_bkd])
                    (rd, b_rd) = rd_rot.next()
                    add("dve", lambda e, rd=rd, bkd=bkd: e.reciprocal(out=rd, in_=bkd[:, :]), reads=[b_bkd], writes=[b_rd])
                    for cc in range(2):
                        (bk, b_bk) = psum()
                        for mchunk in range(2):
                            add("pe", lambda e, bk=bk, px=px, mchunk=mchunk, h=h, cc=cc: e.matmul(
                                bk[:, :], lhsT=vm[:, mchunk, (2 * h + cc) * 128:(2 * h + cc + 1) * 128], rhs=px[:, mchunk, :],
                                start=(mchunk == 0), stop=(mchunk == 1)), reads=[b_vm, b_px], writes=[b_bk])
                        add("dve", lambda e, bk=bk, rd=rd, oT=oT, h=h, cc=cc: e.tensor_tensor(
                            out=oT[:, 2 * h + cc, :], in0=bk[:, :], in1=rd, op=ALU.mult), reads=[b_bk, b_rd], writes=[b_oT])

                e_scores(0)
                for h in range(4):
                    if h + 1 < 4:
                        e_scores(h + 1)
                    e_pv(h)
                for t in range(4):
                    (ht, b_ht, gi) = hts[t]
                    (h2t, b_h2t) = h2_rot.next()
                    for n in range(2):
                        (bk, b_bk) = psum()
                        for c in range(KC):
                            add("pe", lambda e, bk=bk, c=c, t=t, n=n, oT=oT: e.matmul(
                                bk[:, :], lhsT=oT[:, c, t * 128:(t + 1) * 128], rhs=wox[:, c, n * 512:(n + 1) * 512],
                                start=(c == 0), stop=(c == KC - 1)), reads=[b_oT, b_wox], writes=[b_bk])
                        add("dve", lambda e, bk=bk, ht=ht, h2t=h2t, n=n: e.tensor_tensor(
                            out=h2t[:, n * 512:(n + 1) * 512], in0=bk[:, :], in1=ht[:, n * 512:(n + 1) * 512], op=ALU.add),
                            reads=[b_bk, b_ht], writes=[b_h2t])
                    dma("sp", H[gi * 128:(gi + 1) * 128, :], h2t, [b_h2t], [B_H[gi]])
                    if dev:
                        dma("sp", dbg["h2"][gi * 128:(gi + 1) * 128, :], h2t, [b_h2t], [B_OUT])
                    lst1, lst2 = [], []
                    cur_add[0] = make_recorder(lst1)
                    (hn3f, b_hn3f) = hn3f_rot.next()
                    (hn3b, b_hn3b) = hn3b_rot.next()
                    rms_tile(h2t, b_h2t, gmoe_t, b_gmoe, hn3b, b_hn3b, small, junk, dst_f32=hn3f, b_dst32=b_hn3f)
                    dma("sp", HN3[gi * 128:(gi + 1) * 128, :], hn3b, [b_hn3b], [B_HN3])
                    (hn3T, b_hn3T) = hn3T_rot.next()
                    (bk, b_bk) = psum()
                    for half in range(2):
                        for c4 in range(4):
                            c = half * 4 + c4
                            add("pe", lambda e, bk=bk, c=c, c4=c4, hn3f=hn3f: e.transpose(
                                out=bk[:, c4 * 128:(c4 + 1) * 128], in_=hn3f[:, c * 128:(c + 1) * 128], identity=ident_f),
                                reads=[b_hn3f, b_ident_f], writes=[b_bk])
                        add("act", lambda e, bk=bk, half=half, hn3T=hn3T: e.copy(
                            out=hn3T[:, half * 4:(half + 1) * 4, :], in_=bk[:, :].rearrange("p (c t) -> p c t", c=4)),
                            reads=[b_bk], writes=[b_hn3T])
                    (bk, b_bk) = psum()
                    (bkL, b_bkL) = (bk, b_bk)
                    for c in range(KC):
                        add("pe", lambda e, bk=bk, c=c, hn3T=hn3T: e.matmul(
                            bk[:, 0:36], lhsT=hn3T[:, c, :], rhs=wr_t[:, c, :], start=(c == 0), stop=(c == KC - 1)),
                            reads=[b_hn3T, b_wr], writes=[b_bk])
                    (ls, b_ls) = ls_rot.next()
                    (rs, b_rs) = rs_rot.next()
                    (el, b_el) = el_rot.next()
                    (mk, b_mk) = mk_rot.next()
                    (mkb, b_mkb) = mkb_rot.next()
                    (t8, b_t8) = t8_rot.next()
                    (rec, b_rec) = rec_rot.next()
                    add("dve", lambda e, ls=ls, bk=bk: e.tensor_tensor(out=ls, in0=bk[:, 0:36], in1=br_t, op=ALU.add),
                        reads=[b_bk, b_br], writes=[b_ls])
                    add("dve", lambda e, ls=ls, rs=rs: e.reduce_max(out=rs[:, 0:1], in_=ls[:, 0:4], axis=AX.X), reads=[b_ls], writes=[b_rs])
                    add("dve", lambda e, rs=rs: e.tensor_scalar(out=rs[:, 1:2], in0=rs[:, 0:1], scalar1=-1.0, scalar2=None, op0=ALU.mult),
                        reads=[b_rs], writes=[b_rs])
                    add("pool", lambda e, rs=rs: e.memset(rs[:, 2:3], 0.0), writes=[b_rs])
                    add("act", lambda e, ls=ls, rs=rs, mk=mk: e.activation(out=mk[:, 2, 0:4], in_=ls[:, 0:4], func=AF.Exp, bias=rs[:, 1:2], scale=1.0,
                                                                          accum_out=rs[:, 2:3]), reads=[b_ls, b_rs], writes=[b_mk, b_rs])
                    add("dve", lambda e, rs=rs: e.reciprocal(out=rs[:, 3:4], in_=rs[:, 2:3]), reads=[b_rs], writes=[b_rs])
                    add("dve", lambda e, ls=ls, rs=rs, mk=mk: e.tensor_scalar(out=mk[:, 2, 8:12], in0=ls[:, 0:4], scalar1=rs[:, 0:1], scalar2=None, op0=ALU.is_ge),
                        reads=[b_ls, b_rs], writes=[b_mk])
                    add("dve", lambda e, mk=mk: e.tensor_scalar(out=mk[:, 2, 8:12], in0=mk[:, 2, 8:12], scalar1=1e30, scalar2=-1e30, op0=ALU.mult, op1=ALU.add),
                        reads=[b_mk], writes=[b_mk])
                    add("dve", lambda e, ls=ls, el=el, mk=mk: e.tensor_tensor(
                        out=el.rearrange("p (g x) -> p g x", g=4), in0=ls[:, 4:36].rearrange("p (g x) -> p g x", g=4),
                        in1=mk[:, 2, 8:12].unsqueeze(2).to_broadcast([128, 4, 8]), op=ALU.add), reads=[b_ls, b_mk], writes=[b_el])
                    add("dve", lambda e, el=el, t8=t8: e.max(out=t8, in_=el), reads=[b_el], writes=[b_t8])
                    add("dve", lambda e, el=el, t8=t8, mk=mk: e.tensor_scalar(out=mk[:, 0, :], in0=el, scalar1=t8[:, 0:1], scalar2=None, op0=ALU.is_ge),
                        reads=[b_el, b_t8], writes=[b_mk])
                    add("dve", lambda e, el=el, t8=t8, mk=mk: e.tensor_scalar(out=mk[:, 1, :], in0=el, scalar1=t8[:, 1:2], scalar2=None, op0=ALU.is_ge),
                        reads=[b_el, b_t8], writes=[b_mk])
                    add("dve", lambda e, mk=mk, mkb=mkb: e.tensor_copy(out=mkb, in_=mk[:, 1, :]), reads=[b_mk], writes=[b_mkb])
                    add("dve", lambda e, mk=mk: e.tensor_tensor(out=mk[:, 1, :], in0=mk[:, 1, :], in1=mk[:, 0, :], op=ALU.subtract),
                        reads=[b_mk], writes=[b_mk])
                    add("dve", lambda e, t8=t8, rs=rs: e.tensor_tensor(out=rs[:, 4:5], in0=t8[:, 0:1], in1=t8[:, 1:2], op=ALU.subtract),
                        reads=[b_t8], writes=[b_rs])
                    add("act", lambda e, rs=rs: e.activation(out=rs[:, 5:6], in_=rs[:, 4:5], func=AF.Exp, scale=-1.0), reads=[b_rs], writes=[b_rs])
                    add("dve", lambda e, rs=rs: e.tensor_scalar(out=rs[:, 5:6], in0=rs[:, 5:6], scalar1=1.0, scalar2=None, op0=ALU.add), reads=[b_rs], writes=[b_rs])
                    add("dve", lambda e, rs=rs: e.reciprocal(out=rs[:, 5:6], in_=rs[:, 5:6]), reads=[b_rs], writes=[b_rs])
                    add("dve", lambda e, rs=rs: e.tensor_tensor(out=rs[:, 6:7], in0=rs[:, 5:6], in1=rs[:, 3:4], op=ALU.mult), reads=[b_rs], writes=[b_rs])
                    add("dve", lambda e, rs=rs: e.tensor_tensor(out=rs[:, 7:8], in0=rs[:, 3:4], in1=rs[:, 6:7], op=ALU.subtract), reads=[b_rs], writes=[b_rs])
                    (bkp, b_bkp) = (bkL, b_bkL)
                    add("pe", lambda e, bkp=bkp, mkb=mkb: e.matmul(bkp[:, 64:96], lhsT=ltri_b, rhs=mkb, start=True, stop=True),
                        reads=[b_ltri, b_mkb], writes=[b_bkp])
                    add("pe", lambda e, bkp=bkp, mkb=mkb: e.matmul(bkp[:, 96:128], lhsT=ones_b, rhs=mkb, start=True, stop=True),
                        reads=[b_ones, b_mkb], writes=[b_bkp])
                    cur_add[0] = make_recorder(lst2)
                    add("dve", lambda e, bkp=bkp, mk=mk: e.tensor_tensor(out=mk[:, 2, :], in0=bkp[:, 64:96], in1=base_t, op=ALU.add),
                        reads=[b_bkp, b_base], writes=[b_mk])
                    add("dve", lambda e, bkp=bkp: e.tensor_tensor(out=base_t, in0=bkp[:, 96:128], in1=base_t, op=ALU.add),
                        reads=[b_bkp, b_base], writes=[b_base])
                    add("dve", lambda e, el=el, mk=mk: e.tensor_scalar(out=el, in0=mk[:, 2, :], scalar1=float(C), scalar2=1e6, op0=ALU.is_ge, op1=ALU.mult),
                        reads=[b_mk], writes=[b_el])
                    add("dve", lambda e, el=el, mk=mk: e.tensor_tensor(out=mk[:, 2, :], in0=mk[:, 2, :], in1=el, op=ALU.add), reads=[b_mk, b_el], writes=[b_mk])
                    add("dve", lambda e, mk=mk: e.tensor_tensor(out=mk[:, 2, :], in0=mk[:, 2, :], in1=ec_t, op=ALU.add), reads=[b_mk, b_ec], writes=[b_mk])
                    for sl in range(2):
                        add("dve", lambda e, mk=mk, el=el, sl=sl: e.tensor_tensor(out=el, in0=mk[:, sl, :], in1=mk[:, 2, :], op=ALU.mult),
                            reads=[b_mk], writes=[b_el])
                        add("dve", lambda e, el=el, rs=rs, sl=sl: e.reduce_sum(out=rs[:, 8 + sl:9 + sl], in_=el, axis=AX.X), reads=[b_el], writes=[b_rs])
                    add("dve", lambda e, rs=rs: e.tensor_scalar_min(out=rs[:, 8:10], in0=rs[:, 8:10], scalar1=float(NSLOT)), reads=[b_rs], writes=[b_rs])
                    add("dve", lambda e, rs=rs, gi=gi: e.tensor_copy(out=dest_all[:, gi, :], in_=rs[:, 8:10]), reads=[b_rs], writes=[b_dest])
                    for sl in range(2):
                        add("dve", lambda e, rec=rec, sl=sl, rs=rs: e.tensor_copy(out=rec[:, sl, 1:2].bitcast(F32), in_=rs[:, 6 + sl:7 + sl]),
                            reads=[b_rs], writes=[b_rec])
                        add("dve", lambda e, rec=rec, sl=sl, gi=gi: e.tensor_scalar(out=rec[:, sl, 0:1], in0=iota_f[:, 0:1], scalar1=float(gi * 128),
                                                                                    scalar2=None, op0=ALU.add), reads=[b_iota], writes=[b_rec])
                    for sl in range(2):
                        add("pool", lambda e, rec=rec, sl=sl, gi=gi: e.indirect_dma_start(
                            out=SLOT, out_offset=bass.IndirectOffsetOnAxis(ap=dest_all[:, gi, sl:sl + 1], axis=0),
                            in_=rec[:, sl, :], in_offset=None, bounds_check=PR["bc_slot"], oob_is_err=False),
                            reads=[b_rec, b_dest], writes=[B_SLOT], dma=True)
                    cur_add[0] = outer_add
                    pending.append((lst1, lst2))
                    if len(pending) == 1:
                        flush_router()
            stbox = []

            def run_s1(k_):
                psum_pool[0] = "a"
                stbox.append(e_stage1(k_))
                psum_pool[0] = None

            def run_s2(k_, st_):
                psum_pool[0] = "b"
                e_stage2(k_, st_)
                psum_pool[0] = None

            run_s1(0)
            for k in range(NBK):
                st_cur = stbox[k]
                la = record(lambda: run_s1(k + 1)) if k + 1 < NBK else []
                lb = record(lambda: run_s2(k, st_cur))
                emit_merged(la, lb)
            A.release(m_e)

        if dev:
            dma("sp", dbg["dest"], dest_all, [b_dest], [B_OUT])

        dma("sp", cnt_out, base_t, [b_base], [B_OUT])
        A.release(persist_mark)
        m_f = A.mark()
        wge_rot = Rot("wge", [KC, FF], BF16, 3)
        wue_rot = Rot("wue", [KC, FF], BF16, 3)
        wde_rot = Rot("wde", [4, D], BF16, 3)
        srec_rot = Rot("srec", [2], I32, 2 * CT)
        xe_rot = Rot("xe", [D], BF16, 4)
        xeT_rot = Rot("xeT", [KC, C], BF16, 2)
        aT_rot = Rot("aT", [4, C], BF16, 2)
        sl_rot = Rot("silu", [512], F32, 3)
        ys_rot = Rot("ysb", [D], F32, 3)
        segs = [(s0, min(512, C - s0)) for s0 in range(0, C, 512)]

        def f_load(ex):
            (wge, b_wge) = wge_rot.next()
            (wue, b_wue) = wue_rot.next()
            (wde, b_wde) = wde_rot.next()
            dma("pool", wge, w_ge[ex].rearrange("(kc p) f -> p kc f", p=128), [], [b_wge])
            dma("pool", wue, w_ue[ex].rearrange("(kc p) f -> p kc f", p=128), [], [b_wue])
            dma("pool", wde, w_de[ex].rearrange("(f p) n -> p f n", p=128), [], [b_wde])
            (xeT, b_xeT) = xeT_rot.next()
            srecs = []
            for j in range(CT):
                (srec, b_srec) = srec_rot.next()
                (xe, b_xe) = xe_rot.next()
                row0 = ex * C + j * 128
                dma("sp", srec, SLOT[row0:row0 + 128, :], [B_SLOT], [b_srec])
                add("pool", lambda e, xe=xe, srec=srec: e.indirect_dma_start(
                    out=xe, out_offset=None, in_=HN3, in_offset=bass.IndirectOffsetOnAxis(ap=srec[:, 0:1], axis=0),
                    bounds_check=PR["bc_tok"], oob_is_err=False),
                    reads=[b_srec, B_HN3], writes=[b_xe], dma=True)
                transpose_to(xe, b_xe, xeT[:, :, j * 128:(j + 1) * 128], b_xeT, eng="act" if j % 2 == 0 else "dve")
                srecs.append((srec, b_srec))
            return (wge, b_wge, wue, b_wue, wde, b_wde, xeT, b_xeT, srecs)

        def f_gateup(st):
            (wge, b_wge, wue, b_wue, wde, b_wde, xeT, b_xeT, srecs) = st
            (aT, b_aT) = aT_rot.next()
            for f in range(4):
                for (s0, sn) in segs:
                    (bkg, b_bkg) = psum()
                    (bku, b_bku) = psum()
                    for kc in range(KC):
                        add("pe", lambda e, bkg=bkg, kc=kc, f=f, s0=s0, sn=sn, wge=wge, xeT=xeT: e.matmul(
                            bkg[:, 0:sn], lhsT=wge[:, kc, f * 128:(f + 1) * 128], rhs=xeT[:, kc, s0:s0 + sn],
                            start=(kc == 0), stop=(kc == KC - 1)), reads=[b_wge, b_xeT], writes=[b_bkg])
                    for kc in range(KC):
                        add("pe", lambda e, bku=bku, kc=kc, f=f, s0=s0, sn=sn, wue=wue, xeT=xeT: e.matmul(
                            bku[:, 0:sn], lhsT=wue[:, kc, f * 128:(f + 1) * 128], rhs=xeT[:, kc, s0:s0 + sn],
                            start=(kc == 0), stop=(kc == KC - 1)), reads=[b_wue, b_xeT], writes=[b_bku])
                    (sl_, b_sl) = sl_rot.next()
                    add("act", lambda e, sl_=sl_, bkg=bkg, sn=sn: e.activation(out=sl_[:, 0:sn], in_=bkg[:, 0:sn], func=AF.Silu),
                        reads=[b_bkg], writes=[b_sl])
                    add("dve", lambda e, sl_=sl_, bku=bku, aT=aT, f=f, s0=s0, sn=sn: e.tensor_tensor(
                        out=aT[:, f, s0:s0 + sn], in0=bku[:, 0:sn], in1=sl_[:, 0:sn], op=ALU.mult), reads=[b_bku, b_sl], writes=[b_aT])
            return (aT, b_aT)

        def f_down(ex, st, aTb):
            (wge, b_wge, wue, b_wue, wde, b_wde, xeT, b_xeT, srecs) = st
            (aT, b_aT) = aTb
            for j in range(CT):
                (srec, b_srec) = srecs[j]
                (ysb, b_ysb) = ys_rot.next()
                row0 = ex * C + j * 128
                for n in range(2):
                    (bk, b_bk) = psum()
                    for f in range(4):
                        add("pe", lambda e, bk=bk, f=f, j=j, n=n, aT=aT, wde=wde: e.matmul(
                            bk[:, :], lhsT=aT[:, f, j * 128:(j + 1) * 128], rhs=wde[:, f, n * 512:(n + 1) * 512],
                            start=(f == 0), stop=(f == 3)), reads=[b_aT, b_wde], writes=[b_bk])
                    if n == 0:
                        add("act", lambda e, bk=bk, ysb=ysb, n=n, srec=srec: e.activation(
                            out=ysb[:, n * 512:(n + 1) * 512], in_=bk[:, :], func=AF.Copy, scale=srec[:, 1:2].bitcast(F32)),
                            reads=[b_bk, b_srec], writes=[b_ysb])
                    else:
                        add("dve", lambda e, bk=bk, ysb=ysb, n=n, srec=srec: e.tensor_scalar(
                            out=ysb[:, n * 512:(n + 1) * 512], in0=bk[:, :], scalar1=srec[:, 1:2].bitcast(F32), scalar2=None, op0=ALU.mult),
                            reads=[b_bk, b_srec], writes=[b_ysb])
                dma("sp", YS[row0:row0 + 128, :], ysb, [b_ysb], [B_YS[ex]])

        st_next = f_load(0)
        for ex in range(NEXP):
            st_cur = st_next
            aTb = f_gateup(st_cur)
            if ex + 1 < NEXP:
                st_next = f_load(ex + 1)
            f_down(ex, st_cur, aTb)
        A.release(m_f)

        m_g = A.mark()
        hg_rot = Rot("hg", [D], F32, 5)
        y_rot = Rot("yg", [2, D], F32, 5)
        og_rot = Rot("og", [D], F32, 3)
        small = Rot("ssg", [2], F32, 4)
        junk = A.alloc("junkg", [D], BF16)
        gfin_t, b_gfin = A.alloc("gfin", [D], F32)
        dma("sp", gfin_t, g_fin.partition_broadcast(128), [], [b_gfin])
        g_pref = {}

        def g_prefetch(gi):
            (hg, b_hg) = hg_rot.next()
            (yg, b_yg) = y_rot.next()
            dma("sp", hg, H[gi * 128:(gi + 1) * 128, :], [B_H[gi]], [b_hg])
            for sl in range(2):
                add("pool", lambda e, yg=yg, sl=sl, gi=gi: e.indirect_dma_start(
                    out=yg[:, sl, :], out_offset=None, in_=YS, in_offset=bass.IndirectOffsetOnAxis(ap=dest_all[:, gi, sl:sl + 1], axis=0),
                    bounds_check=PR["bc_slot"], oob_is_err=False),
                    reads=[b_dest] + B_YS, writes=[b_yg], dma=True)
            g_pref[gi] = (hg, b_hg, yg, b_yg)

        G_AHEAD = 3
        for gi in range(min(G_AHEAD, NTT)):
            g_prefetch(gi)
        for gi in range(NTT):
            if gi + G_AHEAD < NTT:
                g_prefetch(gi + G_AHEAD)
            (hg, b_hg, yg, b_yg) = g_pref.pop(gi)
            (og, b_og) = og_rot.next()
            add("dve", lambda e, hg=hg, yg=yg: e.tensor_tensor(out=hg, in0=hg, in1=yg[:, 0, :], op=ALU.add), reads=[b_hg, b_yg], writes=[b_hg])
            add("dve", lambda e, hg=hg, yg=yg: e.tensor_tensor(out=hg, in0=hg, in1=yg[:, 1, :], op=ALU.add), reads=[b_hg, b_yg], writes=[b_hg])
            (ss, b_ss) = small.next()
            (jk, b_jk) = junk
            add("pool", lambda e, ss=ss: e.memset(ss, 0.0), writes=[b_ss])
            add("act", lambda e, ss=ss, hg=hg, jk=jk: e.activation(out=jk, in_=hg, func=AF.Square, accum_out=ss[:, 0:1]),
                reads=[b_hg], writes=[b_jk, b_ss])
            add("act", lambda e, ss=ss: e.activation(out=ss[:, 1:2], in_=ss[:, 0:1], func=AF.Ln, bias=eps_rms[:, 0:1], scale=1.0 / D),
                reads=[b_ss, b_eps], writes=[b_ss])
            add("act", lambda e, ss=ss: e.activation(out=ss[:, 0:1], in_=ss[:, 1:2], func=AF.Exp, scale=-0.5), reads=[b_ss], writes=[b_ss])
            add("dve", lambda e, ss=ss, hg=hg, og=og: e.scalar_tensor_tensor(out=og, in0=hg, scalar=ss[:, 0:1], in1=gfin_t, op0=ALU.mult, op1=ALU.mult),
                reads=[b_hg, b_ss, b_gfin], writes=[b_og])
            dma("sp", out[gi * 128:(gi + 1) * 128, :], og, [b_og], [B_OUT])
        A.release(m_g)
        if dev:
            dsl, b_dsl = A.alloc("dsl", [NSLOT // 128, 2], I32)
            dma("sp", dsl, SLOT[0:NSLOT, :].rearrange("(p a) c -> p a c", p=128), [B_SLOT], [b_dsl])
            dma("sp", dbg["slot"].rearrange("(p a) c -> p a c", p=128), dsl, [b_dsl], [B_OUT])
            dy_rot = Rot("dy", [D], F32, 2)
            dh_rot = Rot("dh", [D], BF16, 2)
            for i_ in range((NSLOT + 128) // 128):
                (dy, b_dy) = dy_rot.next()
                dma("sp", dy, YS[i_ * 128:(i_ + 1) * 128, :], B_YS, [b_dy])
                dma("sp", dbg["ys"][i_ * 128:(i_ + 1) * 128, :], dy, [b_dy], [B_OUT])
            for i_ in range((TOK + 128) // 128):
                (dh, b_dh) = dh_rot.next()
                dma("sp", dh, HN3[i_ * 128:(i_ + 1) * 128, :], [B_HN3], [b_dh])
                dma("sp", dbg["hn3"][i_ * 128:(i_ + 1) * 128, :], dh, [b_dh], [B_OUT])

        Sc.emit(block, esems, dsems)
        build.stats = dict(nops=Sc.nops, peak=A.peak)
    return nc


def _consts(C):
    p = np.arange(128)
    half = 32
    inv_freq = (10000.0 ** (-np.arange(half, dtype=np.float32) / half)).astype(np.float32)
    c_invf = inv_freq[p % 32].reshape(128, 1).astype(np.float32)
    kk = np.arange(128)[:, None]
    qq = np.arange(128)[None, :]
    c_mask = np.concatenate([(kk <= qq), (kk >= qq)], axis=1).astype(np.float32)
    c_ltri = (kk < qq).astype(np.float32)
    c_ident = np.eye(128, dtype=np.float32)
    c_ec = np.broadcast_to((np.arange(32, dtype=np.float32) * C)[None, :], (128, 32)).copy()
    c_sel = np.zeros((65, 64), np.float32)
    c_sel[64, :] = 1.0
    c_iota = np.arange(128, dtype=np.float32).reshape(128, 1)
    return dict(c_invf=c_invf, c_mask=c_mask, c_ltri=c_ltri, c_ident=c_ident, c_ec=c_ec, c_sel=c_sel, c_iota=c_iota)


def _w_in_perm():
    cols = []
    for g in range(3):
        heads = range(4 * g, 4 * g + 4)
        for base in (0, 768):
            for part in (0, 32):
                for h in heads:
                    cols.extend(range(base + 64 * h + part, base + 64 * h + part + 32))
        cols.extend(range(1536 + 256 * g, 1536 + 256 * (g + 1)))
    cols.extend(range(2304, 5376))
    return np.asarray(cols)


def prep_weights(inp, C):
    f = lambda a: np.ascontiguousarray(np.asarray(a, dtype=np.float32))
    w = {}
    w["w_in"] = f(np.asarray(inp["w_in"])[0][:, _w_in_perm()])
    w["g_mix"] = f(inp["mix_norm_g"][0])
    w["g_x"] = f(inp["xattn_norm_g"][0])
    w["g_mem"] = f(inp["mem_norm_g"][0])
    w["g_moe"] = f(inp["moe_norm_g"][0])
    w["g_fin"] = f(inp["final_norm_g"])
    w["bgt"] = f(np.asarray(inp["b_gates"])[0].reshape(16, 128).T)
    w["ws_t"] = f(np.asarray(inp["w_spatial"])[0].transpose(0, 2, 1))
    w["bsp"] = f(np.asarray(inp["b_spatial"])[0].reshape(512))
    w["vgam"] = f(inp["v_norm_g"][0])
    w["vbet_t"] = f(np.asarray(inp["v_norm_b"])[0].reshape(4, 128).T)
    w["w_oa"] = f(inp["w_out_a"][0])
    w["w_ob"] = f(inp["w_out_b"][0])
    w["w_o"] = f(inp["w_out"][0])
    w["w_qx"] = f(inp["w_q_x"][0])
    w["w_kvx"] = f(inp["w_kv_x"][0])
    w["w_ox"] = f(inp["w_o_x"][0])
    w["w_r"] = f(np.concatenate([np.asarray(inp["w_router_grp"])[0], np.asarray(inp["w_router_exp"])[0]], axis=1))
    w["b_r"] = f(np.concatenate([np.asarray(inp["b_router_grp"])[0], np.asarray(inp["b_router_exp"])[0]], axis=0))
    w["w_ge"] = f(inp["w_gate_e"][0])
    w["w_ue"] = f(inp["w_up_e"][0])
    w["w_de"] = f(inp["w_down_e"][0])
    w.update(_consts(C))
    return w


def run(inp, n_cores, NB, S, C, dev=False):
    nc = build(NB, S, C, dev=dev)
    w = prep_weights(inp, C)
    x = np.asarray(inp["x"], dtype=np.float32)
    mem = np.asarray(inp["mem"], dtype=np.float32)
    pos = np.asarray(inp["positions"], dtype=np.int32)
    in_maps = []
    for c in range(n_cores):
        m = dict(w)
        m["x"] = np.ascontiguousarray(x[c * NB:(c + 1) * NB])
        m["mem"] = np.ascontiguousarray(mem[c * NB:(c + 1) * NB])
        m["pos"] = np.ascontiguousarray(pos[c * NB:(c + 1) * NB])
        in_maps.append(m)
    res = run_bass_kernel_spmd(nc, in_maps, core_ids=list(range(n_cores)))
    outs = [r["out"].reshape(NB, S, D) for r in res.results]
    try:
        print("[kernel] max routed rows per (core, expert):", [int(r["cnt"][0].max()) for r in res.results], "capacity", C, flush=True)
    except Exception:
        pass
    full = np.concatenate(outs, axis=0).astype(np.float32)
    if dev:
        return full, res.results
    return full


def kernel(**inputs):
    return run(inputs, n_cores=8, NB=2, S=4096, C=1024)
```

```python
import math
from contextlib import ExitStack

import numpy as np
import concourse.bass as bass
import concourse.mybir as mybir
from concourse.bass_utils import run_bass_kernel_spmd

F32 = mybir.dt.float32
BF16 = mybir.dt.bfloat16
I32 = mybir.dt.int32
ALU = mybir.AluOpType
AF = mybir.ActivationFunctionType
AX = mybir.AxisListType

D = 1024
KC = 8
NMEM = 256
NEXP = 32
FF = 512
RMS_EPS = 1e-6
LN_EPS = 1e-5
TWO_PI = 2.0 * math.pi
CW1 = 6.28125
CW2 = TWO_PI - CW1
PI_SAFE = 3.1415925

ENGS = ("pe", "act", "dve", "pool", "sp")


class Buf:
    __slots__ = ("name", "w", "r")

    def __init__(self, name):
        self.name = name
        self.w = None
        self.r = []


class Op:
    __slots__ = ("eng", "fn", "dma", "deps", "signal", "count", "sem", "semval", "prev_on_sem")

    def __init__(self, eng, fn, dma):
        self.eng = eng
        self.fn = fn
        self.dma = dma
        self.deps = []
        self.signal = False
        self.count = 0
        self.sem = None
        self.semval = 0
        self.prev_on_sem = None


class Sched:
    def __init__(self, n_dma_sems=40):
        self.ops = {e: [] for e in ENGS}
        self.n_dma_sems = n_dma_sems
        self.dma_rr = 0
        self.dma_rr_sw = 0
        self.dma_last = [None] * n_dma_sems
        self.dma_val = [0] * n_dma_sems
        self.nops = 0
        self.pool_regs = {}
        self.pool_reg_handles = {}

    def add(self, eng, fn, reads=(), writes=(), dma=False):
        op = Op(eng, fn, dma)
        self.nops += 1
        deps = {}
        for b in reads:
            if b.w is not None:
                deps[id(b.w)] = (b.w, "raw")
        for b in writes:
            if b.w is not None and id(b.w) not in deps:
                deps[id(b.w)] = (b.w, "waw")
            for r in b.r:
                if id(r) not in deps:
                    deps[id(r)] = (r, "war")
        for b in reads:
            b.r.append(op)
        for b in writes:
            b.w = op
            b.r = []
        for d, kind in deps.values():
            if d is op:
                continue
            if d.dma:
                op.deps.append(d)
            elif d.eng != eng:
                op.deps.append(d)
                d.signal = True
            elif kind == "raw" and eng != "pe":
                op.deps.append(d)
                d.signal = True
        if dma:
            n_sw = self.n_dma_sems // 3
            if eng == "pool":
                s = self.n_dma_sems - n_sw + (self.dma_rr_sw % n_sw)
                self.dma_rr_sw += 1
            else:
                s = self.dma_rr % (self.n_dma_sems - n_sw)
                self.dma_rr += 1
            op.sem = s
            op.prev_on_sem = self.dma_last[s]
            self.dma_val[s] += 16
            op.semval = self.dma_val[s]
            self.dma_last[s] = op
        self.ops[eng].append(op)
        return op

    def emit(self, block, esems, dsems):
        for e in ENGS:
            c = 0
            for op in self.ops[e]:
                if not op.dma and op.signal:
                    c += 1
                    op.count = c
        sched = self

        def run(e, eng):
            seen = {}
            for op in sched.ops[e]:
                best = {}
                if op.dma and op.prev_on_sem is not None:
                    p = op.prev_on_sem
                    best[("d", p.sem)] = p.semval
                for d in op.deps:
                    k = ("d", d.sem) if d.dma else ("e", d.eng)
                    v = d.semval if d.dma else d.count
                    if v > best.get(k, 0):
                        best[k] = v
                for k, v in best.items():
                    if seen.get(k, 0) >= v:
                        continue
                    seen[k] = v
                    eng.wait_ge(dsems[k[1]] if k[0] == "d" else esems[k[1]], v)
                ins = op.fn(eng)
                if op.dma:
                    ins.then_inc(dsems[op.sem], 16)
                elif op.signal:
                    ins.then_inc(esems[e], 1)

        @block.tensor
        def _(eng):
            run("pe", eng)

        @block.scalar
        def _(eng):
            run("act", eng)

        @block.vector
        def _(eng):
            run("dve", eng)

        @block.gpsimd
        def _(eng):
            for name_, val_ in sched.pool_regs.items():
                r_ = eng.alloc_register(name_)
                eng.reg_mov(r_, val_)
                sched.pool_reg_handles[name_] = r_
            run("pool", eng)

        @block.sync
        def _(eng):
            run("sp", eng)
            for s_ in range(sched.n_dma_sems):
                if sched.dma_val[s_] > 0:
                    eng.wait_ge(dsems[s_], sched.dma_val[s_])


class Arena:
    def __init__(self, base_ap, nbytes):
        self.base = base_ap
        self.cap = nbytes
        self.top = 0
        self.hi = nbytes
        self.live = []
        self.freed = []
        self.peak = 0

    def alloc(self, name, free_shape, dtype, nbufs=1, parts=128, top=False):
        esz = 2 if dtype == BF16 else 4
        n = int(np.prod(free_shape))
        nbytes = (n * esz + 63) // 64 * 64
        if top:
            end = self.hi
            start = end - nbytes
            assert start >= self.top, f"SBUF arena overflow (top) allocating {name}"
            self.hi = start
        else:
            start = self.top
            end = start + nbytes
            assert end <= self.hi, f"SBUF arena overflow allocating {name}: {end} > {self.hi}"
            self.top = end
        self.peak = max(self.peak, self.top + (self.cap - self.hi))
        v = self.base[0:parts, start // 2:start // 2 + n * esz // 2]
        if dtype != BF16:
            v = v.bitcast(dtype)
        if len(free_shape) == 2:
            v = v.rearrange("p (a b) -> p a b", a=free_shape[0])
        elif len(free_shape) == 3:
            v = v.rearrange("p (a b c) -> p a b c", a=free_shape[0], b=free_shape[1])
        hz = []
        for (s, e, ops) in self.freed:
            if s < end and e > start:
                hz.extend(ops)
        bufs = []
        for i in range(nbufs):
            b = Buf(f"{name}{i}")
            b.r = list(hz)
            bufs.append(b)
        self.live.append((start, end, bufs))
        return (v, bufs[0]) if nbufs == 1 else (v, bufs)

    def mark(self):
        return self.top

    def release_top(self):
        keep = []
        for (s, e, bufs) in self.live:
            if s >= self.hi:
                ops = []
                for b in bufs:
                    if b.w is not None:
                        ops.append(b.w)
                    ops.extend(b.r)
                self.freed.append((s, e, ops))
            else:
                keep.append((s, e, bufs))
        self.live = keep
        self.hi = self.cap

    def release(self, mark):
        keep = []
        for (s, e, bufs) in self.live:
            if s >= mark and e <= self.hi:
                ops = []
                for b in bufs:
                    if b.w is not None:
                        ops.append(b.w)
                    ops.extend(b.r)
                self.freed.append((s, e, ops))
            else:
                keep.append((s, e, bufs))
        self.live = keep
        self.top = mark


def build(NB, S, C, dev=False):
    NT = S // 128
    NBK = S // 512
    TOK = NB * S
    NTT = TOK // 128
    CT = C // 128
    NSLOT = NEXP * C
    DIL = (1, 4, 16)

    nc = bass.Bass("TRN2", target_bir_lowering=False)

    def din(name, shape, dt=F32):
        return nc.dram_tensor(name, shape, dt, kind="ExternalInput").ap()

    x = din("x", [NB, S, D])
    mem = din("mem", [NB, NMEM, D])
    pos = din("pos", [NB, S], I32)
    w_in = din("w_in", [D, 5376])
    g_mix = din("g_mix", [D])
    g_x = din("g_x", [D])
    g_mem = din("g_mem", [D])
    g_moe = din("g_moe", [D])
    g_fin = din("g_fin", [D])
    bgt = din("bgt", [128, 16])
    ws_t = din("ws_t", [4, 128, 128])
    bsp = din("bsp", [512])
    vgam = din("vgam", [512])
    vbet_t = din("vbet_t", [128, 4])
    w_oa = din("w_oa", [256, D])
    w_ob = din("w_ob", [512, D])
    w_o = din("w_o", [D, D])
    w_qx = din("w_qx", [D, D])
    w_kvx = din("w_kvx", [D, 2 * D])
    w_ox = din("w_ox", [D, D])
    w_r = din("w_r", [D, 36])
    b_r = din("b_r", [36])
    w_ge = din("w_ge", [NEXP, D, FF])
    w_ue = din("w_ue", [NEXP, D, FF])
    w_de = din("w_de", [NEXP, FF, D])
    c_invf = din("c_invf", [128, 1])
    c_mask = din("c_mask", [128, 256])
    c_ltri = din("c_ltri", [128, 128])
    c_ident = din("c_ident", [128, 128])
    c_ec = din("c_ec", [128, 32])
    c_sel = din("c_sel", [65, 64])
    c_iota = din("c_iota", [128, 1])

    out = nc.dram_tensor("out", [TOK, D], F32, kind="ExternalOutput").ap()
    cnt_out = nc.dram_tensor("cnt", [128, 32], F32, kind="ExternalOutput").ap()
    dbg = {}
    if dev:
        dbg["ya"] = nc.dram_tensor("dbg_ya", [NB, 64, 4, S], F32, kind="ExternalOutput").ap()
        dbg["yb"] = nc.dram_tensor("dbg_yb", [NB, 128, 4, S], F32, kind="ExternalOutput").ap()
        dbg["h1"] = nc.dram_tensor("dbg_h1", [TOK, D], F32, kind="ExternalOutput").ap()
        dbg["h2"] = nc.dram_tensor("dbg_h2", [TOK, D], F32, kind="ExternalOutput").ap()
        dbg["dest"] = nc.dram_tensor("dbg_dest", [128, NTT, 2], I32, kind="ExternalOutput").ap()
        dbg["slot"] = nc.dram_tensor("dbg_slot", [NSLOT, 2], I32, kind="ExternalOutput").ap()
        dbg["ys"] = nc.dram_tensor("dbg_ys", [NSLOT + 128, D], F32, kind="ExternalOutput").ap()
        dbg["hn3"] = nc.dram_tensor("dbg_hn3", [TOK + 128, D], BF16, kind="ExternalOutput").ap()

    COS = nc.dram_tensor("scr_cos", [NB, 128, S], F32, kind="Internal").ap()
    SIN = nc.dram_tensor("scr_sin", [NB, 128, S], F32, kind="Internal").ap()
    H = nc.dram_tensor("scr_h", [TOK, D], F32, kind="Internal").ap()
    XN = nc.dram_tensor("scr_xn", [NB, 128, KC, S], BF16, kind="Internal").ap()
    HN3 = nc.dram_tensor("scr_hn3", [TOK + 128, D], BF16, kind="Internal").ap()
    SLOT = nc.dram_tensor("scr_slot", [NSLOT + 128, 2], I32, kind="Internal").ap()
    YS = nc.dram_tensor("scr_ys", [NSLOT + 128, D], F32, kind="Internal").ap()

    ARENA_BYTES = 204 * 1024
    es = ExitStack()
    with es:
        arena_t = es.enter_context(nc.sbuf_tensor("arena", [128, ARENA_BYTES // 2], BF16))
        banks = [es.enter_context(nc.psum_tensor(f"bank{i}", [128, 512], F32)) for i in range(8)]
        esems = {e: es.enter_context(nc.semaphore("es_" + e)) for e in ENGS}
        NDS = 40
        dsems = [es.enter_context(nc.semaphore(f"ds{i}")) for i in range(NDS)]
        block = es.enter_context(nc.Block())
        Sc = Sched(NDS)
        Sc.pool_regs = {"bc_slot": NSLOT + 127, "bc_tok": TOK + 127}
        PR = Sc.pool_reg_handles
        A = Arena(arena_t[:, :], ARENA_BYTES)
        real_add = Sc.add
        cur_add = [Sc.add]

        def add(*a_, **k_):
            return cur_add[0](*a_, **k_)

        def record(fn_):
            lst = []
            prev_ = cur_add[0]
            cur_add[0] = make_recorder(lst)
            fn_()
            cur_add[0] = prev_
            return lst

        def emit_interleaved(lists):
            n_ = max(len(l_) for l_ in lists)
            for i_ in range(n_):
                for l_ in lists:
                    if i_ < len(l_):
                        cur_add[0](*l_[i_])

        def emit_merged(la, lb):
            na, nb_ = len(la), len(lb)
            ia = 0
            for ib in range(nb_):
                cur_add[0](*lb[ib])
                want = ((ib + 1) * na) // max(nb_, 1)
                while ia < want:
                    cur_add[0](*la[ia])
                    ia += 1
            while ia < na:
                cur_add[0](*la[ia])
                ia += 1

        def make_recorder(lst):
            def rec_(eng, fn, reads=(), writes=(), dma=False):
                lst.append((eng, fn, tuple(reads), tuple(writes), dma))
            return rec_

        bank_bufs = [Buf(f"bank{i}") for i in range(8)]
        bank_rr = [0]

        psum_pool = [None]
        pool_rr = {"a": 0, "b": 0}
        pool_banks = {"a": [0, 1, 2, 3], "b": [4, 5, 6, 7]}

        def psum():
            if psum_pool[0] is not None:
                pl = pool_banks[psum_pool[0]]
                i = pl[pool_rr[psum_pool[0]] % len(pl)]
                pool_rr[psum_pool[0]] += 1
                return banks[i], bank_bufs[i]
            i = bank_rr[0]
            bank_rr[0] = (i + 1) % 8
            return banks[i], bank_bufs[i]

        B_COS = [Buf(f"cos{b}") for b in range(NB)]
        B_H = [Buf(f"H{i}") for i in range(NTT)]
        B_HN3 = Buf("HN3")
        B_XN = [Buf(f"XN{k}") for k in range(NBK)]
        B_SLOT = Buf("SLOT")
        B_YS = [Buf(f"YS{e}") for e in range(NEXP)]
        B_OUT = Buf("OUT")

        class Rot:
            def __init__(self, name, free_shape, dtype, n, parts=128):
                self.items = [A.alloc(f"{name}{i}", free_shape, dtype, parts=parts) for i in range(n)]
                self.i = 0

            def next(self):
                it = self.items[self.i]
                self.i = (self.i + 1) % len(self.items)
                return it

        def dma(eng, out_ap, in_ap, reads, writes):
            return add(eng, lambda e: e.dma_start(out=out_ap, in_=in_ap), reads=reads, writes=writes, dma=True)

        ident_b, b_ident_b = A.alloc("ident_b", [128], BF16)
        ident_f, b_ident_f = A.alloc("ident_f", [128], F32)
        ones_b, b_ones = A.alloc("ones_b", [128], BF16)
        mask_b, b_mask = A.alloc("mask_b", [256], BF16)
        mask4, b_mask4 = A.alloc("mask4", [4, 256], BF16)
        ltri_b, b_ltri = A.alloc("ltri_b", [128], BF16)
        invf, b_invf = A.alloc("invf", [1], F32)
        ec_t, b_ec = A.alloc("ec", [32], F32)
        sel_f, b_sel = A.alloc("sel", [64], BF16)
        iota_f, b_iota = A.alloc("iota", [1], F32)
        bgt_t, b_bgt = A.alloc("bgt", [16], F32)
        wr_t, b_wr = A.alloc("wr", [KC, 36], F32)
        br_t, b_br = A.alloc("br", [36], F32)
        base_t, b_base = A.alloc("base", [32], F32)
        dest_all, b_dest = A.alloc("dest_all", [NTT, 2], I32)
        eps_rms, b_eps = A.alloc("eps_rms", [1], F32)
        eps_ln, b_epsln = A.alloc("eps_ln", [1], F32)

        dma("pool", ident_b, c_ident, [], [b_ident_b])
        dma("sp", ident_f, c_ident, [], [b_ident_f])
        dma("pool", mask_b, c_mask, [], [b_mask])
        dma("pool", ltri_b, c_ltri, [], [b_ltri])
        dma("sp", invf, c_invf, [], [b_invf])
        dma("sp", ec_t, c_ec, [], [b_ec])
        dma("pool", sel_f[0:65, :], c_sel, [], [b_sel])
        dma("sp", iota_f, c_iota, [], [b_iota])
        dma("sp", bgt_t, bgt, [], [b_bgt])
        dma("sp", wr_t, w_r.rearrange("(kc p) n -> p kc n", p=128), [], [b_wr])
        dma("sp", br_t, b_r.partition_broadcast(128), [], [b_br])
        add("pool", lambda e: e.memset(ones_b, 1.0), writes=[b_ones])
        for hh_ in range(4):
            add("pool", lambda e, hh_=hh_: e.tensor_copy(out=mask4[:, hh_, :], in_=mask_b), reads=[b_mask], writes=[b_mask4])
        add("pool", lambda e: e.memset(base_t, 0.0), writes=[b_base])
        add("pool", lambda e: e.memset(eps_rms, RMS_EPS), writes=[b_eps])
        add("pool", lambda e: e.memset(eps_ln, LN_EPS), writes=[b_epsln])

        m0 = A.mark()
        zrow, b_zrow = A.alloc("zrow", [D], BF16)
        slot0, b_slot0 = A.alloc("slot0", [NSLOT // 128, 2], I32)
        add("pool", lambda e: e.memset(zrow, 0.0), writes=[b_zrow])
        dma("sp", HN3[TOK:TOK + 128, :], zrow, [b_zrow], [B_HN3])
        zrowf, b_zrowf = A.alloc("zrowf", [D], F32)
        add("pool", lambda e: e.memset(zrowf, 0.0), writes=[b_zrowf])
        dma("sp", YS[NSLOT:NSLOT + 128, :], zrowf, [b_zrowf], [B_YS[0]])
        add("pool", lambda e: e.memset(slot0[:, :, 0:1], TOK), writes=[b_slot0])
        add("pool", lambda e: e.memset(slot0[:, :, 1:2], 0), writes=[b_slot0])
        dma("sp", SLOT[0:NSLOT, :].rearrange("(p a) c -> p a c", p=128), slot0, [b_slot0], [B_SLOT])
        A.release(m0)

        def rms_tile(src_ap, b_src, g_tile, b_g, dst_bf, b_dst, small, junk, dst_f32=None, b_dst32=None):
            (ss, b_ss) = small.next()
            (jk, b_jk) = junk
            add("pool", lambda e: e.memset(ss, 0.0), writes=[b_ss])
            add("act", lambda e: e.activation(out=jk, in_=src_ap, func=AF.Square, accum_out=ss[:, 0:1]),
                reads=[b_src], writes=[b_jk, b_ss])
            add("act", lambda e: e.activation(out=ss[:, 1:2], in_=ss[:, 0:1], func=AF.Ln, bias=eps_rms[:, 0:1],
                                              scale=1.0 / D), reads=[b_ss, b_eps], writes=[b_ss])
            add("act", lambda e: e.activation(out=ss[:, 0:1], in_=ss[:, 1:2], func=AF.Exp, scale=-0.5), reads=[b_ss], writes=[b_ss])
            if dst_f32 is not None:
                add("dve", lambda e: e.scalar_tensor_tensor(out=dst_f32, in0=src_ap, scalar=ss[:, 0:1], in1=g_tile,
                                                            op0=ALU.mult, op1=ALU.mult),
                    reads=[b_src, b_ss, b_g], writes=[b_dst32])
                add("act", lambda e: e.copy(out=dst_bf, in_=dst_f32), reads=[b_dst32], writes=[b_dst])
            else:
                add("dve", lambda e: e.scalar_tensor_tensor(out=dst_bf, in0=src_ap, scalar=ss[:, 0:1], in1=g_tile,
                                                            op0=ALU.mult, op1=ALU.mult),
                    reads=[b_src, b_ss, b_g], writes=[b_dst])

        def transpose_to(src_bf, b_src, dst_ap3, b_dst, eng="act"):
            (bk, b_bk) = psum()
            pv = bk[:, :].bitcast(BF16)
            for c in range(KC):
                add("pe", lambda e, c=c: e.transpose(out=pv[:, c * 128:(c + 1) * 128], in_=src_bf[:, c * 128:(c + 1) * 128],
                                                     identity=ident_b), reads=[b_src, b_ident_b], writes=[b_bk])
            pv3 = pv.rearrange("p (c t) -> p c t", c=KC)
            if eng == "act":
                add("act", lambda e: e.copy(out=dst_ap3, in_=pv3), reads=[b_bk], writes=[b_dst])
            else:
                add("dve", lambda e: e.tensor_copy(out=dst_ap3, in_=pv3), reads=[b_bk], writes=[b_dst])

        def gelu_from_psum(bk, b_bk, dst, b_dst, tmp_rot, width):
            (t1, b_t1) = tmp_rot.next()
            (t2, b_t2) = tmp_rot.next()
            pz = bk[:, 0:width]
            add("act", lambda e: e.activation(out=t1[:, 0:width], in_=pz, func=AF.Square), reads=[b_bk], writes=[b_t1])
            add("dve", lambda e: e.tensor_scalar(out=t1[:, 0:width], in0=t1[:, 0:width], scalar1=0.044715, scalar2=1.0,
                                                 op0=ALU.mult, op1=ALU.add), reads=[b_t1], writes=[b_t1])
            add("dve", lambda e: e.tensor_tensor(out=t2[:, 0:width], in0=pz, in1=t1[:, 0:width], op=ALU.mult),
                reads=[b_bk, b_t1], writes=[b_t2])
            add("act", lambda e: e.activation(out=t2[:, 0:width], in_=t2[:, 0:width], func=AF.Sigmoid,
                                              scale=1.5957691216057308), reads=[b_t2], writes=[b_t2])
            add("dve", lambda e: e.tensor_tensor(out=dst, in0=pz, in1=t2[:, 0:width], op=ALU.mult),
                reads=[b_bk, b_t2], writes=[b_dst])

        wsT, b_wsT = A.alloc("wsT", [4, 128], BF16)
        B2, b_B2 = A.alloc("B2", [4, 128], F32)
        gamrow, b_gamrow = A.alloc("gamrow", [512], F32)
        m0 = A.mark()
        bspb, b_bspb = A.alloc("bspb", [4, 128], F32)
        bet_t, b_bet = A.alloc("bet", [4], F32)
        dma("pool", wsT, ws_t.rearrange("g s t -> s g t"), [], [b_wsT])
        dma("sp", bspb, bsp.partition_broadcast(128).rearrange("p (g t) -> p g t", g=4), [], [b_bspb])
        dma("sp", bet_t, vbet_t, [], [b_bet])
        dma("sp", gamrow, vgam.partition_broadcast(128), [], [b_gamrow])
        add("dve", lambda e: e.tensor_tensor(out=wsT, in0=wsT, in1=mask_b[:, 0:128].unsqueeze(1).to_broadcast([128, 4, 128]),
                                             op=ALU.mult), reads=[b_wsT, b_mask], writes=[b_wsT])
        (bk, b_bk) = psum()
        add("pe", lambda e: e.matmul(bk[:, :], lhsT=ones_b, rhs=wsT.rearrange("p g t -> p (g t)"), start=True, stop=True),
            reads=[b_ones, b_wsT], writes=[b_bk])
        for g in range(4):
            add("dve", lambda e, g=g: e.scalar_tensor_tensor(out=B2[:, g, :], in0=bk[:, g * 128:(g + 1) * 128],
                                                             scalar=bet_t[:, g:g + 1], in1=bspb[:, g, :],
                                                             op0=ALU.mult, op1=ALU.add),
                reads=[b_bk, b_bet, b_bspb], writes=[b_B2])
        A.release(m0)

        persist_mark = A.mark()

        for b in range(NB):
            m_r = A.mark()
            posi, b_posi = A.alloc("posi", [S], I32)
            dma("sp", posi, pos[b].partition_broadcast(128), [], [b_posi])
            rt = Rot("rt", [512], F32, 8)
            ri = Rot("ri", [512], I32, 8)
            ang_rot = Rot("ang", [512], F32, 4)
            tb_rot = Rot("tb", [512], F32, 8)
            def r_block(k, b=b):
                blk = slice(k * 512, (k + 1) * 512)
                (ang, b_ang) = ang_rot.next()
                add("dve", lambda e, ang=ang, blk=blk: e.tensor_copy(out=ang, in_=posi[:, blk]), reads=[b_posi], writes=[b_ang])
                add("dve", lambda e, ang=ang: e.tensor_scalar(out=ang, in0=ang, scalar1=invf[:, 0:1], scalar2=None, op0=ALU.mult),
                    reads=[b_ang, b_invf], writes=[b_ang])
                for (dst, shift) in ((SIN, 0.0), (COS, 0.5 * math.pi)):
                    (u, b_u) = rt.next()
                    (ki, b_ki) = ri.next()
                    (tb, b_tb) = tb_rot.next()
                    add("dve", lambda e, u=u, ang=ang, shift=shift: e.tensor_scalar(
                        out=u, in0=ang, scalar1=shift, scalar2=1.0 / TWO_PI, op0=ALU.add, op1=ALU.mult),
                        reads=[b_ang], writes=[b_u])
                    add("dve", lambda e, u=u, ki=ki: e.tensor_copy(out=ki, in_=u), reads=[b_u], writes=[b_ki])
                    add("dve", lambda e, u=u, ki=ki: e.tensor_copy(out=u, in_=ki), reads=[b_ki], writes=[b_u])
                    add("dve", lambda e, u=u, ang=ang, tb=tb: e.scalar_tensor_tensor(
                        out=tb, in0=u, scalar=-CW1, in1=ang, op0=ALU.mult, op1=ALU.add), reads=[b_u, b_ang], writes=[b_tb])
                    add("dve", lambda e, u=u, tb=tb: e.scalar_tensor_tensor(
                        out=tb, in0=u, scalar=-CW2, in1=tb, op0=ALU.mult, op1=ALU.add), reads=[b_u, b_tb], writes=[b_tb])
                    add("dve", lambda e, tb=tb, shift=shift: e.tensor_scalar(
                        out=tb, in0=tb, scalar1=shift, scalar2=PI_SAFE, op0=ALU.add, op1=ALU.min), reads=[b_tb], writes=[b_tb])
                    add("dve", lambda e, tb=tb: e.tensor_scalar_max(out=tb, in0=tb, scalar1=-PI_SAFE), reads=[b_tb], writes=[b_tb])
                    add("act", lambda e, tb=tb: e.activation(out=tb, in_=tb, func=AF.Sin), reads=[b_tb], writes=[b_tb])
                    dma("sp", dst[b, :, blk], tb, [b_tb], [B_COS[b]])
            for k0 in range(0, NBK, 4):
                emit_interleaved([record(lambda k=k: r_block(k)) for k in range(k0, min(NBK, k0 + 4))])
            A.release(m_r)

        for b in range(NB):
            A.release(persist_mark)

            xnT, b_xnT = A.alloc("xnT", [KC, S], BF16, nbufs=NBK)
            m_a = A.mark()
            xt_rot = Rot("xt", [D], F32, 4)
            xnb_rot = Rot("xnb", [D], BF16, 4)
            small = Rot("ss", [2], F32, 8)
            junk = A.alloc("junk", [D], BF16)
            gmix_t, b_gmix = A.alloc("gmix", [D], F32)
            dma("sp", gmix_t, g_mix.partition_broadcast(128), [], [b_gmix])
            def a_tile(i, b=b):
                (xt, b_xt) = xt_rot.next()
                (xnb, b_xnb) = xnb_rot.next()
                dma("sp", xt, x[b, i * 128:(i + 1) * 128, :], [], [b_xt])
                rms_tile(xt, b_xt, gmix_t, b_gmix, xnb, b_xnb, small, junk)
                transpose_to(xnb, b_xnb, xnT[:, :, i * 128:(i + 1) * 128], b_xnT[i // 4], eng="act" if i % 2 == 0 else "dve")

            for kk in range(NBK):
                emit_interleaved([record(lambda i=i: a_tile(i)) for i in range(kk * 4, kk * 4 + 4)])
                dma("sp", XN[b, :, :, kk * 512:(kk + 1) * 512], xnT[:, :, kk * 512:(kk + 1) * 512], [b_xnT[kk]], [B_XN[kk]])
            A.release(m_a)

            acc, b_acc = A.alloc("acc", [4, S], BF16, parts=128)
            m_b = A.mark()
            qk, b_qk = A.alloc("qk", [4, S], BF16)
            wg_rot = Rot("wg", [KC, 768], BF16, 1)
            cs_rot = Rot("cs", [2, 512], F32, 2)
            ev_rot = Rot("ev", [512], F32, 4)
            rp_rot = Rot("rp", [512], F32, 4)
            pe_rot = Rot("pexp", [4, 256], BF16, 4)
            v_rot = [A.alloc(f"vv{i}", [4, 65], BF16) for i in range(4)]
            for (vv, b_vv) in v_rot:
                add("pool", lambda e, vv=vv: e.memset(vv, 1.0), writes=[b_vv])
            for g in range(3):
                d = DIL[g]
                L = S // d
                nb = L // 128
                (wg, b_wg) = wg_rot.next()
                dma("pool", wg, w_in.rearrange("(kc p) n -> p kc n", p=128)[:, :, g * 768:(g + 1) * 768], [], [b_wg])
                for k in range(NBK):
                    blk = slice(k * 512, (k + 1) * 512)
                    (cs, b_cs) = cs_rot.next()
                    dma("sp", cs[:, 0, :], COS[b, :, blk], [B_COS[b]], [b_cs])
                    dma("sp", cs[:, 1, :], SIN[b, :, blk], [B_COS[b]], [b_cs])
                    for which in range(2):
                        evs = []
                        for part in range(2):
                            col = (which * 2 + part) * 128
                            (bk, b_bk) = psum()
                            for kc in range(KC):
                                add("pe", lambda e, bk=bk, kc=kc, col=col, blk=blk, wg=wg: e.matmul(
                                    bk[:, :], lhsT=wg[:, kc, col:col + 128], rhs=xnT[:, kc, blk],
                                    start=(kc == 0), stop=(kc == KC - 1)), reads=[b_wg, b_xnT[k]], writes=[b_bk])
                            (ev, b_ev) = ev_rot.next()
                            add("act", lambda e, ev=ev, bk=bk: e.copy(out=ev, in_=bk[:, :]), reads=[b_bk], writes=[b_ev])
                            evs.append((ev, b_ev))
                        (eA, b_eA), (eB, b_eB) = evs
                        (t1, b_t1) = rp_rot.next()
                        (t2, b_t2) = rp_rot.next()
                        oA = qk[:, which * 2, blk]
                        oB = qk[:, which * 2 + 1, blk]
                        add("dve", lambda e, t1=t1, eA=eA, cs=cs: e.tensor_tensor(out=t1, in0=eA, in1=cs[:, 0, :], op=ALU.mult),
                            reads=[b_eA, b_cs], writes=[b_t1])
                        add("dve", lambda e, t2=t2, eB=eB, cs=cs: e.tensor_tensor(out=t2, in0=eB, in1=cs[:, 1, :], op=ALU.mult),
                            reads=[b_eB, b_cs], writes=[b_t2])
                        add("dve", lambda e, t1=t1, t2=t2, oA=oA: e.tensor_tensor(out=oA, in0=t1, in1=t2, op=ALU.subtract),
                            reads=[b_t1, b_t2], writes=[b_qk])
                        add("dve", lambda e, t1=t1, eB=eB, cs=cs: e.tensor_tensor(out=t1, in0=eB, in1=cs[:, 0, :], op=ALU.mult),
                            reads=[b_eB, b_cs], writes=[b_t1])
                        add("dve", lambda e, t2=t2, eA=eA, cs=cs: e.tensor_tensor(out=t2, in0=eA, in1=cs[:, 1, :], op=ALU.mult),
                            reads=[b_eA, b_cs], writes=[b_t2])
                        add("dve", lambda e, t1=t1, t2=t2, oB=oB: e.tensor_tensor(out=oB, in0=t1, in1=t2, op=ALU.add),
                            reads=[b_t1, b_t2], writes=[b_qk])
                vcount = [0]
                for r in range(d):
                    def tsl(jj, n=1, r=r):
                        st = r + d * 128 * jj
                        return slice(st, st + d * (128 * n - 1) + 1, d) if d > 1 else slice(st, st + 128 * n)
                    items = []

                    def emit_pv(j, items=items, tsl=tsl):
                        (vv, b_vv, px, b_px) = items[j]
                        (bk3, b_bk3) = psum()
                        for hh in range(4):
                            o_ap = bk3[0:65, hh * 128:(hh + 1) * 128]
                            if j > 0:
                                (pvv, b_pvv, ppx, b_ppx) = items[j - 1]
                                add("pe", lambda e, o_ap=o_ap, pvv=pvv, ppx=ppx, hh=hh: e.matmul(
                                    o_ap, lhsT=pvv[:, hh, :], rhs=ppx[:, hh, 128:256], start=True, stop=False),
                                    reads=[b_pvv, b_ppx], writes=[b_bk3])
                            add("pe", lambda e, o_ap=o_ap, vv=vv, px=px, hh=hh, first=(j == 0): e.matmul(
                                o_ap, lhsT=vv[:, hh, :], rhs=px[:, hh, 0:128], start=first, stop=True),
                                reads=[b_vv, b_px], writes=[b_bk3])
                        a_ap = acc[0:65, :, tsl(j)]
                        p_ap = bk3[0:65, :].rearrange("p (h q) -> p h q", h=4)
                        if g == 0:
                            add("dve", lambda e, a_ap=a_ap, p_ap=p_ap: e.tensor_copy(out=a_ap, in_=p_ap), reads=[b_bk3], writes=[b_acc])
                        else:
                            add("dve", lambda e, a_ap=a_ap, p_ap=p_ap: e.tensor_tensor(out=a_ap, in0=p_ap, in1=a_ap, op=ALU.add),
                                reads=[b_bk3, b_acc], writes=[b_acc])

                    for j in range(nb):
                        (vv, b_vv) = v_rot[vcount[0] % 4]
                        vcount[0] += 1
                        (bk, b_bk) = psum()
                        for kc in range(KC):
                            add("pe", lambda e, bk=bk, kc=kc, wg=wg, ts=tsl(j): e.matmul(
                                bk[:, 0:256], lhsT=xnT[:, kc, ts], rhs=wg[:, kc, 512:768],
                                start=(kc == 0), stop=(kc == KC - 1)), reads=[b_wg] + b_xnT, writes=[b_bk])
                        add("act", lambda e, vv=vv, bk=bk: e.copy(out=vv[:, :, 0:64], in_=bk[:, 0:256].rearrange("p (h d) -> p h d", h=4)),
                            reads=[b_bk], writes=[b_vv])
                        nq = 2 if j < nb - 1 else 1
                        (px, b_px) = pe_rot.next()
                        for hh in range(4):
                            (bk2, b_bk2) = psum()
                            for part in range(2):
                                add("pe", lambda e, bk2=bk2, hh=hh, part=part, ks=tsl(j), qs=tsl(j, nq), nq=nq: e.matmul(
                                    bk2[:, 0:128 * nq], lhsT=qk[32 * hh:32 * hh + 32, 2 + part, ks],
                                    rhs=qk[32 * hh:32 * hh + 32, part, qs], start=(part == 0), stop=(part == 1),
                                    tile_position=(32 * hh, 0)), reads=[b_qk], writes=[b_bk2])
                            add("act", lambda e, px=px, bk2=bk2, hh=hh, nq=nq: e.activation(
                                out=px[:, hh, 0:128 * nq], in_=bk2[:, 0:128 * nq], func=AF.Exp, scale=0.125),
                                reads=[b_bk2], writes=[b_px])
                        add("dve", lambda e, px=px, nq=nq: e.tensor_tensor(
                            out=px[:, :, 0:128 * nq], in0=px[:, :, 0:128 * nq],
                            in1=mask4[:, :, 0:128 * nq], op=ALU.mult),
                            reads=[b_px, b_mask4], writes=[b_px])
                        items.append((vv, b_vv, px, b_px))
                        if j >= 2:
                            emit_pv(j - 2)
                    if nb >= 2:
                        emit_pv(nb - 2)
                    emit_pv(nb - 1)
            A.release(m_b)
            yaT, b_yaT = A.alloc("yaT", [4, S], BF16, top=True)
            m_n = A.mark()
            rd_rot = Rot("rden", [512], F32, 2)
            for k in range(NBK):
                blk = slice(k * 512, (k + 1) * 512)
                for hh in range(4):
                    (bk, b_bk) = psum()
                    add("pe", lambda e, bk=bk, hh=hh, blk=blk: e.matmul(bk[0:64, :], lhsT=sel_f[0:65, :], rhs=acc[0:65, hh, blk],
                                                                     start=True, stop=True), reads=[b_sel, b_acc], writes=[b_bk])
                    (rd, b_rd) = rd_rot.next()
                    add("dve", lambda e, rd=rd, bk=bk: e.reciprocal(out=rd[0:64, :], in_=bk[0:64, :]), reads=[b_bk], writes=[b_rd])
                    add("dve", lambda e, rd=rd, hh=hh, blk=blk: e.tensor_tensor(out=yaT[0:64, hh, blk], in0=acc[0:64, hh, blk],
                                                                             in1=rd[0:64, :], op=ALU.mult),
                        reads=[b_acc, b_rd], writes=[b_yaT])
            if dev:
                (dd, b_dd) = A.alloc("dbgya", [4, S], F32)
                add("dve", lambda e, dd=dd: e.tensor_copy(out=dd[0:64], in_=yaT[0:64]), reads=[b_yaT], writes=[b_dd])
                dma("sp", dbg["ya"][b], dd[0:64], [b_dd], [B_OUT])
            A.release(m_n)
            A.release(persist_mark)

            ybT, b_ybT = A.alloc("ybT", [4, S], BF16, top=True)
            m_c = A.mark()
            xs_rot = Rot("xs", [KC, 512], BF16, 2)
            wuv, b_wuv = A.alloc("wuv", [KC, 1024], BF16)
            dma("pool", wuv, w_in.rearrange("(kc p) n -> p kc n", p=128)[:, :, 2304:3328], [], [b_wuv])
            uT_rot = Rot("uT", [4, 512], BF16, 2)
            gtmp = Rot("gtmp", [512], F32, 8)
            vg_rot = Rot("vg", [512], F32, 4)
            vh_rot = Rot("vh", [512], BF16, 4)
            st_rot = Rot("lnst", [4], F32, 8)
            ljunk = A.alloc("ljunk", [512], BF16)
            yt_rot = Rot("ytmp", [512], F32, 4)
            for k in range(NBK):
                blk = slice(k * 512, (k + 1) * 512)
                (uT, b_uT) = uT_rot.next()
                (xs, b_xs) = xs_rot.next()
                dma("sp", xs, XN[b, :, :, blk], [B_XN[k]], [b_xs])
                def c_uchunk(c, xs=xs, b_xs=b_xs, uT=uT, b_uT=b_uT):
                    (bk, b_bk) = psum()
                    for kc in range(KC):
                        add("pe", lambda e, bk=bk, kc=kc, c=c, xs=xs: e.matmul(
                            bk[:, :], lhsT=wuv[:, kc, c * 128:(c + 1) * 128], rhs=xs[:, kc, :],
                            start=(kc == 0), stop=(kc == KC - 1)), reads=[b_wuv, b_xs], writes=[b_bk])
                    gelu_from_psum(bk, b_bk, uT[:, c, :], b_uT, gtmp, 512)

                def c_tile(t, k=k, xs=xs, b_xs=b_xs, uT=uT, b_uT=b_uT):
                    tok = slice(k * 512 + t * 128, k * 512 + (t + 1) * 128)
                    (bk, b_bk) = psum()
                    for kc in range(KC):
                        add("pe", lambda e, bk=bk, kc=kc, t=t, xs=xs: e.matmul(
                            bk[:, :], lhsT=xs[:, kc, t * 128:(t + 1) * 128], rhs=wuv[:, kc, 512:1024],
                            start=(kc == 0), stop=(kc == KC - 1)), reads=[b_wuv, b_xs], writes=[b_bk])
                    (vg, b_vg) = vg_rot.next()
                    gelu_from_psum(bk, b_bk, vg, b_vg, gtmp, 512)
                    (st, b_st) = st_rot.next()
                    (lj, b_lj) = ljunk
                    add("pool", lambda e, st=st: e.memset(st, 0.0), writes=[b_st])
                    add("act", lambda e, st=st, vg=vg, lj=lj: e.activation(out=lj, in_=vg, func=AF.Copy, accum_out=st[:, 0:1]),
                        reads=[b_vg], writes=[b_lj, b_st])
                    add("act", lambda e, st=st, vg=vg, lj=lj: e.activation(out=lj, in_=vg, func=AF.Square, accum_out=st[:, 1:2]),
                        reads=[b_vg], writes=[b_lj, b_st])
                    add("dve", lambda e, st=st: e.tensor_scalar(out=st[:, 0:2], in0=st[:, 0:2], scalar1=1.0 / 512, scalar2=None, op0=ALU.mult),
                        reads=[b_st], writes=[b_st])
                    add("dve", lambda e, st=st: e.tensor_tensor(out=st[:, 2:3], in0=st[:, 0:1], in1=st[:, 0:1], op=ALU.mult),
                        reads=[b_st], writes=[b_st])
                    add("dve", lambda e, st=st: e.tensor_tensor(out=st[:, 1:2], in0=st[:, 1:2], in1=st[:, 2:3], op=ALU.subtract),
                        reads=[b_st], writes=[b_st])
                    add("act", lambda e, st=st: e.activation(out=st[:, 1:2], in_=st[:, 1:2], func=AF.Sqrt, bias=eps_ln[:, 0:1], scale=1.0),
                        reads=[b_st, b_epsln], writes=[b_st])
                    add("dve", lambda e, st=st: e.reciprocal(out=st[:, 1:2], in_=st[:, 1:2]), reads=[b_st], writes=[b_st])
                    add("dve", lambda e, st=st, vg=vg: e.tensor_scalar(out=vg, in0=vg, scalar1=st[:, 0:1], scalar2=st[:, 1:2],
                                                                     op0=ALU.subtract, op1=ALU.mult), reads=[b_vg, b_st], writes=[b_vg])
                    (vh, b_vh) = vh_rot.next()
                    add("dve", lambda e, vh=vh, vg=vg: e.tensor_tensor(out=vh, in0=vg, in1=gamrow, op=ALU.mult),
                        reads=[b_vg, b_gamrow], writes=[b_vh])
                    (bk2, b_bk2) = psum()
                    for g in range(4):
                        add("pe", lambda e, bk2=bk2, g=g, vh=vh: e.matmul(
                            bk2[:, g * 128:(g + 1) * 128], lhsT=vh[:, g * 128:(g + 1) * 128], rhs=wsT[:, g, :],
                            start=True, stop=True), reads=[b_vh, b_wsT], writes=[b_bk2])
                    (yt, b_yt) = yt_rot.next()
                    add("dve", lambda e, yt=yt, bk2=bk2: e.tensor_tensor(out=yt, in0=bk2[:, :], in1=B2.rearrange("p g t -> p (g t)"), op=ALU.add),
                        reads=[b_bk2, b_B2], writes=[b_yt])
                    add("dve", lambda e, yt=yt, uT=uT, t=t, tok=tok: e.tensor_tensor(
                        out=ybT[:, :, tok], in0=yt.rearrange("p (g t) -> p g t", g=4), in1=uT[:, :, t * 128:(t + 1) * 128], op=ALU.mult),
                        reads=[b_yt, b_uT], writes=[b_ybT])

                emit_interleaved([record(lambda c=c: c_uchunk(c)) for c in range(4)])
                emit_interleaved([record(lambda t=t: c_tile(t)) for t in (0, 1)])
                emit_interleaved([record(lambda t=t: c_tile(t)) for t in (2, 3)])
            if dev:
                (dd, b_dd) = A.alloc("dbgyb", [4, S], F32)
                add("dve", lambda e, dd=dd: e.tensor_copy(out=dd, in_=ybT), reads=[b_ybT], writes=[b_dd])
                dma("sp", dbg["yb"][b], dd, [b_dd], [B_OUT])
            A.release(m_c)

            m_d = A.mark()
            wgt, b_wgt = A.alloc("wgt", [KC, 2048], BF16)
            woa, b_woa = A.alloc("woa", [4, D], BF16)
            wob, b_wob = A.alloc("wob", [4, D], BF16)
            wo, b_wo = A.alloc("wo", [KC, D], BF16)
            dma("pool", wgt, w_in.rearrange("(kc p) n -> p kc n", p=128)[:, :, 3328:5376], [], [b_wgt])
            dma("pool", woa[0:64], w_oa.rearrange("(h p) n -> p h n", p=64), [], [b_woa])
            dma("pool", wob, w_ob.rearrange("(g p) n -> p g n", p=128), [], [b_wob])
            dma("pool", wo, w_o.rearrange("(kc p) n -> p kc n", p=128), [], [b_wo])
            mg_rot = Rot("merged", [KC, 512], BF16, 2)
            sg_rot = Rot("sg", [512], F32, 4)
            mm_rot = Rot("mm", [512], F32, 4)
            xr_rot = Rot("xr", [D], F32, 2)
            xs_rot = Rot("xsd", [KC, 512], BF16, 2)
            for k in range(NBK):
                blk = slice(k * 512, (k + 1) * 512)
                (mg, b_mg) = mg_rot.next()
                (xs, b_xs) = xs_rot.next()
                dma("sp", xs, XN[b, :, :, blk], [B_XN[k]], [b_xs])
                for c in range(KC):
                    (bka, b_bka) = psum()
                    (bkb, b_bkb) = psum()
                    (bkA, b_bkA) = psum()
                    (bkB, b_bkB) = psum()
                    for kc in range(KC):
                        add("pe", lambda e, bka=bka, kc=kc, c=c, xs=xs: e.matmul(
                            bka[:, :], lhsT=wgt[:, kc, c * 128:(c + 1) * 128], rhs=xs[:, kc, :],
                            start=(kc == 0), stop=(kc == KC - 1)), reads=[b_wgt, b_xs], writes=[b_bka])
                    for kc in range(KC):
                        add("pe", lambda e, bkb=bkb, kc=kc, c=c, xs=xs: e.matmul(
                            bkb[:, :], lhsT=wgt[:, kc, 1024 + c * 128:1024 + (c + 1) * 128], rhs=xs[:, kc, :],
                            start=(kc == 0), stop=(kc == KC - 1)), reads=[b_wgt, b_xs], writes=[b_bkb])
                    for hh in range(4):
                        add("pe", lambda e, bkA=bkA, hh=hh, c=c, blk=blk: e.matmul(
                            bkA[:, :], lhsT=woa[0:64, hh, c * 128:(c + 1) * 128], rhs=yaT[0:64, hh, blk],
                            start=(hh == 0), stop=(hh == 3)), reads=[b_woa, b_yaT], writes=[b_bkA])
                    for g in range(4):
                        add("pe", lambda e, bkB=bkB, g=g, c=c, blk=blk: e.matmul(
                            bkB[:, :], lhsT=wob[:, g, c * 128:(c + 1) * 128], rhs=ybT[:, g, blk],
                            start=(g == 0), stop=(g == 3)), reads=[b_wob, b_ybT], writes=[b_bkB])
                    (sa, b_sa) = sg_rot.next()
                    (sb_, b_sb) = sg_rot.next()
                    add("act", lambda e, sa=sa, bka=bka, c=c: e.activation(out=sa, in_=bka[:, :], func=AF.Sigmoid, bias=bgt_t[:, c:c + 1], scale=1.0),
                        reads=[b_bka, b_bgt], writes=[b_sa])
                    add("act", lambda e, sb_=sb_, bkb=bkb, c=c: e.activation(out=sb_, in_=bkb[:, :], func=AF.Sigmoid, bias=bgt_t[:, 8 + c:9 + c], scale=1.0),
                        reads=[b_bkb, b_bgt], writes=[b_sb])
                    (m1, b_m1) = mm_rot.next()
                    (m2, b_m2) = mm_rot.next()
                    add("dve", lambda e, m1=m1, sa=sa, bkA=bkA: e.tensor_tensor(out=m1, in0=bkA[:, :], in1=sa, op=ALU.mult),
                        reads=[b_bkA, b_sa], writes=[b_m1])
                    add("dve", lambda e, m2=m2, sb_=sb_, bkB=bkB: e.tensor_tensor(out=m2, in0=bkB[:, :], in1=sb_, op=ALU.mult),
                        reads=[b_bkB, b_sb], writes=[b_m2])
                    add("dve", lambda e, m1=m1, m2=m2, mg=mg, c=c: e.tensor_tensor(out=mg[:, c, :], in0=m1, in1=m2, op=ALU.add),
                        reads=[b_m1, b_m2], writes=[b_mg])
                for t in range(4):
                    ti = k * 4 + t
                    gi = b * NT + ti
                    (xr, b_xr) = xr_rot.next()
                    (h1t, b_h1t) = (xr, b_xr)
                    dma("sp", xr, x[b, ti * 128:(ti + 1) * 128, :], [], [b_xr])
                    for n in range(2):
                        (bk, b_bk) = psum()
                        for c in range(KC):
                            add("pe", lambda e, bk=bk, c=c, t=t, n=n, mg=mg: e.matmul(
                                bk[:, :], lhsT=mg[:, c, t * 128:(t + 1) * 128], rhs=wo[:, c, n * 512:(n + 1) * 512],
                                start=(c == 0), stop=(c == KC - 1)), reads=[b_mg, b_wo], writes=[b_bk])
                        add("dve", lambda e, bk=bk, xr=xr, h1t=h1t, n=n: e.tensor_tensor(
                            out=h1t[:, n * 512:(n + 1) * 512], in0=bk[:, :], in1=xr[:, n * 512:(n + 1) * 512], op=ALU.add),
                            reads=[b_bk, b_xr], writes=[b_h1t])
                    dma("sp", H[gi * 128:(gi + 1) * 128, :], h1t, [b_h1t], [B_H[gi]])
                    if dev:
                        dma("sp", dbg["h1"][gi * 128:(gi + 1) * 128, :], h1t, [b_h1t], [B_OUT])
            A.release(m_d)
            A.release(persist_mark)
            A.release_top()

            m_e = A.mark()
            kT, b_kT = A.alloc("kT", [KC, NMEM], BF16)
            vm, b_vm = A.alloc("vm", [2, D], BF16)
            wq, b_wq = A.alloc("wq", [KC, D], BF16)
            wox, b_wox = A.alloc("wox", [KC, D], BF16)
            dma("pool", wq, w_qx.rearrange("(kc p) n -> p kc n", p=128), [], [b_wq])
            dma("pool", wox, w_ox.rearrange("(kc p) n -> p kc n", p=128), [], [b_wox])
            m_e0 = A.mark()
            wkv, b_wkv = A.alloc("wkv", [KC, 2 * D], BF16)
            dma("pool", wkv, w_kvx.rearrange("(kc p) n -> p kc n", p=128), [], [b_wkv])
            gmem_t, b_gmem = A.alloc("gmem", [D], F32)
            dma("sp", gmem_t, g_mem.partition_broadcast(128), [], [b_gmem])
            mnT, b_mnT = A.alloc("mnT", [KC, NMEM], BF16)
            mt_rot = Rot("mt", [D], F32, 2)
            mb_rot = Rot("mb", [D], BF16, 2)
            small = Rot("ssm", [2], F32, 4)
            junk = A.alloc("junkm", [D], BF16)
            for i in range(2):
                (mt, b_mt) = mt_rot.next()
                (mb, b_mb) = mb_rot.next()
                dma("sp", mt, mem[b, i * 128:(i + 1) * 128, :], [], [b_mt])
                rms_tile(mt, b_mt, gmem_t, b_gmem, mb, b_mb, small, junk)
                transpose_to(mb, b_mb, mnT[:, :, i * 128:(i + 1) * 128], b_mnT)
            for oc in range(KC):
                (bk, b_bk) = psum()
                for kc in range(KC):
                    add("pe", lambda e, bk=bk, kc=kc, oc=oc: e.matmul(
                        bk[:, 0:NMEM], lhsT=wkv[:, kc, oc * 128:(oc + 1) * 128], rhs=mnT[:, kc, :],
                        start=(kc == 0), stop=(kc == KC - 1)), reads=[b_wkv, b_mnT], writes=[b_bk])
                add("act", lambda e, bk=bk, oc=oc: e.copy(out=kT[:, oc, :], in_=bk[:, 0:NMEM]), reads=[b_bk], writes=[b_kT])
            for mchunk in range(2):
                for n in range(2):
                    (bk, b_bk) = psum()
                    for kc in range(KC):
                        add("pe", lambda e, bk=bk, kc=kc, mchunk=mchunk, n=n: e.matmul(
                            bk[:, :], lhsT=mnT[:, kc, mchunk * 128:(mchunk + 1) * 128], rhs=wkv[:, kc, D + n * 512:D + (n + 1) * 512],
                            start=(kc == 0), stop=(kc == KC - 1)), reads=[b_wkv, b_mnT], writes=[b_bk])
                    add("act", lambda e, bk=bk, mchunk=mchunk, n=n: e.copy(out=vm[:, mchunk, n * 512:(n + 1) * 512], in_=bk[:, :]),
                        reads=[b_bk], writes=[b_vm])
            A.release(m_e0)

            gx_t, b_gx = A.alloc("gx", [D], F32)
            gmoe_t, b_gmoe = A.alloc("gmoe", [D], F32)
            dma("sp", gx_t, g_x.partition_broadcast(128), [], [b_gx])
            dma("sp", gmoe_t, g_moe.partition_broadcast(128), [], [b_gmoe])
            h_rot = Rot("ht", [D], F32, 8)
            hb_rot = Rot("hb", [D], BF16, 4)
            small = Rot("sse", [2], F32, 8)
            junk = A.alloc("junke", [D], BF16)
            hnT_rot = Rot("hnT", [KC, 512], BF16, 2)
            qT_rot = Rot("qT", [KC, 512], BF16, 2)
            oT_rot = Rot("oT", [KC, 512], BF16, 2)
            px_rot = Rot("pxx", [2, 512], BF16, 3)
            rd_rot = Rot("rdx", [512], F32, 2)
            h2_rot = Rot("h2t", [D], F32, 2)
            hn3f_rot = Rot("hn3f", [D], F32, 2)
            hn3b_rot = Rot("hn3b", [D], BF16, 2)
            hn3T_rot = Rot("hn3T", [KC, 128], F32, 2)
            rs_rot = Rot("rsm", [16], F32, 4)
            ls_rot = Rot("ls", [36], F32, 2)
            el_rot = Rot("el", [32], F32, 2)
            mk_rot = Rot("mk", [3, 32], F32, 2)
            mkb_rot = Rot("mkb", [32], BF16, 2)
            t8_rot = Rot("t8", [8], F32, 2)
            rec_rot = Rot("rec", [2, 2], I32, 2)
            dsc_rot = Rot("dsc", [2], I32, 2)

            def e_stage1(k, b=b):
                (hnT, b_hnT) = hnT_rot.next()
                hts = []

                def e_tile(t):
                    gi = b * NT + k * 4 + t
                    (ht, b_ht) = h_rot.next()
                    (hb, b_hb) = hb_rot.next()
                    dma("sp", ht, H[gi * 128:(gi + 1) * 128, :], [B_H[gi]], [b_ht])
                    rms_tile(ht, b_ht, gx_t, b_gx, hb, b_hb, small, junk)
                    transpose_to(hb, b_hb, hnT[:, :, t * 128:(t + 1) * 128], b_hnT, eng="act" if t % 2 == 0 else "dve")
                    hts.append((ht, b_ht, gi))

                emit_interleaved([record(lambda t=t: e_tile(t)) for t in range(4)])
                (qT, b_qT) = qT_rot.next()
                for oc in range(KC):
                    (bk, b_bk) = psum()
                    for kc in range(KC):
                        add("pe", lambda e, bk=bk, kc=kc, oc=oc, hnT=hnT: e.matmul(
                            bk[:, :], lhsT=wq[:, kc, oc * 128:(oc + 1) * 128], rhs=hnT[:, kc, :],
                            start=(kc == 0), stop=(kc == KC - 1)), reads=[b_wq, b_hnT], writes=[b_bk])
                    if oc % 2 == 0:
                        add("act", lambda e, bk=bk, oc=oc, qT=qT: e.copy(out=qT[:, oc, :], in_=bk[:, :]), reads=[b_bk], writes=[b_qT])
                    else:
                        add("dve", lambda e, bk=bk, oc=oc, qT=qT: e.tensor_copy(out=qT[:, oc, :], in_=bk[:, :]), reads=[b_bk], writes=[b_qT])
                return (hts, qT, b_qT)

            def e_stage2(k, st1, b=b):
                (hts, qT, b_qT) = st1
                pending = []
                outer_add = cur_add[0]

                def flush_router():
                    n1 = max(len(p[0]) for p in pending)
                    for i_ in range(n1):
                        for p in pending:
                            if i_ < len(p[0]):
                                outer_add(*p[0][i_])
                    for p in pending:
                        for it in p[1]:
                            outer_add(*it)
                    pending.clear()
                (oT, b_oT) = oT_rot.next()
                pxs = {}

                def e_scores(h):
                    (px, b_px) = px_rot.next()
                    pxs[h] = (px, b_px)
                    for mchunk in range(2):
                        (bk, b_bk) = psum()
                        for cc in range(2):
                            add("pe", lambda e, bk=bk, cc=cc, h=h, mchunk=mchunk, qT=qT: e.matmul(
                                bk[:, :], lhsT=kT[:, 2 * h + cc, mchunk * 128:(mchunk + 1) * 128], rhs=qT[:, 2 * h + cc, :],
                                start=(cc == 0), stop=(cc == 1)), reads=[b_kT, b_qT], writes=[b_bk])
                        add("act", lambda e, bk=bk, px=px, mchunk=mchunk: e.activation(out=px[:, mchunk, :], in_=bk[:, :], func=AF.Exp, scale=1.0 / 16.0),
                            reads=[b_bk], writes=[b_px])

                def e_pv(h):
                    (px, b_px) = pxs[h]
                    (bkd, b_bkd) = psum()
                    for mchunk in range(2):
                        add("pe", lambda e, bkd=bkd, px=px, mchunk=mchunk: e.matmul(
                            bkd[:, :], lhsT=ones_b, rhs=px[:, mchunk, :], start=(mchunk == 0), stop=(mchunk == 1)),
                            reads=[b_ones, b_px], writes=[b_bkd])
                    (rd, b_rd) = rd_rot.next()
                    add("dve", lambda e, rd=rd, bkd=bkd: e.reciprocal(out=rd, in_=bkd[:, :]), reads=[b_bkd], writes=[b_rd])
                    for cc in range(2):
                        (bk, b_bk) = psum()
                        for mchunk in range(2):
                            add("pe", lambda e, bk=bk, px=px, mchunk=mchunk, h=h, cc=cc: e.matmul(
                                bk[:, :], lhsT=vm[:, mchunk, (2 * h + cc) * 128:(2 * h + cc + 1) * 128], rhs=px[:, mchunk, :],
                                start=(mchunk == 0), stop=(mchunk == 1)), reads=[b_vm, b_px], writes=[b_bk])
                        add("dve", lambda e, bk=bk, rd=rd, oT=oT, h=h, cc=cc: e.tensor_tensor(
                            out=oT[:, 2 * h + cc, :], in0=bk[:, :], in1=rd, op=ALU.mult), reads=[b_bk, b_rd], writes=[b_oT])

                e_scores(0)
                for h in range(4):
                    if h + 1 < 4:
                        e_scores(h + 1)
                    e_pv(h)
                for t in range(4):
                    (ht, b_ht, gi) = hts[t]
                    (h2t, b_h2t) = h2_rot.next()
                    for n in range(2):
                        (bk, b_bk) = psum()
                        for c in range(KC):
                            add("pe", lambda e, bk=bk, c=c, t=t, n=n, oT=oT: e.matmul(
                                bk[:, :], lhsT=oT[:, c, t * 128:(t + 1) * 128], rhs=wox[:, c, n * 512:(n + 1) * 512],
                                start=(c == 0), stop=(c == KC - 1)), reads=[b_oT, b_wox], writes=[b_bk])
                        add("dve", lambda e, bk=bk, ht=ht, h2t=h2t, n=n: e.tensor_tensor(
                            out=h2t[:, n * 512:(n + 1) * 512], in0=bk[:, :], in1=ht[:, n * 512:(n + 1) * 512], op=ALU.add),
                            reads=[b_bk, b_ht], writes=[b_h2t])
                    dma("sp", H[gi * 128:(gi + 1) * 128, :], h2t, [b_h2t], [B_H[gi]])
                    if dev:
                        dma("sp", dbg["h2"][gi * 128:(gi + 1) * 128, :], h2t, [b_h2t], [B_OUT])
                    lst1, lst2 = [], []
                    cur_add[0] = make_recorder(lst1)
                    (hn3f, b_hn3f) = hn3f_rot.next()
                    (hn3b, b_hn3b) = hn3b_rot.next()
                    rms_tile(h2t, b_h2t, gmoe_t, b_gmoe, hn3b, b_hn3b, small, junk, dst_f32=hn3f, b_dst32=b_hn3f)
                    dma("sp", HN3[gi * 128:(gi + 1) * 128, :], hn3b, [b_hn3b], [B_HN3])
                    (hn3T, b_hn3T) = hn3T_rot.next()
                    (bk, b_bk) = psum()
                    for half in range(2):
                        for c4 in range(4):
                            c = half * 4 + c4
                            add("pe", lambda e, bk=bk, c=c, c4=c4, hn3f=hn3f: e.transpose(
                                out=bk[:, c4 * 128:(c4 + 1) * 128], in_=hn3f[:, c * 128:(c + 1) * 128], identity=ident_f),
                                reads=[b_hn3f, b_ident_f], writes=[b_bk])
                        add("act", lambda e, bk=bk, half=half, hn3T=hn3T: e.copy(
                            out=hn3T[:, half * 4:(half + 1) * 4, :], in_=bk[:, :].rearrange("p (c t) -> p c t", c=4)),
                            reads=[b_bk], writes=[b_hn3T])
                    (bk, b_bk) = psum()
                    (bkL, b_bkL) = (bk, b_bk)
                    for c in range(KC):
                        add("pe", lambda e, bk=bk, c=c, hn3T=hn3T: e.matmul(
                            bk[:, 0:36], lhsT=hn3T[:, c, :], rhs=wr_t[:, c, :], start=(c == 0), stop=(c == KC - 1)),
                            reads=[b_hn3T, b_wr], writes=[b_bk])
                    (ls, b_ls) = ls_rot.next()
                    (rs, b_rs) = rs_rot.next()
                    (el, b_el) = el_rot.next()
                    (mk, b_mk) = mk_rot.next()
                    (mkb, b_mkb) = mkb_rot.next()
                    (t8, b_t8) = t8_rot.next()
                    (rec, b_rec) = rec_rot.next()
                    add("dve", lambda e, ls=ls, bk=bk: e.tensor_tensor(out=ls, in0=bk[:, 0:36], in1=br_t, op=ALU.add),
                        reads=[b_bk, b_br], writes=[b_ls])
                    add("dve", lambda e, ls=ls, rs=rs: e.reduce_max(out=rs[:, 0:1], in_=ls[:, 0:4], axis=AX.X), reads=[b_ls], writes=[b_rs])
                    add("dve", lambda e, rs=rs: e.tensor_scalar(out=rs[:, 1:2], in0=rs[:, 0:1], scalar1=-1.0, scalar2=None, op0=ALU.mult),
                        reads=[b_rs], writes=[b_rs])
                    add("pool", lambda e, rs=rs: e.memset(rs[:, 2:3], 0.0), writes=[b_rs])
                    add("act", lambda e, ls=ls, rs=rs, mk=mk: e.activation(out=mk[:, 2, 0:4], in_=ls[:, 0:4], func=AF.Exp, bias=rs[:, 1:2], scale=1.0,
                                                                          accum_out=rs[:, 2:3]), reads=[b_ls, b_rs], writes=[b_mk, b_rs])
                    add("dve", lambda e, rs=rs: e.reciprocal(out=rs[:, 3:4], in_=rs[:, 2:3]), reads=[b_rs], writes=[b_rs])
                    add("dve", lambda e, ls=ls, rs=rs, mk=mk: e.tensor_scalar(out=mk[:, 2, 8:12], in0=ls[:, 0:4], scalar1=rs[:, 0:1], scalar2=None, op0=ALU.is_ge),
                        reads=[b_ls, b_rs], writes=[b_mk])
                    add("dve", lambda e, mk=mk: e.tensor_scalar(out=mk[:, 2, 8:12], in0=mk[:, 2, 8:12], scalar1=1e30, scalar2=-1e30, op0=ALU.mult, op1=ALU.add),
                        reads=[b_mk], writes=[b_mk])
                    add("dve", lambda e, ls=ls, el=el, mk=mk: e.tensor_tensor(
                        out=el.rearrange("p (g x) -> p g x", g=4), in0=ls[:, 4:36].rearrange("p (g x) -> p g x", g=4),
                        in1=mk[:, 2, 8:12].unsqueeze(2).to_broadcast([128, 4, 8]), op=ALU.add), reads=[b_ls, b_mk], writes=[b_el])
                    add("dve", lambda e, el=el, t8=t8: e.max(out=t8, in_=el), reads=[b_el], writes=[b_t8])
                    add("dve", lambda e, el=el, t8=t8, mk=mk: e.tensor_scalar(out=mk[:, 0, :], in0=el, scalar1=t8[:, 0:1], scalar2=None, op0=ALU.is_ge),
                        reads=[b_el, b_t8], writes=[b_mk])
                    add("dve", lambda e, el=el, t8=t8, mk=mk: e.tensor_scalar(out=mk[:, 1, :], in0=el, scalar1=t8[:, 1:2], scalar2=None, op0=ALU.is_ge),
                        reads=[b_el, b_t8], writes=[b_mk])
                    add("dve", lambda e, mk=mk, mkb=mkb: e.tensor_copy(out=mkb, in_=mk[:, 1, :]), reads=[b_mk], writes=[b_mkb])
                    add("dve", lambda e, mk=mk: e.tensor_tensor(out=mk[:, 1, :], in0=mk[:, 1, :], in1=mk[:, 0, :], op=ALU.subtract),
                        reads=[b_mk], writes=[b_mk])
                    add("dve", lambda e, t8=t8, rs=rs: e.tensor_tensor(out=rs[:, 4:5], in0=t8[:, 0:1], in1=t8[:, 1:2], op=ALU.subtract),
                        reads=[b_t8], writes=[b_rs])
                    add("act", lambda e, rs=rs: e.activation(out=rs[:, 5:6], in_=rs[:, 4:5], func=AF.Exp, scale=-1.0), reads=[b_rs], writes=[b_rs])
                    add("dve", lambda e, rs=rs: e.tensor_scalar(out=rs[:, 5:6], in0=rs[:, 5:6], scalar1=1.0, scalar2=None, op0=ALU.add), reads=[b_rs], writes=[b_rs])
                    add("dve", lambda e, rs=rs: e.reciprocal(out=rs[:, 5:6], in_=rs[:, 5:6]), reads=[b_rs], writes=[b_rs])
                    add("dve", lambda e, rs=rs: e.tensor_tensor(out=rs[:, 6:7], in0=rs[:, 5:6], in1=rs[:, 3:4], op=ALU.mult), reads=[b_rs], writes=[b_rs])
                    add("dve", lambda e, rs=rs: e.tensor_tensor(out=rs[:, 7:8], in0=rs[:, 3:4], in1=rs[:, 6:7], op=ALU.subtract), reads=[b_rs], writes=[b_rs])
                    (bkp, b_bkp) = (bkL, b_bkL)
                    add("pe", lambda e, bkp=bkp, mkb=mkb: e.matmul(bkp[:, 64:96], lhsT=ltri_b, rhs=mkb, start=True, stop=True),
                        reads=[b_ltri, b_mkb], writes=[b_bkp])
                    add("pe", lambda e, bkp=bkp, mkb=mkb: e.matmul(bkp[:, 96:128], lhsT=ones_b, rhs=mkb, start=True, stop=True),
                        reads=[b_ones, b_mkb], writes=[b_bkp])
                    cur_add[0] = make_recorder(lst2)
                    add("dve", lambda e, bkp=bkp, mk=mk: e.tensor_tensor(out=mk[:, 2, :], in0=bkp[:, 64:96], in1=base_t, op=ALU.add),
                        reads=[b_bkp, b_base], writes=[b_mk])
                    add("dve", lambda e, bkp=bkp: e.tensor_tensor(out=base_t, in0=bkp[:, 96:128], in1=base_t, op=ALU.add),
                        reads=[b_bkp, b_base], writes=[b_base])
                    add("dve", lambda e, el=el, mk=mk: e.tensor_scalar(out=el, in0=mk[:, 2, :], scalar1=float(C), scalar2=1e6, op0=ALU.is_ge, op1=ALU.mult),
                        reads=[b_mk], writes=[b_el])
                    add("dve", lambda e, el=el, mk=mk: e.tensor_tensor(out=mk[:, 2, :], in0=mk[:, 2, :], in1=el, op=ALU.add), reads=[b_mk, b_el], writes=[b_mk])
                    add("dve", lambda e, mk=mk: e.tensor_tensor(out=mk[:, 2, :], in0=mk[:, 2, :], in1=ec_t, op=ALU.add), reads=[b_mk, b_ec], writes=[b_mk])
                    for sl in range(2):
                        add("dve", lambda e, mk=mk, el=el, sl=sl: e.tensor_tensor(out=el, in0=mk[:, sl, :], in1=mk[:, 2, :], op=ALU.mult),
                            reads=[b_mk], writes=[b_el])
                        add("dve", lambda e, el=el, rs=rs, sl=sl: e.reduce_sum(out=rs[:, 8 + sl:9 + sl], in_=el, axis=AX.X), reads=[b_el], writes=[b_rs])
                    add("dve", lambda e, rs=rs: e.tensor_scalar_min(out=rs[:, 8:10], in0=rs[:, 8:10], scalar1=float(NSLOT)), reads=[b_rs], writes=[b_rs])
                    add("dve", lambda e, rs=rs, gi=gi: e.tensor_copy(out=dest_all[:, gi, :], in_=rs[:, 8:10]), reads=[b_rs], writes=[b_dest])
                    for sl in range(2):
                        add("dve", lambda e, rec=rec, sl=sl, rs=rs: e.tensor_copy(out=rec[:, sl, 1:2].bitcast(F32), in_=rs[:, 6 + sl:7 + sl]),
                            reads=[b_rs], writes=[b_rec])
                        add("dve", lambda e, rec=rec, sl=sl, gi=gi: e.tensor_scalar(out=rec[:, sl, 0:1], in0=iota_f[:, 0:1], scalar1=float(gi * 128),
                                                                                    scalar2=None, op0=ALU.add), reads=[b_iota], writes=[b_rec])
                    for sl in range(2):
                        add("pool", lambda e, rec=rec, sl=sl, gi=gi: e.indirect_dma_start(
                            out=SLOT, out_offset=bass.IndirectOffsetOnAxis(ap=dest_all[:, gi, sl:sl + 1], axis=0),
                            in_=rec[:, sl, :], in_offset=None, bounds_check=PR["bc_slot"], oob_is_err=False),
                            reads=[b_rec, b_dest], writes=[B_SLOT], dma=True)
                    cur_add[0] = outer_add
                    pending.append((lst1, lst2))
                    if len(pending) == 1:
                        flush_router()
            stbox = []

            def run_s1(k_):
                psum_pool[0] = "a"
                stbox.append(e_stage1(k_))
                psum_pool[0] = None

            def run_s2(k_, st_):
                psum_pool[0] = "b"
                e_stage2(k_, st_)
                psum_pool[0] = None

            run_s1(0)
            for k in range(NBK):
                st_cur = stbox[k]
                la = record(lambda: run_s1(k + 1)) if k + 1 < NBK else []
                lb = record(lambda: run_s2(k, st_cur))
                emit_merged(la, lb)
            A.release(m_e)

        if dev:
            dma("sp", dbg["dest"], dest_all, [b_dest], [B_OUT])

        dma("sp", cnt_out, base_t, [b_base], [B_OUT])
        A.release(persist_mark)
        m_f = A.mark()
        wge_rot = Rot("wge", [KC, FF], BF16, 3)
        wue_rot = Rot("wue", [KC, FF], BF16, 3)
        wde_rot = Rot("wde", [4, D], BF16, 3)
        srec_rot = Rot("srec", [2], I32, 2 * CT)
        xe_rot = Rot("xe", [D], BF16, 4)
        xeT_rot = Rot("xeT", [KC, C], BF16, 2)
        aT_rot = Rot("aT", [4, C], BF16, 2)
        sl_rot = Rot("silu", [512], F32, 3)
        ys_rot = Rot("ysb", [D], F32, 3)
        segs = [(s0, min(512, C - s0)) for s0 in range(0, C, 512)]

        def f_load(ex):
            (wge, b_wge) = wge_rot.next()
            (wue, b_wue) = wue_rot.next()
            (wde, b_wde) = wde_rot.next()
            dma("pool", wge, w_ge[ex].rearrange("(kc p) f -> p kc f", p=128), [], [b_wge])
            dma("pool", wue, w_ue[ex].rearrange("(kc p) f -> p kc f", p=128), [], [b_wue])
            dma("pool", wde, w_de[ex].rearrange("(f p) n -> p f n", p=128), [], [b_wde])
            (xeT, b_xeT) = xeT_rot.next()
            srecs = []
            for j in range(CT):
                (srec, b_srec) = srec_rot.next()
                (xe, b_xe) = xe_rot.next()
                row0 = ex * C + j * 128
                dma("sp", srec, SLOT[row0:row0 + 128, :], [B_SLOT], [b_srec])
                add("pool", lambda e, xe=xe, srec=srec: e.indirect_dma_start(
                    out=xe, out_offset=None, in_=HN3, in_offset=bass.IndirectOffsetOnAxis(ap=srec[:, 0:1], axis=0),
                    bounds_check=PR["bc_tok"], oob_is_err=False),
                    reads=[b_srec, B_HN3], writes=[b_xe], dma=True)
                transpose_to(xe, b_xe, xeT[:, :, j * 128:(j + 1) * 128], b_xeT, eng="act" if j % 2 == 0 else "dve")
                srecs.append((srec, b_srec))
            return (wge, b_wge, wue, b_wue, wde, b_wde, xeT, b_xeT, srecs)

        def f_gateup(st):
            (wge, b_wge, wue, b_wue, wde, b_wde, xeT, b_xeT, srecs) = st
            (aT, b_aT) = aT_rot.next()
            for f in range(4):
                for (s0, sn) in segs:
                    (bkg, b_bkg) = psum()
                    (bku, b_bku) = psum()
                    for kc in range(KC):
                        add("pe", lambda e, bkg=bkg, kc=kc, f=f, s0=s0, sn=sn, wge=wge, xeT=xeT: e.matmul(
                            bkg[:, 0:sn], lhsT=wge[:, kc, f * 128:(f + 1) * 128], rhs=xeT[:, kc, s0:s0 + sn],
                            start=(kc == 0), stop=(kc == KC - 1)), reads=[b_wge, b_xeT], writes=[b_bkg])
                    for kc in range(KC):
                        add("pe", lambda e, bku=bku, kc=kc, f=f, s0=s0, sn=sn, wue=wue, xeT=xeT: e.matmul(
                            bku[:, 0:sn], lhsT=wue[:, kc, f * 128:(f + 1) * 128], rhs=xeT[:, kc, s0:s0 + sn],
                            start=(kc == 0), stop=(kc == KC - 1)), reads=[b_wue, b_xeT], writes=[b_bku])
                    (sl_, b_sl) = sl_rot.next()
                    add("act", lambda e, sl_=sl_, bkg=bkg, sn=sn: e.activation(out=sl_[:, 0:sn], in_=bkg[:, 0:sn], func=AF.Silu),
                        reads=[b_bkg], writes=[b_sl])
                    add("dve", lambda e, sl_=sl_, bku=bku, aT=aT, f=f, s0=s0, sn=sn: e.tensor_tensor(
                        out=aT[:, f, s0:s0 + sn], in0=bku[:, 0:sn], in1=sl_[:, 0:sn], op=ALU.mult), reads=[b_bku, b_sl], writes=[b_aT])
            return (aT, b_aT)

        def f_down(ex, st, aTb):
            (wge, b_wge, wue, b_wue, wde, b_wde, xeT, b_xeT, srecs) = st
            (aT, b_aT) = aTb
            for j in range(CT):
                (srec, b_srec) = srecs[j]
                (ysb, b_ysb) = ys_rot.next()
                row0 = ex * C + j * 128
                for n in range(2):
                    (bk, b_bk) = psum()
                    for f in range(4):
                        add("pe", lambda e, bk=bk, f=f, j=j, n=n, aT=aT, wde=wde: e.matmul(
                            bk[:, :], lhsT=aT[:, f, j * 128:(j + 1) * 128], rhs=wde[:, f, n * 512:(n + 1) * 512],
                            start=(f == 0), stop=(f == 3)), reads=[b_aT, b_wde], writes=[b_bk])
                    if n == 0:
                        add("act", lambda e, bk=bk, ysb=ysb, n=n, srec=srec: e.activation(
                            out=ysb[:, n * 512:(n + 1) * 512], in_=bk[:, :], func=AF.Copy, scale=srec[:, 1:2].bitcast(F32)),
                            reads=[b_bk, b_srec], writes=[b_ysb])
                    else:
                        add("dve", lambda e, bk=bk, ysb=ysb, n=n, srec=srec: e.tensor_scalar(
                            out=ysb[:, n * 512:(n + 1) * 512], in0=bk[:, :], scalar1=srec[:, 1:2].bitcast(F32), scalar2=None, op0=ALU.mult),
                            reads=[b_bk, b_srec], writes=[b_ysb])
                dma("sp", YS[row0:row0 + 128, :], ysb, [b_ysb], [B_YS[ex]])

        st_next = f_load(0)
        for ex in range(NEXP):
            st_cur = st_next
            aTb = f_gateup(st_cur)
            if ex + 1 < NEXP:
                st_next = f_load(ex + 1)
            f_down(ex, st_cur, aTb)
        A.release(m_f)

        m_g = A.mark()
        hg_rot = Rot("hg", [D], F32, 5)
        y_rot = Rot("yg", [2, D], F32, 5)
        og_rot = Rot("og", [D], F32, 3)
        small = Rot("ssg", [2], F32, 4)
        junk = A.alloc("junkg", [D], BF16)
        gfin_t, b_gfin = A.alloc("gfin", [D], F32)
        dma("sp", gfin_t, g_fin.partition_broadcast(128), [], [b_gfin])
        g_pref = {}

        def g_prefetch(gi):
            (hg, b_hg) = hg_rot.next()
            (yg, b_yg) = y_rot.next()
            dma("sp", hg, H[gi * 128:(gi + 1) * 128, :], [B_H[gi]], [b_hg])
            for sl in range(2):
                add("pool", lambda e, yg=yg, sl=sl, gi=gi: e.indirect_dma_start(
                    out=yg[:, sl, :], out_offset=None, in_=YS, in_offset=bass.IndirectOffsetOnAxis(ap=dest_all[:, gi, sl:sl + 1], axis=0),
                    bounds_check=PR["bc_slot"], oob_is_err=False),
                    reads=[b_dest] + B_YS, writes=[b_yg], dma=True)
            g_pref[gi] = (hg, b_hg, yg, b_yg)

        G_AHEAD = 3
        for gi in range(min(G_AHEAD, NTT)):
            g_prefetch(gi)
        for gi in range(NTT):
            if gi + G_AHEAD < NTT:
                g_prefetch(gi + G_AHEAD)
            (hg, b_hg, yg, b_yg) = g_pref.pop(gi)
            (og, b_og) = og_rot.next()
            add("dve", lambda e, hg=hg, yg=yg: e.tensor_tensor(out=hg, in0=hg, in1=yg[:, 0, :], op=ALU.add), reads=[b_hg, b_yg], writes=[b_hg])
            add("dve", lambda e, hg=hg, yg=yg: e.tensor_tensor(out=hg, in0=hg, in1=yg[:, 1, :], op=ALU.add), reads=[b_hg, b_yg], writes=[b_hg])
            (ss, b_ss) = small.next()
            (jk, b_jk) = junk
            add("pool", lambda e, ss=ss: e.memset(ss, 0.0), writes=[b_ss])
            add("act", lambda e, ss=ss, hg=hg, jk=jk: e.activation(out=jk, in_=hg, func=AF.Square, accum_out=ss[:, 0:1]),
                reads=[b_hg], writes=[b_jk, b_ss])
            add("act", lambda e, ss=ss: e.activation(out=ss[:, 1:2], in_=ss[:, 0:1], func=AF.Ln, bias=eps_rms[:, 0:1], scale=1.0 / D),
                reads=[b_ss, b_eps], writes=[b_ss])
            add("act", lambda e, ss=ss: e.activation(out=ss[:, 0:1], in_=ss[:, 1:2], func=AF.Exp, scale=-0.5), reads=[b_ss], writes=[b_ss])
            add("dve", lambda e, ss=ss, hg=hg, og=og: e.scalar_tensor_tensor(out=og, in0=hg, scalar=ss[:, 0:1], in1=gfin_t, op0=ALU.mult, op1=ALU.mult),
                reads=[b_hg, b_ss, b_gfin], writes=[b_og])
            dma("sp", out[gi * 128:(gi + 1) * 128, :], og, [b_og], [B_OUT])
        A.release(m_g)
        if dev:
            dsl, b_dsl = A.alloc("dsl", [NSLOT // 128, 2], I32)
            dma("sp", dsl, SLOT[0:NSLOT, :].rearrange("(p a) c -> p a c", p=128), [B_SLOT], [b_dsl])
            dma("sp", dbg["slot"].rearrange("(p a) c -> p a c", p=128), dsl, [b_dsl], [B_OUT])
            dy_rot = Rot("dy", [D], F32, 2)
            dh_rot = Rot("dh", [D], BF16, 2)
            for i_ in range((NSLOT + 128) // 128):
                (dy, b_dy) = dy_rot.next()
                dma("sp", dy, YS[i_ * 128:(i_ + 1) * 128, :], B_YS, [b_dy])
                dma("sp", dbg["ys"][i_ * 128:(i_ + 1) * 128, :], dy, [b_dy], [B_OUT])
            for i_ in range((TOK + 128) // 128):
                (dh, b_dh) = dh_rot.next()
                dma("sp", dh, HN3[i_ * 128:(i_ + 1) * 128, :], [B_HN3], [b_dh])
                dma("sp", dbg["hn3"][i_ * 128:(i_ + 1) * 128, :], dh, [b_dh], [B_OUT])

        Sc.emit(block, esems, dsems)
        build.stats = dict(nops=Sc.nops, peak=A.peak)
    return nc


def _consts(C):
    p = np.arange(128)
    half = 32
    inv_freq = (10000.0 ** (-np.arange(half, dtype=np.float32) / half)).astype(np.float32)
    c_invf = inv_freq[p % 32].reshape(128, 1).astype(np.float32)
    kk = np.arange(128)[:, None]
    qq = np.arange(128)[None, :]
    c_mask = np.concatenate([(kk <= qq), (kk >= qq)], axis=1).astype(np.float32)
    c_ltri = (kk < qq).astype(np.float32)
    c_ident = np.eye(128, dtype=np.float32)
    c_ec = np.broadcast_to((np.arange(32, dtype=np.float32) * C)[None, :], (128, 32)).copy()
    c_sel = np.zeros((65, 64), np.float32)
    c_sel[64, :] = 1.0
    c_iota = np.arange(128, dtype=np.float32).reshape(128, 1)
    return dict(c_invf=c_invf, c_mask=c_mask, c_ltri=c_ltri, c_ident=c_ident, c_ec=c_ec, c_sel=c_sel, c_iota=c_iota)


def _w_in_perm():
    cols = []
    for g in range(3):
        heads = range(4 * g, 4 * g + 4)
        for base in (0, 768):
            for part in (0, 32):
                for h in heads:
                    cols.extend(range(base + 64 * h + part, base + 64 * h + part + 32))
        cols.extend(range(1536 + 256 * g, 1536 + 256 * (g + 1)))
    cols.extend(range(2304, 5376))
    return np.asarray(cols)


def prep_weights(inp, C):
    f = lambda a: np.ascontiguousarray(np.asarray(a, dtype=np.float32))
    w = {}
    w["w_in"] = f(np.asarray(inp["w_in"])[0][:, _w_in_perm()])
    w["g_mix"] = f(inp["mix_norm_g"][0])
    w["g_x"] = f(inp["xattn_norm_g"][0])
    w["g_mem"] = f(inp["mem_norm_g"][0])
    w["g_moe"] = f(inp["moe_norm_g"][0])
    w["g_fin"] = f(inp["final_norm_g"])
    w["bgt"] = f(np.asarray(inp["b_gates"])[0].reshape(16, 128).T)
    w["ws_t"] = f(np.asarray(inp["w_spatial"])[0].transpose(0, 2, 1))
    w["bsp"] = f(np.asarray(inp["b_spatial"])[0].reshape(512))
    w["vgam"] = f(inp["v_norm_g"][0])
    w["vbet_t"] = f(np.asarray(inp["v_norm_b"])[0].reshape(4, 128).T)
    w["w_oa"] = f(inp["w_out_a"][0])
    w["w_ob"] = f(inp["w_out_b"][0])
    w["w_o"] = f(inp["w_out"][0])
    w["w_qx"] = f(inp["w_q_x"][0])
    w["w_kvx"] = f(inp["w_kv_x"][0])
    w["w_ox"] = f(inp["w_o_x"][0])
    w["w_r"] = f(np.concatenate([np.asarray(inp["w_router_grp"])[0], np.asarray(inp["w_router_exp"])[0]], axis=1))
    w["b_r"] = f(np.concatenate([np.asarray(inp["b_router_grp"])[0], np.asarray(inp["b_router_exp"])[0]], axis=0))
    w["w_ge"] = f(inp["w_gate_e"][0])
    w["w_ue"] = f(inp["w_up_e"][0])
    w["w_de"] = f(inp["w_down_e"][0])
    w.update(_consts(C))
    return w


def run(inp, n_cores, NB, S, C, dev=False):
    nc = build(NB, S, C, dev=dev)
    w = prep_weights(inp, C)
    x = np.asarray(inp["x"], dtype=np.float32)
    mem = np.asarray(inp["mem"], dtype=np.float32)
    pos = np.asarray(inp["positions"], dtype=np.int32)
    in_maps = []
    for c in range(n_cores):
        m = dict(w)
        m["x"] = np.ascontiguousarray(x[c * NB:(c + 1) * NB])
        m["mem"] = np.ascontiguousarray(mem[c * NB:(c + 1) * NB])
        m["pos"] = np.ascontiguousarray(pos[c * NB:(c + 1) * NB])
        in_maps.append(m)
    res = run_bass_kernel_spmd(nc, in_maps, core_ids=list(range(n_cores)))
    outs = [r["out"].reshape(NB, S, D) for r in res.results]
    try:
        print("[kernel] max routed rows per (core, expert):", [int(r["cnt"][0].max()) for r in res.results], "capacity", C, flush=True)
    except Exception:
        pass
    full = np.concatenate(outs, axis=0).astype(np.float32)
    if dev:
        return full, res.results
    return full


def kernel(**inputs):
    return run(inputs, n_cores=8, NB=2, S=4096, C=1024)
```
